# Optimizing a Trainium2 kernel written in Bass

```python
import jax, jax.numpy as jnp
from jax import lax
import numpy as np

D_MODEL = 1024
BATCH = 16
SEQ = 4096
DEPTH = 1

CONV_WIDTH = D_MODEL
CONV_K = 3
SSM_EXPAND = 2
D_INNER = SSM_EXPAND * D_MODEL
SSM_HEAD_DIM = 64
SSM_HEADS = D_INNER // SSM_HEAD_DIM
SSM_GROUPS = 4
SSM_STATE = 128
SSM_CONV_K = 4
SSM_CHUNK = 256
D_XBC = D_INNER + 2 * SSM_GROUPS * SSM_STATE
N_EXPERTS = 32
TOP_K = 4
D_FF = D_MODEL
SWIGLU_LIMIT = 7.0
SWIGLU_ALPHA = 1.702
MOE_BLOCK = 256
EPS = 1e-6
N_MOD = 6

IN_SIZES = (CONV_WIDTH, CONV_WIDTH, CONV_WIDTH, D_INNER, D_XBC, SSM_HEADS, D_MODEL, D_MODEL)
IN_SPLITS = tuple(int(s) for s in np.cumsum(IN_SIZES)[:-1])
D_IN_PROJ = int(sum(IN_SIZES))

kernel_name = 'hybrid_shortconv_ssd_moe_adaln'


def rms_norm(x, g):
    xf = x.astype(jnp.float32)
    y = xf * lax.rsqrt(jnp.mean(xf * xf, axis=-1, keepdims=True) + EPS)
    return (y * g.astype(jnp.float32)).astype(x.dtype)


def causal_depthwise_conv(u, w):
    k, ch = w.shape
    return lax.conv_general_dilated(
        u, w[:, None, :].astype(u.dtype), window_strides=(1,), padding=[(k - 1, 0)],
        dimension_numbers=('NWC', 'WIO', 'NWC'), feature_group_count=ch)


def ssd_chunked_scan(xh, dt, a, bm, cm):
    bsz, seq, nh, hp = xh.shape
    ng, ns = bm.shape[2], bm.shape[3]
    hg = nh // ng
    pad = (-seq) % SSM_CHUNK
    padw = lambda t: jnp.pad(t, [(0, 0), (0, pad)] + [(0, 0)] * (t.ndim - 2))
    xh, dt, bm, cm = padw(xh), padw(dt), padw(bm), padw(cm)
    nc = (seq + pad) // SSM_CHUNK
    L = SSM_CHUNK
    x_c = jnp.moveaxis(xh.reshape(bsz, nc, L, ng, hg, hp), 1, 0)
    dt_c = jnp.moveaxis(dt.reshape(bsz, nc, L, ng, hg), 1, 0)
    b_c = jnp.moveaxis(bm.reshape(bsz, nc, L, ng, ns), 1, 0)
    c_c = jnp.moveaxis(cm.reshape(bsz, nc, L, ng, ns), 1, 0)
    a_g = a.reshape(ng, hg)
    causal = jnp.tril(jnp.ones((L, L), dtype=bool))[None, :, :, None, None]

    def step(state, inp):
        xc, dtc, bc, cc = inp
        a_cum = jnp.cumsum(dtc * a_g, axis=1)
        seg = a_cum[:, :, None] - a_cum[:, None, :]
        decay = jnp.exp(jnp.where(causal, seg, -jnp.inf))
        cb = jnp.einsum('bign,bjgn->bijg', cc, bc)
        scores = cb[..., None] * decay
        xdt = xc * dtc[..., None]
        y_diag = jnp.einsum('bijgh,bjghp->bighp', scores, xdt)
        y_off = jnp.einsum('bign,bghpn->bighp', cc, state) * jnp.exp(a_cum)[..., None]
        decay_end = jnp.exp(a_cum[:, -1:] - a_cum)
        new_state = (state * jnp.exp(a_cum[:, -1])[..., None, None]
                     + jnp.einsum('bjgh,bjghp,bjgn->bghpn', decay_end, xdt, bc))
        return new_state, y_diag + y_off

    init = jnp.zeros((bsz, ng, hg, hp, ns), jnp.float32)
    _, y = lax.scan(step, init, (x_c, dt_c, b_c, c_c))
    y = jnp.moveaxis(y, 0, 1).reshape(bsz, nc * L, nh, hp)
    return y[:, :seq]


def gated_group_rms_norm(y, z, g):
    yf = y.astype(jnp.float32) * jax.nn.silu(z.astype(jnp.float32))
    yg = yf.reshape(yf.shape[:-1] + (SSM_GROUPS, D_INNER // SSM_GROUPS))
    yg = yg * lax.rsqrt(jnp.mean(yg * yg, axis=-1, keepdims=True) + EPS)
    return yg.reshape(yf.shape) * g.astype(jnp.float32)


def token_mixer(h, w_in, w_sconv, w_sconv_out, w_ssm_conv, b_ssm_conv, dt_bias, a_log,
                d_skip, g_ssm_norm, w_ssm_out, w_o):
    bsz, seq, _ = h.shape
    proj = h @ w_in
    sb, sc, sx, z, xbc, dt_raw, gate_a, gate_b = jnp.split(proj, IN_SPLITS, axis=-1)
    y_a = (sc * causal_depthwise_conv(sb * sx, w_sconv)) @ w_sconv_out
    xbc = jax.nn.silu(causal_depthwise_conv(xbc, w_ssm_conv) + b_ssm_conv)
    xs, bs, cs = jnp.split(xbc, (D_INNER, D_INNER + SSM_GROUPS * SSM_STATE), axis=-1)
    dt = jax.nn.softplus(dt_raw.astype(jnp.float32) + dt_bias.astype(jnp.float32))
    a = -jnp.exp(a_log.astype(jnp.float32))
    xh = xs.reshape(bsz, seq, SSM_HEADS, SSM_HEAD_DIM).astype(jnp.float32)
    y = ssd_chunked_scan(xh, dt, a,
                         bs.reshape(bsz, seq, SSM_GROUPS, SSM_STATE).astype(jnp.float32),
                         cs.reshape(bsz, seq, SSM_GROUPS, SSM_STATE).astype(jnp.float32))
    y = (y + d_skip.astype(jnp.float32)[:, None] * xh).reshape(bsz, seq, D_INNER)
    y_b = gated_group_rms_norm(y, z, g_ssm_norm).astype(h.dtype) @ w_ssm_out
    m = jax.nn.sigmoid(gate_a) * y_a + jax.nn.sigmoid(gate_b) * y_b
    return m @ w_o


def moe_ffn(h, w_router, b_router, w_gu, b_gu, w_down, b_down):
    bsz, seq, d = h.shape
    n_tok = bsz * seq
    n_rows = n_tok * TOP_K
    hf = h.reshape(n_tok, d)
    logits = (hf @ w_router + b_router).astype(jnp.float32)
    top_val, top_idx = lax.top_k(logits, TOP_K)
    top_w = jax.nn.softmax(top_val, axis=-1)
    e_flat = top_idx.reshape(-1).astype(jnp.int32)
    tok_flat = jnp.arange(n_rows, dtype=jnp.int32) // TOP_K
    w_flat = top_w.reshape(-1)
    order = jnp.argsort(e_flat)
    e_sorted = e_flat[order]
    counts = jnp.bincount(e_flat, length=N_EXPERTS)
    padded = (counts + MOE_BLOCK - 1) // MOE_BLOCK * MOE_BLOCK
    start = jnp.cumsum(counts) - counts
    pend = jnp.cumsum(padded)
    pstart = pend - padded
    dest = pstart[e_sorted] + (jnp.arange(n_rows, dtype=jnp.int32) - start[e_sorted])
    n_blocks = -(-n_rows // MOE_BLOCK) + N_EXPERTS
    r_tot = n_blocks * MOE_BLOCK
    row_tok = jnp.zeros((r_tot,), jnp.int32).at[dest].set(tok_flat[order])
    row_w = jnp.zeros((r_tot,), jnp.float32).at[dest].set(w_flat[order])
    block_e = jnp.searchsorted(pend, jnp.arange(n_blocks) * MOE_BLOCK, side='right')
    block_e = jnp.minimum(block_e, N_EXPERTS - 1).astype(jnp.int32)

    def expert_block(args):
        toks, e = args
        gu = hf[toks] @ w_gu[e] + b_gu[e]
        gate = jnp.minimum(gu[:, :D_FF], SWIGLU_LIMIT)
        up = jnp.clip(gu[:, D_FF:], -SWIGLU_LIMIT, SWIGLU_LIMIT)
        act = (up + 1) * gate * jax.nn.sigmoid(SWIGLU_ALPHA * gate)
        return act @ w_down[e] + b_down[e]

    ys = lax.map(expert_block, (row_tok.reshape(n_blocks, MOE_BLOCK), block_e))
    ys = ys.reshape(r_tot, d) * row_w[:, None].astype(ys.dtype)
    out = jax.ops.segment_sum(ys, row_tok, num_segments=n_tok)
    return out.reshape(bsz, seq, d)


def setup_inputs(seed: int = 0) -> dict:
    key = jax.random.key(seed)
    k = jax.random.split(key, 24)
    n = lambda kk, shape, s: jax.random.normal(kk, shape, jnp.float32) * s
    dt0 = jnp.exp(jax.random.uniform(k[8], (DEPTH, SSM_HEADS), jnp.float32,
                                     np.log(1e-3), np.log(1e-1)))
    return {
        'x': n(k[0], (BATCH, SEQ, D_MODEL), 1.0),
        'c': n(k[1], (BATCH, D_MODEL), 1.0),
        'w_ada': n(k[2], (DEPTH, D_MODEL, N_MOD * D_MODEL), 0.5 * D_MODEL ** -0.5),
        'b_ada': n(k[3], (DEPTH, N_MOD * D_MODEL), 0.02),
        'g_mix': 1.0 + n(k[4], (DEPTH, D_MODEL), 0.05),
        'w_in': n(k[5], (DEPTH, D_MODEL, D_IN_PROJ), D_MODEL ** -0.5),
        'w_sconv': n(k[6], (DEPTH, CONV_K, CONV_WIDTH), CONV_K ** -0.5),
        'w_sconv_out': n(k[7], (DEPTH, CONV_WIDTH, D_MODEL), CONV_WIDTH ** -0.5),
        'w_ssm_conv': n(k[9], (DEPTH, SSM_CONV_K, D_XBC), SSM_CONV_K ** -0.5),
        'b_ssm_conv': n(k[10], (DEPTH, D_XBC), 0.02),
        'dt_bias': dt0 + jnp.log(-jnp.expm1(-dt0)),
        'a_log': jnp.log(jax.random.uniform(k[11], (DEPTH, SSM_HEADS), jnp.float32, 1.0, 16.0)),
        'd_skip': 1.0 + n(k[12], (DEPTH, SSM_HEADS), 0.05),
        'g_ssm_norm': 1.0 + n(k[13], (DEPTH, D_INNER), 0.05),
        'w_ssm_out': n(k[14], (DEPTH, D_INNER, D_MODEL), D_INNER ** -0.5),
        'w_o': n(k[15], (DEPTH, D_MODEL, D_MODEL), D_MODEL ** -0.5),
        'g_ffn': 1.0 + n(k[16], (DEPTH, D_MODEL), 0.05),
        'w_router': n(k[17], (DEPTH, D_MODEL, N_EXPERTS), D_MODEL ** -0.5),
        'b_router': n(k[18], (DEPTH, N_EXPERTS), 0.01),
        'w_gu': n(k[19], (DEPTH, N_EXPERTS, D_MODEL, 2 * D_FF), D_MODEL ** -0.5),
        'b_gu': n(k[20], (DEPTH, N_EXPERTS, 2 * D_FF), 0.02),
        'w_down': n(k[21], (DEPTH, N_EXPERTS, D_FF, D_MODEL), D_FF ** -0.5),
        'b_down': n(k[22], (DEPTH, N_EXPERTS, D_MODEL), 0.02),
        'g_final': 1.0 + n(k[23], (D_MODEL,), 0.05),
    }


def reference(x, c, w_ada, b_ada, g_mix, w_in, w_sconv, w_sconv_out, w_ssm_conv, b_ssm_conv,
              dt_bias, a_log, d_skip, g_ssm_norm, w_ssm_out, w_o, g_ffn, w_router, b_router,
              w_gu, b_gu, w_down, b_down, g_final):
    cond = jax.nn.silu(c)
    for l in range(DEPTH):
        mod = (cond @ w_ada[l] + b_ada[l])[:, None, :]
        sh1, sc1, gt1, sh2, sc2, gt2 = jnp.split(mod, N_MOD, axis=-1)
        h = rms_norm(x, g_mix[l]) * (1 + sc1) + sh1
        x = x + gt1 * token_mixer(h, w_in[l], w_sconv[l], w_sconv_out[l], w_ssm_conv[l],
                                  b_ssm_conv[l], dt_bias[l], a_log[l], d_skip[l],
                                  g_ssm_norm[l], w_ssm_out[l], w_o[l])
        h = rms_norm(x, g_ffn[l]) * (1 + sc2) + sh2
        x = x + gt2 * moe_ffn(h, w_router[l], b_router[l], w_gu[l], b_gu[l], w_down[l], b_down[l])
    return rms_norm(x, g_final)
```

```python
import numpy as np
from contextlib import ExitStack
import concourse.bass as bass
import concourse.mybir as mybir
from concourse.bass_utils import run_bass_kernel_spmd

F32 = mybir.dt.float32
BF16 = mybir.dt.bfloat16
I32 = mybir.dt.int32
U32 = mybir.dt.uint32
ALU = mybir.AluOpType
AF = mybir.ActivationFunctionType
AX = mybir.AxisListType

ENGS = ("pe", "act", "dve", "pool", "sp")
SAME_ENGINE_SYNC = ("act", "dve", "pool")
SEM_EPOCH = 30000
LAZY = ("pe",)

D = 1024
KC = 8
DI = 2048
NH = 32
HP = 64
NG = 4
NS = 128
NCOL = 10272
C_SB, C_SC, C_SX, C_Z, C_XBC, C_DT, C_GA, C_GB = 0, 1024, 2048, 3072, 5120, 8192, 8224, 9248
NE = 32
TOPK = 4
BLK = 512
T = 256
EPS = 1e-6


class Buf:
    __slots__ = ("t", "w", "r", "sem", "semcnt", "name")

    def __init__(self, t, name=""):
        self.t = t
        self.w = None
        self.r = []
        self.sem = None
        self.semcnt = 0
        self.name = name


class Rot:
    def __init__(self, bufs):
        self.bufs = bufs
        self.i = 0

    def next(self):
        b = self.bufs[self.i % len(self.bufs)]
        self.i += 1
        return b


class Sched:
    def __init__(self, nc, stack):
        self.nc = nc
        self.stack = stack
        self.q = {e: [] for e in ENGS}
        self.nsem = 0
        self.dma_bufs = []
        self.esem = {}
        self.ecnt = {}
        self.allsems = {e: [] for e in ENGS}
        self.lastcell = {}
        for e in ENGS:
            self._new_epoch(e)
        self.waited = {e: {} for e in ENGS}

    def _new_sem(self, name):
        self.nsem += 1
        return self.stack.enter_context(self.nc.semaphore(f"{name}_{self.nsem}"))

    def _new_epoch(self, e):
        self.esem[e] = self._new_sem("e" + e)
        self.ecnt[e] = 0
        self.allsems[e].append(self.esem[e])

    def sbuf(self, name, shape, dt, stack=None):
        st = stack or self.stack
        return Buf(st.enter_context(self.nc.sbuf_tensor("s_" + name, list(shape), dt)), name)

    def psum(self, name, shape, dt):
        return Buf(self.stack.enter_context(self.nc.psum_tensor("p_" + name, list(shape), dt)), name)

    def _flush(self, e):
        cell = self.lastcell.get(e)
        if cell is None or cell["inc"]:
            return
        cell["inc"] = True
        self.ecnt[e] += 1
        self.lastcell[e] = None
        if self.ecnt[e] >= SEM_EPOCH:
            self._new_epoch(e)

    def _collect(self, eng, reads, writes):
        waits = {}

        def need(ev):
            if ev is None:
                return
            if ev[0] == "e":
                _, e2, sem, c = ev
                if e2 == eng and eng not in SAME_ENGINE_SYNC:
                    return
                if sem is self.esem[e2] and c > self.ecnt[e2]:
                    self._flush(e2)
                val = c
            else:
                sem = ev[1].sem
                val = ev[1].semcnt
            key = id(sem)
            if key not in waits or waits[key][1] < val:
                waits[key] = (sem, val)

        for b in reads:
            need(b.w)
        for b in writes:
            need(b.w)
            for ev in b.r:
                need(ev)
        out = []
        wd = self.waited[eng]
        for key, (sem, val) in waits.items():
            if wd.get(key, 0) >= val:
                continue
            wd[key] = val
            out.append((sem, val))
        return out

    def op(self, eng, fn, reads=(), writes=(), inc=True):
        waits = self._collect(eng, reads, writes)
        sem = self.esem[eng]
        cnt = self.ecnt[eng] + 1
        if eng in LAZY:
            cell = {"inc": False}
            self.lastcell[eng] = cell
        else:
            cell = {"inc": True}
            self.ecnt[eng] = cnt
            if cnt >= SEM_EPOCH:
                self._new_epoch(eng)

        def rec(E, waits=waits, fn=fn, sem=sem, cell=cell):
            for s, v in waits:
                E.wait_ge(s, v)
            ins = fn(E)
            if cell["inc"]:
                ins.then_inc(sem, 1)

        self.q[eng].append(rec)
        ev = ("e", eng, sem, cnt)
        for b in writes:
            b.w = ev
            b.r = []
        for b in reads:
            b.r.append(ev)

    def dma(self, eng, fn, owner, reads=(), writes=()):
        waits = self._collect(eng, reads, writes)
        if owner.sem is None:
            owner.sem = self._new_sem("d" + owner.name)
            self.dma_bufs.append(owner)
        owner.semcnt += 16
        sem = owner.sem

        def rec(E, waits=waits, fn=fn, sem=sem):
            for s, v in waits:
                E.wait_ge(s, v)
            fn(E).then_inc(sem, 16)

        self.q[eng].append(rec)
        ev = ("d", owner)
        for b in writes:
            b.w = ev
            b.r = []
        for b in reads:
            b.r.append(ev)

    def barrier(self):
        for e in ENGS:
            self._flush(e)
        for eng in ENGS:
            waits = []
            wd = self.waited[eng]
            for e2 in ENGS:
                if e2 == eng or self.ecnt[e2] == 0:
                    continue
                sem, val = self.esem[e2], self.ecnt[e2]
                if wd.get(id(sem), 0) < val:
                    wd[id(sem)] = val
                    waits.append((sem, val))
            for ob in self.dma_bufs:
                if wd.get(id(ob.sem), 0) < ob.semcnt:
                    wd[id(ob.sem)] = ob.semcnt
                    waits.append((ob.sem, ob.semcnt))

            def rec(E, waits=waits):
                for s, v in waits:
                    E.wait_ge(s, v)

            self.q[eng].append(rec)

    def emit(self):
        q = self.q
        with self.nc.Block() as block:
            @block.sync
            def _(E):
                for r in q["sp"]:
                    r(E)

            @block.tensor
            def _(E):
                for r in q["pe"]:
                    r(E)

            @block.scalar
            def _(E):
                for r in q["act"]:
                    r(E)

            @block.vector
            def _(E):
                for r in q["dve"]:
                    r(E)

            @block.gpsimd
            def _(E):
                for r in q["pool"]:
                    r(E)


def build_nc(NSEQ, SEQ, STOP=99):
    nc = bass.Bass("TRN2", target_bir_lowering=False)
    NT = NSEQ * SEQ
    NTT = NT // 128
    NTL = SEQ // T
    NROWS = NT * TOPK
    NBLK = NROWS // BLK + NE
    RTOT = NBLK * BLK

    def din(name, shape, dt=F32):
        return nc.dram_tensor(name, list(shape), dt, kind="ExternalInput").ap()

    def dscr(name, shape, dt):
        return nc.dram_tensor(name, list(shape), dt, kind="Internal").ap()

    x_d = din("x", [NT, D])
    cT_d = din("cT", [128, KC * NSEQ])
    wada_d = din("w_ada", [D, 6 * D])
    bada_fm_d = din("b_ada_fm", [128, 16])
    bada_bc_d = din("b_ada_bc", [128, 4 * D])
    gmix_fm_d = din("g_mix_fm", [128, KC])
    gffn_bc_d = din("g_ffn_bc", [128, D])
    gfin_bc_d = din("g_final_bc", [128, D])
    win_d = din("w_in", [D, NCOL])
    wsc_fm_d = din("w_sconv_fm", [128, KC * 3])
    wsco_d = din("w_sconv_out", [D, D])
    wxc_fm_d = din("w_ssm_conv_fm", [128, 24 * 4])
    bxc_fm_d = din("b_ssm_conv_fm", [128, 24])
    dtb_bc_d = din("dt_bias_bc", [128, NH])
    alog_bc_d = din("a_log_bc", [128, NH])
    dsk_bc_d = din("d_skip_bc", [128, NH])
    gssm_bc_d = din("g_ssm_bc", [128, DI])
    wsso_d = din("w_ssm_out", [DI, D])
    wo_d = din("w_o", [D, D])
    wr_d = din("w_router", [D, NE])
    br_bc_d = din("b_router_bc", [128, NE])
    wgu_d = din("w_gu", [NE * D, 2 * D])
    bgu_l_d = din("b_gu_l", [NE * 128, 16])
    wd_d = din("w_down", [NE * D, D])
    bd_d = din("b_down", [NE, D])
    out_d = nc.dram_tensor("out", [NT, D], F32, kind="ExternalOutput").ap()

    win_bf = dscr("win_bf", [D, NCOL], BF16)
    wsco_bf = dscr("wsco_bf", [D, D], BF16)
    wsso_bf = dscr("wsso_bf", [DI, D], BF16)
    wo_bf = dscr("wo_bf", [D, D], BF16)
    wg_bf = dscr("wg_bf", [NE * 128, KC * D], BF16)
    wu_bf = dscr("wu_bf", [NE * 128, KC * D], BF16)
    wd_bf = dscr("wd_bf", [NE * 128, KC * D], BF16)
    x1_h = dscr("x1_h", [NT, D], F32)
    h2_h = dscr("h2_h", [NT, D], BF16)
    xs_h = dscr("xs_h", [RTOT, D], BF16)
    ys_h = dscr("ys_h", [RTOT, D], F32)

    with ExitStack() as st:
        S = Sched(nc, st)
        def OP(eng, fn, r=(), w=()):
            S.op(eng, fn, reads=r, writes=w)

        def MM(outb, out_ap, pairs, reads):
            n = len(pairs)
            for i, (l, r_) in enumerate(pairs):
                S.op("pe", lambda E, l=l, r_=r_, i=i: E.matmul(out_ap, l, r_, start=(i == 0), stop=(i == n - 1)),
                     reads=reads, writes=[outb], inc=(i == n - 1))

        def TR(outb, out_ap, in_ap, idn, reads):
            S.op("pe", lambda E: E.transpose(out_ap, in_ap, idn.t[:]), reads=list(reads) + [idn], writes=[outb])

        def LOAD(eng, dst, dst_ap, src_ap, reads=()):
            S.dma(eng, lambda E: E.dma_start(out=dst_ap, in_=src_ap), dst, reads=reads, writes=[dst])

        open_stacks = []

        def finish():
            S.barrier()
            S.emit()
            for stx in reversed(open_stacks):
                stx.close()
            return nc

        B_win, B_wsco, B_wsso, B_wo = Buf(None, "winbf"), Buf(None, "wscobf"), Buf(None, "wssobf"), Buf(None, "wobf")
        B_wgu, B_wd = Buf(None, "wgubf"), Buf(None, "wdbf")
        B_x1, B_h2, B_xs, B_ys, B_out = Buf(None, "x1h"), Buf(None, "h2h"), Buf(None, "xsh"), Buf(None, "ysh"), Buf(None, "outh")

        for kc in range(KC):
            S.dma("pool", lambda E, kc=kc: E.dma_start(out=win_bf[kc * 128:(kc + 1) * 128, :], in_=win_d[kc * 128:(kc + 1) * 128, :]),
                  B_win, writes=[B_win])
        for (dst, src, bb, rows) in ((wsco_bf, wsco_d, B_wsco, D), (wsso_bf, wsso_d, B_wsso, DI), (wo_bf, wo_d, B_wo, D)):
            for r0 in range(0, rows, 512):
                S.dma("pool", lambda E, dst=dst, src=src, r0=r0: E.dma_start(out=dst[r0:r0 + 512, :], in_=src[r0:r0 + 512, :]),
                      bb, writes=[bb])

        G = [S.psum(f"G{i}", [128, 512], F32) for i in range(5)]
        ptb = S.psum("ptb", [128, 2048], BF16)
        ptB = S.psum("ptB", [128, 1024], BF16)
        Grot = Rot(G)

        io = S.sbuf("io", [128, 128], F32)
        ident = S.sbuf("ident", [128, 128], F32)
        identb = S.sbuf("identb", [128, 128], BF16)
        triu = S.sbuf("triu", [128, 128], F32)
        ones = S.sbuf("ones", [128, 128], F32)
        onesb = S.sbuf("onesb", [128, 128], BF16)
        tristb = S.sbuf("tristb", [128, 128], BF16)
        epst = S.sbuf("epst", [128, 1], F32)
        iota32 = S.sbuf("iota32", [128, NE], F32)
        OP("pool", lambda E: E.iota(io.t[:], pattern=[[1, 128]], base=0, channel_multiplier=-1,
                                    allow_small_or_imprecise_dtypes=True), w=[io])
        OP("pool", lambda E: E.iota(iota32.t[:], pattern=[[1, NE]], base=0, channel_multiplier=0,
                                    allow_small_or_imprecise_dtypes=True), w=[iota32])
        OP("dve", lambda E: E.tensor_single_scalar(ident.t[:], io.t[:], 0.0, ALU.is_equal), r=[io], w=[ident])
        OP("dve", lambda E: E.tensor_single_scalar(identb.t[:], io.t[:], 0.0, ALU.is_equal), r=[io], w=[identb])
        OP("dve", lambda E: E.tensor_single_scalar(triu.t[:], io.t[:], 0.0, ALU.is_ge), r=[io], w=[triu])
        OP("dve", lambda E: E.tensor_single_scalar(tristb.t[:], io.t[:], 0.0, ALU.is_gt), r=[io], w=[tristb])
        OP("dve", lambda E: E.memset(ones.t[:], 1.0), w=[ones])
        OP("dve", lambda E: E.memset(onesb.t[:], 1.0), w=[onesb])
        OP("dve", lambda E: E.memset(epst.t[:], EPS), w=[epst])

        def small(name, shape, src, dt=F32):
            b = S.sbuf(name, shape, dt)
            LOAD("sp", b, b.t[:], src)
            return b

        cT = small("cT", [128, KC * NSEQ], cT_d)
        bada_fm = small("bada_fm", [128, 16], bada_fm_d)
        gmix_fm = small("gmix_fm", [128, KC], gmix_fm_d)
        wsc_fm = small("wsc_fm", [128, KC * 3], wsc_fm_d)
        wxc_fm = small("wxc_fm", [128, 96], wxc_fm_d)
        bxc_fm = small("bxc_fm", [128, 24], bxc_fm_d)
        dtb_bc = small("dtb_bc", [128, NH], dtb_bc_d)
        a_bc = small("a_bc", [128, NH], alog_bc_d)
        dsk_bc = small("dsk_bc", [128, NH], dsk_bc_d)
        br_bc = small("br_bc", [128, NE], br_bc_d)
        wr_sb = S.sbuf("wr_sb", [128, KC, NE], F32)
        LOAD("sp", wr_sb, wr_sb.t[:], wr_d.rearrange("(kc p) n -> p kc n", p=128))
        wdt_sb = S.sbuf("wdt_sb", [128, KC, NH], BF16)
        OP("act", lambda E: E.activation(a_bc.t[:], a_bc.t[:], AF.Exp), r=[a_bc], w=[a_bc])
        OP("dve", lambda E: E.tensor_single_scalar(a_bc.t[:], a_bc.t[:], -1.0, ALU.mult), r=[a_bc], w=[a_bc])
        OP("act", lambda E: E.activation(cT.t[:], cT.t[:], AF.Silu), r=[cT], w=[cT])

        gs1 = S.sbuf("gs1", [128, KC * NSEQ], F32)
        sh1 = S.sbuf("sh1", [128, KC * NSEQ], F32)
        wts = S.sbuf("wts", [128, NTT * TOPK], F32)
        dest_i = S.sbuf("dest_i", [128, NTT * TOPK], I32)
        wrow = S.sbuf("wrow", [128, NBLK * KC], I32)
        brow = S.sbuf("brow", [128, NBLK], I32)
        erow = S.sbuf("erow", [128, NBLK], I32)
        plog = ExitStack()
        p1 = ExitStack()
        open_stacks.extend([plog, p1])
        logits = S.sbuf("logits", [128, NTT * NE], F32, plog)
        wadab = Rot([S.sbuf(f"wadab{i}", [128, KC, 128], F32, p1) for i in range(2)])
        badab = Rot([S.sbuf(f"badab{i}", [128, 128], F32, p1) for i in range(2)])
        condrep = S.sbuf("condrep", [128, KC, 128], F32, p1)
        bct = S.sbuf("bct", [128, 3 * D], F32, p1)
        gffn_bc = S.sbuf("gffn_bc", [128, D], F32, p1)
        LOAD("sp", gffn_bc, gffn_bc.t[:], gffn_bc_d)
        M = {"wadab": wadab, "badab": badab, "condrep": condrep, "bct": bct, "off": 0}
        wada_r = wada_d.rearrange("(kc p) n -> p kc n", p=128)

        for ch in range(16):
            wb = wadab.next()
            LOAD("sp", wb, wb.t[:], wada_r[:, :, ch * 128:(ch + 1) * 128])
            for jj in range(1):
                j = ch
                g = Grot.next()
                MM(g, g.t[:, 0:NSEQ], [(wb.t[:, kc, jj * 128:(jj + 1) * 128], cT.t[:, kc * NSEQ:(kc + 1) * NSEQ]) for kc in range(KC)], [wb, cT])
                if j < 8:
                    for b in range(NSEQ):
                        OP("act", lambda E, g=g, j=j, b=b: E.activation(sh1.t[:, j * NSEQ + b:j * NSEQ + b + 1], g.t[:, b:b + 1], AF.Identity,
                                                                         bias=bada_fm.t[:, j:j + 1], scale=1.0), r=[g, bada_fm], w=[sh1])
                else:
                    c = j - 8
                    for b in range(NSEQ):
                        OP("act", lambda E, g=g, j=j, b=b, c=c: E.activation(gs1.t[:, c * NSEQ + b:c * NSEQ + b + 1], g.t[:, b:b + 1], AF.Identity,
                                                                              bias=bada_fm.t[:, j:j + 1], scale=1.0), r=[g, bada_fm], w=[gs1])
                        OP("dve", lambda E, b=b, c=c: E.tensor_scalar(gs1.t[:, c * NSEQ + b:c * NSEQ + b + 1], gs1.t[:, c * NSEQ + b:c * NSEQ + b + 1],
                                                                     1.0, gmix_fm.t[:, c:c + 1], ALU.add, ALU.mult), r=[gs1, gmix_fm], w=[gs1])

        def mod_bc(b, chunks):
            wadab, badab, condrep, bct, off = M["wadab"], M["badab"], M["condrep"], M["bct"], M["off"]
            for kc in range(KC):
                OP("dve", lambda E, kc=kc: E.tensor_copy(condrep.t[:, kc, :], cT.t[:, kc * NSEQ + b:kc * NSEQ + b + 1].to_broadcast([128, 128])),
                   r=[cT], w=[condrep])
            for ch in chunks:
                wb = wadab.next()
                bb = badab.next()
                LOAD("sp", wb, wb.t[:], wada_r[:, :, 2 * D + ch * 128:2 * D + (ch + 1) * 128])
                LOAD("sp", bb, bb.t[:], bada_bc_d[:, ch * 128:(ch + 1) * 128])
                g = Grot.next()
                MM(g, g.t[:, 0:128], [(condrep.t[:, kc, :], wb.t[:, kc, :]) for kc in range(KC)], [wb, condrep])
                dsl = slice((ch - off) * 128, (ch - off + 1) * 128)
                OP("dve", lambda E, g=g, bb=bb, dsl=dsl: E.tensor_tensor(bct.t[:, dsl], g.t[:, 0:128], bb.t[:], ALU.add),
                   r=[g, bb], w=[bct])
                if 16 <= ch < 24:
                    OP("dve", lambda E, ch=ch, dsl=dsl: E.scalar_tensor_tensor(bct.t[:, dsl], bct.t[:, dsl], 1.0,
                                                                      gffn_bc.t[:, (ch - 16) * 128:(ch - 15) * 128], ALU.add, ALU.mult),
                       r=[bct, gffn_bc], w=[bct])

        GT1, SH2, GS2, GT2 = 0, D, 2 * D, 0
        if STOP == 0:
            return finish()

        gssm_bc = S.sbuf("gssm_bc", [128, DI], F32, p1)
        LOAD("sp", gssm_bc, gssm_bc.t[:], gssm_bc_d)
        LOAD("sp", wdt_sb, wdt_sb.t[:], win_bf.rearrange("(kc p) n -> p kc n", p=128)[:, :, C_DT:C_DT + NH], reads=[B_win])
        wst = Rot([S.sbuf(f"wst{i}", [128, KC, 512], BF16, p1) for i in range(4)])
        xr = Rot([S.sbuf(f"xr{i}", [128, D], F32, p1) for i in range(2)])
        junk = S.sbuf("junk", [128, D], BF16, p1)
        ss = S.sbuf("ss", [128, 4], F32, p1)
        rstd = S.sbuf("rstd", [128, 4], F32, p1)
        hT = S.sbuf("hT", [128, KC, T], BF16, p1)
        vT = S.sbuf("vT", [128, KC, T], BF16, p1)
        tmpA = Rot([S.sbuf(f"tmpA{i}", [128, T], F32, p1) for i in range(2)])
        tmpY = Rot([S.sbuf(f"tmpY{i}", [128, T], F32, p1) for i in range(2)])
        utmp = Rot([S.sbuf(f"utmp{i}", [128, T + 2], F32, p1) for i in range(2)])
        uhalo = S.sbuf("uhalo", [128, KC, 2], F32, p1)
        xraw = Rot([S.sbuf(f"xraw{i}", [128, T + 3], F32, p1) for i in range(2)])
        xhalo = S.sbuf("xhalo", [128, 24, 3], F32, p1)
        xsT = S.sbuf("xsT", [128, 16, T], BF16, p1)
        BT = S.sbuf("BT", [128, NG, T], BF16, p1)
        CT = S.sbuf("CT", [128, NG, T], BF16, p1)
        zs = S.sbuf("zs", [128, 2, DI], BF16, p1)
        dtt = S.sbuf("dtt", [128, 2, NH], F32, p1)
        dat = S.sbuf("dat", [128, 2, NH], F32, p1)
        dtmp = S.sbuf("dtmp", [128, NH], F32, p1)
        acum = S.sbuf("acum", [128, NH], F32, p1)
        dend = S.sbuf("dend", [128, NH], F32, p1)
        dchunk = S.sbuf("dchunk", [128, NH], F32, p1)
        eacum = S.sbuf("eacum", [128, NH], F32, p1)
        xs_tok = S.sbuf("xs_tok", [128, DI], BF16, p1)
        xdt = S.sbuf("xdt", [128, DI], BF16, p1)
        xdte = S.sbuf("xdte", [128, DI], BF16, p1)
        B_tok = S.sbuf("B_tok", [128, NG * NS], BF16, p1)
        h2Tv = xdte.t[:].bitcast(F32).rearrange("p (c t) -> p c t", t=128)
        CBm = S.sbuf("CBm", [128, NG, 128], F32, p1)
        seg = Rot([S.sbuf(f"seg{i}", [128, 4, 128], F32, p1) for i in range(2)])
        Sb = S.sbuf("Sb", [128, 8, 128], BF16, p1)
        st32 = S.sbuf("st32", [128, DI], F32, p1)
        stbf = S.sbuf("stbf", [128, DI], BF16, p1)
        yg = Rot([S.sbuf(f"yg{i}", [128, 512], F32, p1) for i in range(2)])
        yt2 = Rot([S.sbuf(f"yt2{i}", [128, 512], F32, p1) for i in range(1)])
        yn = Rot([S.sbuf(f"yn{i}", [128, 512], BF16, p1) for i in range(2)])
        ssg = S.sbuf("ssg", [128, 2], F32, p1)
        ynT = S.sbuf("ynT", [128, 16, T], BF16, p1)
        mpart = S.sbuf("mpart", [128, 8, T], F32, p1)
        mT = S.sbuf("mT", [128, KC, T], BF16, p1)
        h2 = S.sbuf("h2", [128, D], F32, p1)
        h2b = Rot([S.sbuf(f"h2b{i}", [128, D], BF16, p1) for i in range(1)])

        win_r = win_bf.rearrange("(kc p) n -> p kc n", p=128)
        wsco_r = wsco_bf.rearrange("(kc p) n -> p kc n", p=128)
        wsso_r = wsso_bf.rearrange("(kc p) n -> p kc n", p=128)
        wo_r = wo_bf.rearrange("(kc p) n -> p kc n", p=128)

        def wload(src_r, bb, c0, k0=0):
            w = wst.next()
            LOAD("sp", w, w.t[:], src_r[:, k0:k0 + KC, c0:c0 + 512], reads=[bb])
            return w

        def rms_stats(src, col, n):
            OP("act", lambda E: E.activation(junk.t[:, 0:n], src, AF.Square, accum_out=ss.t[:, col:col + 1]), r=[], w=[junk, ss])
            OP("act", lambda E: E.activation(ss.t[:, col:col + 1], ss.t[:, col:col + 1], AF.Sqrt, bias=epst.t[:, 0:1], scale=1.0 / n),
               r=[ss, epst], w=[ss])
            OP("dve", lambda E: E.reciprocal(rstd.t[:, col:col + 1], ss.t[:, col:col + 1]), r=[ss], w=[rstd])

        cast_jobs = []
        for e in range(NE):
            cast_jobs.append((wg_bf, wgu_d, e, 0, B_wgu))
            cast_jobs.append((wu_bf, wgu_d, e, D, B_wgu))
            cast_jobs.append((wd_bf, wd_d, e, 0, B_wd))

        def issue_casts(n):
            for _ in range(n):
                if not cast_jobs:
                    return
                dst, src, e, c0, bb = cast_jobs.pop(0)
                S.dma("pool", lambda E, dst=dst, src=src, e=e, c0=c0: E.dma_start(
                    out=dst[e * 128:(e + 1) * 128, :].rearrange("p (k n) -> p k n", n=D),
                    in_=src[e * D:(e + 1) * D, c0:c0 + D].rearrange("(k p) n -> p k n", p=128)), bb, writes=[bb])

        casts_per_tile = -(-len(cast_jobs) // (NSEQ * NTL))
        zt = S.sbuf("zt", [128, 2 * D], BF16, p1)
        OP("pool", lambda E: E.memset(zt.t[:], 0.0), w=[zt])
        B_xz = Buf(None, "xsz")
        zero_jobs = list(range(0, RTOT, 256))

        def issue_zero(n):
            for _ in range(n):
                if not zero_jobs:
                    return
                q0 = zero_jobs.pop(0)
                S.dma("sp", lambda E, q0=q0: E.dma_start(out=xs_h[q0:q0 + 256, :].rearrange("(p r) d -> p (r d)", r=2), in_=zt.t[:]),
                      B_xz, reads=[zt], writes=[])
            B_xz.w = ("d", B_xz)

        zeros_per_tile = -(-len(zero_jobs) // (NSEQ * NTL))
        for b in range(NSEQ):
            mod_bc(b, range(0, 24))
            OP("dve", lambda E: E.memset(st32.t[:], 0.0), w=[st32])
            OP("dve", lambda E: E.memset(stbf.t[:], 0.0), w=[stbf])
            OP("dve", lambda E: E.memset(uhalo.t[:], 0.0), w=[uhalo])
            OP("dve", lambda E: E.memset(xhalo.t[:], 0.0), w=[xhalo])
            for it in range(NTL):
                r0 = b * SEQ + it * T
                issue_casts(casts_per_tile)
                issue_zero(zeros_per_tile)
                xs_ = []
                for j in range(2):
                    xb = xr.next()
                    xs_.append(xb)
                    LOAD("sp", xb, xb.t[:], x_d[r0 + j * 128:r0 + (j + 1) * 128, :])
                    OP("act", lambda E, xb=xb, j=j: E.activation(junk.t[:], xb.t[:], AF.Square, accum_out=ss.t[:, j:j + 1]), r=[xb], w=[junk, ss])
                    OP("act", lambda E, j=j: E.activation(ss.t[:, j:j + 1], ss.t[:, j:j + 1], AF.Sqrt, bias=epst.t[:, 0:1], scale=1.0 / D),
                       r=[ss, epst], w=[ss])
                    OP("dve", lambda E, j=j: E.reciprocal(rstd.t[:, j:j + 1], ss.t[:, j:j + 1]), r=[ss], w=[rstd])
                    OP("pool", lambda E, xb=xb, j=j: E.tensor_scalar(xb.t[:], xb.t[:], rstd.t[:, j:j + 1], None, ALU.mult), r=[xb, rstd], w=[xb])
                for c in range(KC):
                    g = Grot.next()
                    for j in range(2):
                        TR(g, g.t[:, j * 128:(j + 1) * 128], xs_[j].t[:, c * 128:(c + 1) * 128], ident, [xs_[j]])
                    OP("act", lambda E, g=g, c=c, b=b: E.activation(hT.t[:, c, :], g.t[:, 0:T], AF.Identity,
                                                               bias=sh1.t[:, c * NSEQ + b:c * NSEQ + b + 1],
                                                               scale=gs1.t[:, c * NSEQ + b:c * NSEQ + b + 1]), r=[g, sh1, gs1], w=[hT])

                def proj_fm(w, cc):
                    g = Grot.next()
                    MM(g, g.t[:, 0:T], [(w.t[:, kc, cc * 128:(cc + 1) * 128], hT.t[:, kc, :]) for kc in range(KC)], [w, hT])
                    return g

                if STOP == 11:
                    return finish()
                work = []
                WB = {}

                def b_item(q, cc):
                    if cc == 0:
                        WB["sb"] = wload(win_r, B_win, C_SB + q * 512)
                        WB["sx"] = wload(win_r, B_win, C_SX + q * 512)
                        WB["sc"] = wload(win_r, B_win, C_SC + q * 512)
                    w_sb, w_sx, w_sc = WB["sb"], WB["sx"], WB["sc"]
                    c = q * 4 + cc
                    g_sb = proj_fm(w_sb, cc)
                    g_sx = proj_fm(w_sx, cc)
                    ta = tmpA.next()
                    OP("act", lambda E, ta=ta, g=g_sx: E.activation(ta.t[:], g.t[:, 0:T], AF.Copy), r=[g_sx], w=[ta])
                    u = utmp.next()
                    OP("pool", lambda E, u=u, c=c: E.tensor_copy(u.t[:, 0:2], uhalo.t[:, c, :]), r=[uhalo], w=[u])
                    OP("dve", lambda E, u=u, g=g_sb, ta=ta: E.tensor_tensor(u.t[:, 2:T + 2], g.t[:, 0:T], ta.t[:], ALU.mult), r=[g_sb, ta], w=[u])
                    OP("pool", lambda E, u=u, c=c: E.tensor_copy(uhalo.t[:, c, :], u.t[:, T:T + 2]), r=[u], w=[uhalo])
                    g_sc = proj_fm(w_sc, cc)
                    y = tmpY.next()
                    OP("pool", lambda E, y=y, u=u, c=c: E.tensor_scalar(y.t[:], u.t[:, 2:T + 2], wsc_fm.t[:, c * 3 + 2:c * 3 + 3], None, ALU.mult),
                       r=[u, wsc_fm], w=[y])
                    OP("dve", lambda E, y=y, u=u, c=c: E.scalar_tensor_tensor(y.t[:], u.t[:, 1:T + 1], wsc_fm.t[:, c * 3 + 1:c * 3 + 2], y.t[:], ALU.mult, ALU.add),
                       r=[u, wsc_fm, y], w=[y])
                    OP("dve", lambda E, y=y, u=u, c=c: E.scalar_tensor_tensor(y.t[:], u.t[:, 0:T], wsc_fm.t[:, c * 3:c * 3 + 1], y.t[:], ALU.mult, ALU.add),
                       r=[u, wsc_fm, y], w=[y])
                    OP("dve", lambda E, y=y, g=g_sc, c=c: E.tensor_tensor(vT.t[:, c, :], g.t[:, 0:T], y.t[:], ALU.mult), r=[g_sc, y], w=[vT])

                def e1_item(dq, dcc):
                    if dcc == 0:
                        WB["a"] = wload(wsco_r, B_wsco, dq * 512)
                        WB["ga"] = wload(win_r, B_win, C_GA + dq * 512)
                    w_a, w_ga = WB["a"], WB["ga"]
                    g_y = Grot.next()
                    MM(g_y, g_y.t[:, 0:T], [(w_a.t[:, kc, dcc * 128:(dcc + 1) * 128], vT.t[:, kc, :]) for kc in range(KC)], [w_a, vT])
                    g_g = proj_fm(w_ga, dcc)
                    ta = tmpA.next()
                    OP("act", lambda E, ta=ta, g=g_g: E.activation(ta.t[:], g.t[:, 0:T], AF.Sigmoid), r=[g_g], w=[ta])
                    OP("dve", lambda E, ta=ta, g=g_y, dq=dq, dcc=dcc: E.tensor_tensor(mpart.t[:, dq * 4 + dcc, :], g.t[:, 0:T], ta.t[:], ALU.mult), r=[g_y, ta], w=[mpart])

                for q in range(2):
                    for cc in range(4):
                        work.append((b_item, q, cc))
                for dq in range(2):
                    for dcc in range(4):
                        work.append((e1_item, dq, dcc))

                def do_work(n):
                    for _ in range(n):
                        if work:
                            f_, a_, b_ = work.pop(0)
                            f_(a_, b_)

                if STOP == 12:
                    return finish()
                for zq in range(4):
                    w_z = wload(win_r, B_win, C_Z + zq * 512)
                    for j in range(2):
                        g = Grot.next()
                        MM(g, g.t[:], [(hT.t[:, kc, j * 128:(j + 1) * 128], w_z.t[:, kc, :]) for kc in range(KC)], [w_z, hT])
                        OP("act", lambda E, g=g, j=j, zq=zq: E.activation(zs.t[:, j, zq * 512:(zq + 1) * 512], g.t[:], AF.Silu), r=[g], w=[zs])
                for j in range(2):
                    g = Grot.next()
                    MM(g, g.t[:, 0:NH], [(hT.t[:, kc, j * 128:(j + 1) * 128], wdt_sb.t[:, kc, :]) for kc in range(KC)], [wdt_sb, hT])
                    OP("dve", lambda E, g=g: E.tensor_tensor(dtmp.t[:], g.t[:, 0:NH], dtb_bc.t[:], ALU.add), r=[g, dtb_bc], w=[dtmp])
                    OP("act", lambda E: E.activation(dtmp.t[:], dtmp.t[:], AF.Exp), r=[dtmp], w=[dtmp])
                    OP("act", lambda E, j=j: E.activation(dtt.t[:, j, :], dtmp.t[:], AF.Ln, bias=1.0, scale=1.0), r=[dtmp], w=[dtt])
                    OP("dve", lambda E, j=j: E.tensor_tensor(dat.t[:, j, :], dtt.t[:, j, :], a_bc.t[:], ALU.mult), r=[dtt, a_bc], w=[dat])
                for xq in range(6):
                    w_x = wload(win_r, B_win, C_XBC + xq * 512)
                    for cc in range(4):
                        cx = xq * 4 + cc
                        g = proj_fm(w_x, cc)
                        xw = xraw.next()
                        OP("pool", lambda E, xw=xw, cx=cx: E.tensor_copy(xw.t[:, 0:3], xhalo.t[:, cx, :]), r=[xhalo], w=[xw])
                        OP("act", lambda E, xw=xw, g=g: E.activation(xw.t[:, 3:T + 3], g.t[:, 0:T], AF.Copy), r=[g], w=[xw])
                        OP("pool", lambda E, xw=xw, cx=cx: E.tensor_copy(xhalo.t[:, cx, :], xw.t[:, T:T + 3]), r=[xw], w=[xhalo])
                        y = tmpY.next()
                        OP("dve", lambda E, y=y, xw=xw, cx=cx: E.tensor_scalar(y.t[:], xw.t[:, 3:T + 3], wxc_fm.t[:, cx * 4 + 3:cx * 4 + 4], None, ALU.mult),
                           r=[xw, wxc_fm], w=[y])
                        for k in (2, 1, 0):
                            eng = "dve"
                            OP(eng, lambda E, y=y, xw=xw, cx=cx, k=k: E.scalar_tensor_tensor(y.t[:], xw.t[:, k:k + T], wxc_fm.t[:, cx * 4 + k:cx * 4 + k + 1], y.t[:],
                                                                                           ALU.mult, ALU.add), r=[xw, wxc_fm, y], w=[y])
                        if cx < 16:
                            dstb, dst = xsT, xsT.t[:, cx, :]
                        elif cx < 20:
                            dstb, dst = BT, BT.t[:, cx - 16, :]
                        else:
                            dstb, dst = CT, CT.t[:, cx - 20, :]
                        OP("act", lambda E, y=y, dst=dst, cx=cx: E.activation(dst, y.t[:], AF.Silu, bias=bxc_fm.t[:, cx:cx + 1], scale=1.0),
                           r=[y, bxc_fm], w=[dstb])

                if STOP == 13:
                    return finish()
                for jpos, j in enumerate(_JLIST):
                    SD = (STOP - 100 * jpos) if 141 + 100 * jpos <= STOP <= 148 + 100 * jpos else -1
                    js = slice(j * 128, (j + 1) * 128)
                    g0 = G[0]
                    MM(g0, g0.t[:, 0:NH], [(triu.t[:], dat.t[:, j, :])], [triu, dat])
                    MM(g0, g0.t[:, NH:2 * NH], [(ones.t[:], dat.t[:, j, :])], [ones, dat])
                    OP("dve", lambda E: E.tensor_copy(acum.t[:], g0.t[:, 0:NH]), r=[g0], w=[acum])
                    OP("dve", lambda E: E.tensor_tensor(dend.t[:], g0.t[:, NH:2 * NH], acum.t[:], ALU.subtract), r=[g0, acum], w=[dend])
                    OP("act", lambda E: E.activation(dend.t[:], dend.t[:], AF.Exp), r=[dend], w=[dend])
                    OP("act", lambda E: E.activation(dchunk.t[:], g0.t[:, NH:2 * NH], AF.Exp), r=[g0], w=[dchunk])
                    OP("act", lambda E: E.activation(eacum.t[:], acum.t[:], AF.Exp), r=[acum], w=[eacum])
                    if STOP == 240 and jpos == 1:
                        return finish()
                    for cx in range(16):
                        TR(ptb, ptb.t[:, cx * 128:(cx + 1) * 128], xsT.t[:, cx, js], identb, [xsT])
                    if STOP == 2401 and jpos == 1:
                        return finish()
                    for g_ in range(NG):
                        TR(ptB, ptB.t[:, g_ * 128:(g_ + 1) * 128], BT.t[:, g_, js], identb, [BT])
                    if STOP == 2402 and jpos == 1:
                        return finish()
                    OP("act", lambda E: E.activation(xs_tok.t[:], ptb.t[:], AF.Copy), r=[ptb], w=[xs_tok])
                    if STOP == 2403 and jpos == 1:
                        return finish()
                    OP("dve", lambda E, j=j: E.tensor_tensor(xdt.t[:].rearrange("p (h q) -> p h q", q=HP), xs_tok.t[:].rearrange("p (h q) -> p h q", q=HP),
                                                             dtt.t[:, j, :].unsqueeze(2).to_broadcast([128, NH, HP]), ALU.mult), r=[xs_tok, dtt], w=[xdt])
                    if STOP == 2404 and jpos == 1:
                        return finish()
                    OP("pool", lambda E: E.tensor_tensor(xdte.t[:].rearrange("p (h q) -> p h q", q=HP), xdt.t[:].rearrange("p (h q) -> p h q", q=HP),
                                                         dend.t[:].unsqueeze(2).to_broadcast([128, NH, HP]), ALU.mult), r=[xdt, dend], w=[xdte])
                    OP("act", lambda E: E.activation(B_tok.t[:], ptB.t[:, 0:NG * NS], AF.Copy), r=[ptB], w=[B_tok])
                    if SD == 141:
                        return finish()
                    g1 = G[1]
                    for g_ in range(NG):
                        MM(g1, g1.t[:, g_ * 128:(g_ + 1) * 128], [(BT.t[:, g_, js], CT.t[:, g_, js])], [BT, CT])
                    OP("dve", lambda E: E.tensor_tensor(CBm.t[:], g1.t[:].rearrange("p (g i) -> p g i", i=128),
                                                        triu.t[:].unsqueeze(1).to_broadcast([128, NG, 128]), ALU.mult), r=[g1, triu], w=[CBm])
                    if SD == 142:
                        return finish()
                    for g_ in range(NG):
                        gsl = slice(g_ * 512, (g_ + 1) * 512)
                        for half in range(2):
                            ga_ = G[2 + half]
                            for hh in range(4):
                                h = g_ * 8 + half * 4 + hh
                                MM(ga_, ga_.t[:, hh * 128:(hh + 1) * 128], [(dat.t[:, j, h:h + 1].to_broadcast([128, 128]), triu.t[:])], [dat, triu])
                            sg_ = seg.next()
                            h0 = g_ * 8 + half * 4
                            OP("dve", lambda E, ga_=ga_, sg_=sg_, h0=h0: E.tensor_tensor(sg_.t[:], ga_.t[:].rearrange("p (h i) -> p h i", i=128),
                                                                                       acum.t[:, h0:h0 + 4].unsqueeze(2).to_broadcast([128, 4, 128]), ALU.subtract),
                               r=[ga_, acum], w=[sg_])
                            OP("pool", lambda E, sg_=sg_: E.tensor_single_scalar(sg_.t[:], sg_.t[:], 0.0, ALU.min), r=[sg_], w=[sg_])
                            OP("act", lambda E, sg_=sg_: E.activation(sg_.t[:], sg_.t[:], AF.Exp), r=[sg_], w=[sg_])
                            OP("dve", lambda E, sg_=sg_, half=half, g_=g_: E.tensor_tensor(Sb.t[:, half * 4:half * 4 + 4, :], sg_.t[:],
                                                                                         CBm.t[:, g_:g_ + 1, :].to_broadcast([128, 4, 128]), ALU.mult),
                               r=[sg_, CBm], w=[Sb])
                        if SD == 143:
                            return finish()
                        g4 = G[4]
                        for h8 in range(8):
                            h = g_ * 8 + h8
                            MM(g4, g4.t[:, h8 * HP:(h8 + 1) * HP], [(Sb.t[:, h8, :], xdt.t[:, h * HP:(h + 1) * HP])], [Sb, xdt])
                        MM(g0, g0.t[:], [(CT.t[:, g_, js], stbf.t[:, gsl])], [CT, stbf])
                        y_ = yg.next()
                        OP("dve", lambda E, y_=y_, g_=g_: E.tensor_tensor(y_.t[:].rearrange("p (h q) -> p h q", q=HP), g0.t[:].rearrange("p (h q) -> p h q", q=HP),
                                                                         eacum.t[:, g_ * 8:(g_ + 1) * 8].unsqueeze(2).to_broadcast([128, 8, HP]), ALU.mult),
                           r=[g0, eacum], w=[y_])
                        OP("dve", lambda E, y_=y_: E.tensor_tensor(y_.t[:], g4.t[:], y_.t[:], ALU.add), r=[g4, y_], w=[y_])
                        t2 = yt2.next()
                        OP("pool", lambda E, t2=t2, g_=g_, gsl=gsl: E.tensor_tensor(t2.t[:].rearrange("p (h q) -> p h q", q=HP), xs_tok.t[:, gsl].rearrange("p (h q) -> p h q", q=HP),
                                                                                  dsk_bc.t[:, g_ * 8:(g_ + 1) * 8].unsqueeze(2).to_broadcast([128, 8, HP]), ALU.mult),
                           r=[xs_tok, dsk_bc], w=[t2])
                        OP("pool", lambda E, t2=t2, y_=y_: E.tensor_tensor(y_.t[:], y_.t[:], t2.t[:], ALU.add), r=[t2, y_], w=[y_])
                        if SD == 144:
                            return finish()
                        OP("dve", lambda E, y_=y_, j=j, gsl=gsl: E.tensor_tensor(y_.t[:], y_.t[:], zs.t[:, j, gsl], ALU.mult), r=[y_, zs], w=[y_])
                        OP("act", lambda E, y_=y_: E.activation(junk.t[:, 0:512], y_.t[:], AF.Square, accum_out=ssg.t[:, 0:1]), r=[y_], w=[junk, ssg])
                        OP("act", lambda E: E.activation(ssg.t[:, 0:1], ssg.t[:, 0:1], AF.Sqrt, bias=epst.t[:, 0:1], scale=1.0 / 512), r=[ssg, epst], w=[ssg])
                        OP("dve", lambda E: E.reciprocal(ssg.t[:, 1:2], ssg.t[:, 0:1]), r=[ssg], w=[ssg])
                        yn_ = yn.next()
                        OP("dve", lambda E, y_=y_, yn_=yn_, gsl=gsl: E.scalar_tensor_tensor(yn_.t[:], y_.t[:], ssg.t[:, 1:2], gssm_bc.t[:, gsl], ALU.mult, ALU.mult),
                           r=[y_, ssg, gssm_bc], w=[yn_])
                        for q in range(4):
                            TR(ptB, ptB.t[:, 512 + q * 128:512 + (q + 1) * 128], yn_.t[:, q * 128:(q + 1) * 128], identb, [yn_])
                        OP("act", lambda E, g_=g_, js=js: E.activation(ynT.t[:, g_ * 4:(g_ + 1) * 4, js], ptB.t[:, 512:1024].rearrange("p (q t) -> p q t", t=128), AF.Copy),
                           r=[ptB], w=[ynT])
                        if SD == 145:
                            return finish()
                        MM(g1, g1.t[:], [(B_tok.t[:, g_ * 128:(g_ + 1) * 128], xdte.t[:, gsl])], [B_tok, xdte])
                        OP("pool", lambda E, g_=g_, gsl=gsl: E.tensor_tensor(st32.t[:, gsl].rearrange("p (h q) -> p h q", q=HP), st32.t[:, gsl].rearrange("p (h q) -> p h q", q=HP),
                                                                           dchunk.t[:, g_ * 8:(g_ + 1) * 8].unsqueeze(2).to_broadcast([128, 8, HP]), ALU.mult),
                           r=[st32, dchunk], w=[st32])
                        OP("dve", lambda E, gsl=gsl: E.tensor_tensor(st32.t[:, gsl], g1.t[:], st32.t[:, gsl], ALU.add), r=[g1, st32], w=[st32])
                        OP("act", lambda E, gsl=gsl: E.activation(stbf.t[:, gsl], st32.t[:, gsl], AF.Copy), r=[st32], w=[stbf])
                        do_work(2)
                        if SD == 146:
                            return finish()
                        if SD == 147 and g_ == 1:
                            return finish()
                    if SD == 148:
                        return finish()

                if STOP == 14:
                    return finish()
                do_work(len(work))
                for dq in range(2):
                    w_b0 = wload(wsso_r, B_wsso, dq * 512, 0)
                    w_b1 = wload(wsso_r, B_wsso, dq * 512, 8)
                    w_gb = wload(win_r, B_win, C_GB + dq * 512)
                    for dcc in range(4):
                        g_y = Grot.next()
                        MM(g_y, g_y.t[:, 0:T], [(w_b0.t[:, kc, dcc * 128:(dcc + 1) * 128], ynT.t[:, kc, :]) for kc in range(KC)] +
                           [(w_b1.t[:, kc, dcc * 128:(dcc + 1) * 128], ynT.t[:, 8 + kc, :]) for kc in range(KC)], [w_b0, w_b1, ynT])
                        g_g = proj_fm(w_gb, dcc)
                        ta = tmpA.next()
                        OP("act", lambda E, ta=ta, g=g_g: E.activation(ta.t[:], g.t[:, 0:T], AF.Sigmoid), r=[g_g], w=[ta])
                        OP("dve", lambda E, ta=ta, g=g_y: E.tensor_tensor(ta.t[:], g.t[:, 0:T], ta.t[:], ALU.mult), r=[g_y, ta], w=[ta])
                        OP("pool", lambda E, ta=ta, dq=dq, dcc=dcc: E.tensor_tensor(mT.t[:, dq * 4 + dcc, :], ta.t[:], mpart.t[:, dq * 4 + dcc, :], ALU.add),
                           r=[ta, mpart], w=[mT])

                if STOP == 15:
                    return finish()
                x1b = [xr.next(), xr.next()]
                for j in range(2):
                    LOAD("sp", x1b[j], x1b[j].t[:], x_d[r0 + j * 128:r0 + (j + 1) * 128, :], reads=[])
                for dh in range(2):
                    w_o_ = wload(wo_r, B_wo, dh * 512)
                    dsl = slice(dh * 512, (dh + 1) * 512)
                    for j in range(2):
                        g = Grot.next()
                        MM(g, g.t[:], [(mT.t[:, kc, j * 128:(j + 1) * 128], w_o_.t[:, kc, :]) for kc in range(KC)], [w_o_, mT])
                        t2 = yt2.next()
                        OP("dve", lambda E, g=g, t2=t2, dsl=dsl: E.tensor_tensor(t2.t[:], g.t[:], bct.t[:, GT1 + dsl.start:GT1 + dsl.stop], ALU.mult), r=[g, bct], w=[t2])
                        OP("pool", lambda E, t2=t2, xb_=x1b[j], dsl=dsl: E.tensor_tensor(xb_.t[:, dsl], xb_.t[:, dsl], t2.t[:], ALU.add), r=[t2, x1b[j]], w=[x1b[j]])
                for j in range(2):
                    tt = (r0 // 128) + j
                    xb = x1b[j]
                    S.dma("sp", lambda E, xb=xb, tt=tt: E.dma_start(out=x1_h[tt * 128:(tt + 1) * 128, :], in_=xb.t[:]), xb, reads=[xb], writes=[B_x1])
                    OP("act", lambda E, xb=xb: E.activation(junk.t[:], xb.t[:], AF.Square, accum_out=ss.t[:, 2:3]), r=[xb], w=[junk, ss])
                    OP("act", lambda E: E.activation(ss.t[:, 2:3], ss.t[:, 2:3], AF.Sqrt, bias=epst.t[:, 0:1], scale=1.0 / D), r=[ss, epst], w=[ss])
                    OP("dve", lambda E: E.reciprocal(rstd.t[:, 2:3], ss.t[:, 2:3]), r=[ss], w=[rstd])
                    OP("dve", lambda E, xb=xb: E.scalar_tensor_tensor(h2.t[:], xb.t[:], rstd.t[:, 2:3], bct.t[:, GS2:GS2 + D], ALU.mult, ALU.mult),
                       r=[xb, rstd, bct], w=[h2])
                    OP("pool", lambda E: E.tensor_tensor(h2.t[:], h2.t[:], bct.t[:, SH2:SH2 + D], ALU.add), r=[h2, bct], w=[h2])
                    hb = h2b.next()
                    OP("act", lambda E, hb=hb: E.activation(hb.t[:], h2.t[:], AF.Copy), r=[h2], w=[hb])
                    S.dma("sp", lambda E, hb=hb, tt=tt: E.dma_start(out=h2_h[tt * 128:(tt + 1) * 128, :], in_=hb.t[:]), hb, reads=[hb], writes=[B_h2])
                    for half in range(2):
                        g = Grot.next()
                        for c4 in range(4):
                            c = half * 4 + c4
                            TR(g, g.t[:, c4 * 128:(c4 + 1) * 128], h2.t[:, c * 128:(c + 1) * 128], ident, [h2])
                        OP("act", lambda E, g=g, half=half: E.activation(h2Tv[:, half * 4:half * 4 + 4, :], g.t[:].rearrange("p (c t) -> p c t", t=128), AF.Copy),
                           r=[g], w=[xdte])
                    g = Grot.next()
                    MM(g, g.t[:, 0:NE], [(h2Tv[:, kc, :], wr_sb.t[:, kc, :]) for kc in range(KC)], [xdte, wr_sb])
                    OP("dve", lambda E, g=g, tt=tt: E.tensor_tensor(logits.t[:, tt * NE:(tt + 1) * NE], g.t[:, 0:NE], br_bc.t[:], ALU.add), r=[g, br_bc], w=[logits])

        issue_casts(len(cast_jobs))
        issue_zero(len(zero_jobs))
        if STOP == 1:
            return finish()
        S.barrier()
        p1.close()
        open_stacks.remove(p1)

        p2a = ExitStack()
        open_stacks.append(p2a)
        top8 = S.sbuf("top8", [128, 8], F32, p2a)
        topi = S.sbuf("topi", [128, 8], U32, p2a)
        idxf = S.sbuf("idxf", [128, NTT * TOPK], F32, p2a)
        negm = S.sbuf("negm", [128, 1], F32, p2a)
        esum = S.sbuf("esum", [128, 2], F32, p2a)
        Mb = S.sbuf("Mb", [128, NE], BF16, p2a)
        rank = S.sbuf("rank", [128, NTT * NE], F32, p2a)
        runc = S.sbuf("runc", [128, NE], F32, p2a)
        ca = S.sbuf("ca", [128, NE], F32, p2a)
        cb = S.sbuf("cb", [128, NE], F32, p2a)
        ci = S.sbuf("ci", [128, NE], I32, p2a)
        padded = S.sbuf("padded", [128, NE], F32, p2a)
        pstart = S.sbuf("pstart", [128, NE], F32, p2a)
        base = S.sbuf("base", [128, NE], F32, p2a)
        tmpe = S.sbuf("tmpe", [128, NE], F32, p2a)
        destf = S.sbuf("destf", [128, NTT * TOPK], F32, p2a)
        bstart = S.sbuf("bstart", [128, NBLK], F32, p2a)
        be = S.sbuf("be", [128, NBLK], F32, p2a)
        pio = S.sbuf("pio", [128, KC], F32, p2a)
        wrow_f = S.sbuf("wrow_f", [128, NBLK, KC], F32, p2a)
        brow_f = S.sbuf("brow_f", [128, NBLK], F32, p2a)

        OP("dve", lambda E: E.memset(runc.t[:], 0.0), w=[runc])
        for tt in range(NTT):
            lg = logits.t[:, tt * NE:(tt + 1) * NE]
            OP("dve", lambda E, lg=lg: E.max(top8.t[:], lg), r=[logits], w=[top8])
            OP("dve", lambda E, lg=lg: E.max_index(topi.t[:], top8.t[:], lg), r=[logits, top8], w=[topi])
            OP("dve", lambda E, tt=tt: E.tensor_copy(idxf.t[:, tt * 4:tt * 4 + 4], topi.t[:, 0:4]), r=[topi], w=[idxf])
            OP("dve", lambda E: E.tensor_single_scalar(negm.t[:], top8.t[:, 0:1], -1.0, ALU.mult), r=[top8], w=[negm])
            OP("act", lambda E, tt=tt: E.activation(wts.t[:, tt * 4:tt * 4 + 4], top8.t[:, 0:4], AF.Exp, bias=negm.t[:, 0:1], scale=1.0,
                                                   accum_out=esum.t[:, 0:1]), r=[top8, negm], w=[wts, esum])
            OP("dve", lambda E: E.reciprocal(esum.t[:, 1:2], esum.t[:, 0:1]), r=[esum], w=[esum])
            OP("dve", lambda E, tt=tt: E.tensor_scalar(wts.t[:, tt * 4:tt * 4 + 4], wts.t[:, tt * 4:tt * 4 + 4], esum.t[:, 1:2], None, ALU.mult),
               r=[wts, esum], w=[wts])
            OP("dve", lambda E, tt=tt: E.tensor_scalar(Mb.t[:], iota32.t[:], idxf.t[:, tt * 4:tt * 4 + 1], None, ALU.is_equal), r=[iota32, idxf], w=[Mb])
            for k in range(1, 4):
                OP("dve", lambda E, tt=tt, k=k: E.scalar_tensor_tensor(Mb.t[:], iota32.t[:], idxf.t[:, tt * 4 + k:tt * 4 + k + 1], Mb.t[:], ALU.is_equal, ALU.add),
                   r=[iota32, idxf, Mb], w=[Mb])
            g = Grot.next()
            MM(g, g.t[:, 0:NE], [(tristb.t[:], Mb.t[:])], [tristb, Mb])
            MM(g, g.t[:, NE:2 * NE], [(onesb.t[:], Mb.t[:])], [onesb, Mb])
            OP("dve", lambda E, g=g, tt=tt: E.tensor_tensor(rank.t[:, tt * NE:(tt + 1) * NE], g.t[:, 0:NE], runc.t[:], ALU.add), r=[g, runc], w=[rank])
            OP("dve", lambda E, g=g: E.tensor_tensor(runc.t[:], g.t[:, NE:2 * NE], runc.t[:], ALU.add), r=[g, runc], w=[runc])
        sh = BLK.bit_length() - 1
        OP("dve", lambda E: E.tensor_copy(ci.t[:], runc.t[:]), r=[runc], w=[ci])
        OP("dve", lambda E: E.tensor_single_scalar(ci.t[:], ci.t[:], BLK - 1, ALU.add), r=[ci], w=[ci])
        OP("dve", lambda E: E.tensor_single_scalar(ci.t[:], ci.t[:], sh, ALU.arith_shift_right), r=[ci], w=[ci])
        OP("dve", lambda E: E.tensor_single_scalar(ci.t[:], ci.t[:], sh, ALU.logical_shift_left), r=[ci], w=[ci])
        OP("dve", lambda E: E.tensor_copy(padded.t[:], ci.t[:]), r=[ci], w=[padded])
        OP("dve", lambda E: E.tensor_copy(ca.t[:], padded.t[:]), r=[padded], w=[ca])
        src, dst = ca, cb
        s_ = 1
        while s_ < NE:
            OP("dve", lambda E, src=src, dst=dst, s_=s_: E.tensor_copy(dst.t[:, 0:s_], src.t[:, 0:s_]), r=[src], w=[dst])
            OP("dve", lambda E, src=src, dst=dst, s_=s_: E.tensor_tensor(dst.t[:, s_:NE], src.t[:, s_:NE], src.t[:, 0:NE - s_], ALU.add), r=[src], w=[dst])
            src, dst = dst, src
            s_ *= 2
        pend = src
        OP("dve", lambda E: E.tensor_tensor(pstart.t[:], pend.t[:], padded.t[:], ALU.subtract), r=[pend, padded], w=[pstart])
        for tt in range(NTT):
            OP("dve", lambda E, tt=tt: E.tensor_tensor(base.t[:], rank.t[:, tt * NE:(tt + 1) * NE], pstart.t[:], ALU.add), r=[rank, pstart], w=[base])
            for k in range(4):
                col = tt * 4 + k
                OP("dve", lambda E, col=col: E.scalar_tensor_tensor(tmpe.t[:], iota32.t[:], idxf.t[:, col:col + 1], base.t[:], ALU.is_equal, ALU.mult),
                   r=[iota32, idxf, base], w=[tmpe])
                OP("dve", lambda E, col=col: E.reduce_sum(destf.t[:, col:col + 1], tmpe.t[:], axis=AX.X), r=[tmpe], w=[destf])
        OP("dve", lambda E: E.tensor_copy(dest_i.t[:], destf.t[:]), r=[destf], w=[dest_i])
        OP("pool", lambda E: E.iota(bstart.t[:], pattern=[[BLK, NBLK]], base=0, channel_multiplier=0, allow_small_or_imprecise_dtypes=True), w=[bstart])
        OP("pool", lambda E: E.iota(pio.t[:], pattern=[[128, KC]], base=0, channel_multiplier=1, allow_small_or_imprecise_dtypes=True), w=[pio])
        OP("dve", lambda E: E.memset(be.t[:], 0.0), w=[be])
        for e in range(NE):
            OP("dve", lambda E, e=e: E.scalar_tensor_tensor(be.t[:], bstart.t[:], pend.t[:, e:e + 1], be.t[:], ALU.is_ge, ALU.add), r=[bstart, pend, be], w=[be])
        OP("dve", lambda E: E.tensor_single_scalar(be.t[:], be.t[:], float(NE - 1), ALU.min), r=[be], w=[be])
        for kc in range(KC):
            OP("dve", lambda E, kc=kc: E.tensor_scalar(wrow_f.t[:, :, kc], be.t[:], float(D), pio.t[:, kc:kc + 1], ALU.mult, ALU.add), r=[be, pio], w=[wrow_f])
        OP("dve", lambda E: E.tensor_copy(wrow.t[:], wrow_f.t[:].rearrange("p b k -> p (b k)")), r=[wrow_f], w=[wrow])
        OP("dve", lambda E: E.tensor_scalar(brow_f.t[:], be.t[:], 128.0, pio.t[:, 0:1], ALU.mult, ALU.add), r=[be, pio], w=[brow_f])
        OP("dve", lambda E: E.tensor_copy(brow.t[:], brow_f.t[:]), r=[brow_f], w=[brow])
        OP("dve", lambda E: E.tensor_copy(erow.t[:], be.t[:]), r=[be], w=[erow])

        if STOP == 2:
            return finish()
        S.barrier()
        p2a.close()
        plog.close()
        open_stacks.remove(p2a)
        open_stacks.remove(plog)
        p3 = ExitStack()
        open_stacks.append(p3)
        hrows = Rot([S.sbuf(f"hrows{i}", [128, D], BF16, p3) for i in range(3)])
        for tt in range(NTT):
            hb = hrows.next()
            LOAD("sp", hb, hb.t[:], h2_h[tt * 128:(tt + 1) * 128, :], reads=[B_h2])
            for k in range(4):
                col = tt * 4 + k
                S.dma("pool", lambda E, hb=hb, col=col: E.indirect_dma_start(
                    out=xs_h, out_offset=bass.IndirectOffsetOnAxis(ap=dest_i.t[:, col:col + 1], axis=0),
                    in_=hb.t[:, :], in_offset=None), B_xs, reads=[hb, dest_i, B_xz], writes=[])
        B_xs.w = ("d", B_xs)

        if STOP == 3:
            return finish()
        wunit = Rot([S.sbuf(f"wunit{i}", [128, KC, D], BF16, p3) for i in range(6)])
        bgu_sb = Rot([S.sbuf(f"bgu_sb{i}", [128, 16], F32, p3) for i in range(2)])
        bd_sb = Rot([S.sbuf(f"bd_sb{i}", [128, D], F32, p3) for i in range(2)])
        xrows = Rot([S.sbuf(f"xrows{i}", [128, 4, D], BF16, p3) for i in range(2)])
        gcl = Rot([S.sbuf(f"gcl{i}", [128, BLK], F32, p3) for i in range(2)])
        sgm = Rot([S.sbuf(f"sgm{i}", [128, BLK], F32, p3) for i in range(2)])
        upc = Rot([S.sbuf(f"upc{i}", [128, BLK], F32, p3) for i in range(2)])
        ysb = Rot([S.sbuf(f"ysb{i}", [128, D], F32, p3) for i in range(3)])
        xTs = Rot([S.sbuf(f"xT{i}", [128, KC, BLK], BF16, p3) for i in range(2)])
        actTs = Rot([S.sbuf(f"actT{i}", [128, KC, BLK], BF16, p3) for i in range(2)])

        def blk_loads(blk):
            wgg = wunit.next()
            wgup = wunit.next()
            wdn = wunit.next()
            bg = bgu_sb.next()
            bd = bd_sb.next()
            for (wdst, wsrc, bb) in ((wgg, wg_bf, B_wgu), (wgup, wu_bf, B_wgu), (wdn, wd_bf, B_wd)):
                S.dma("pool", lambda E, wdst=wdst, wsrc=wsrc, blk=blk: E.indirect_dma_start(
                    out=wdst.t[:].rearrange("p k n -> p (k n)"), out_offset=None, in_=wsrc,
                    in_offset=bass.IndirectOffsetOnAxis(ap=brow.t[:, blk:blk + 1], axis=0)), wdst, reads=[bb, brow], writes=[wdst])
            S.dma("pool", lambda E, bg=bg, blk=blk: E.indirect_dma_start(
                out=bg.t[:, :], out_offset=None, in_=bgu_l_d,
                in_offset=bass.IndirectOffsetOnAxis(ap=brow.t[:, blk:blk + 1], axis=0)), bg, reads=[brow], writes=[bg])
            S.dma("pool", lambda E, bd=bd, blk=blk: E.indirect_dma_start(
                out=bd.t[:, :], out_offset=None, in_=bd_d,
                in_offset=bass.IndirectOffsetOnAxis(ap=erow.t[:, blk:blk + 1], axis=0)), bd, reads=[erow], writes=[bd])
            xw = xrows.next()
            LOAD("sp", xw, xw.t[:], xs_h[blk * BLK:(blk + 1) * BLK, :].rearrange("(r p) d -> p r d", p=128), reads=[B_xs])
            return dict(wgg=wgg, wgup=wgup, wdn=wdn, bg=bg, bd=bd, xw=xw)

        def blk_transposes(L):
            xw = L["xw"]
            xT = xTs.next()
            for half in range(2):
                for c4 in range(4):
                    kc = half * 4 + c4
                    for r in range(4):
                        TR(ptb, ptb.t[:, c4 * 512 + r * 128:c4 * 512 + (r + 1) * 128], xw.t[:, r, kc * 128:(kc + 1) * 128], identb, [xw])
                OP("act", lambda E, half=half, xT=xT: E.activation(xT.t[:, half * 4:half * 4 + 4, :], ptb.t[:].rearrange("p (c t) -> p c t", t=BLK), AF.Copy),
                   r=[ptb], w=[xT])
            L["xT"] = xT

        def blk_gu(L):
            wgg, wgup, bg, xT = L["wgg"], L["wgup"], L["bg"], L["xT"]
            actT = actTs.next()
            L["actT"] = actT
            for f in range(KC):
                g_g = Grot.next()
                MM(g_g, g_g.t[:], [(wgg.t[:, kc, f * 128:(f + 1) * 128], xT.t[:, kc, :]) for kc in range(KC)], [wgg, xT])
                g_u = Grot.next()
                MM(g_u, g_u.t[:], [(wgup.t[:, kc, f * 128:(f + 1) * 128], xT.t[:, kc, :]) for kc in range(KC)], [wgup, xT])
                gc_, sg_, up_ = gcl.next(), sgm.next(), upc.next()
                OP("dve", lambda E, g=g_g, gc_=gc_, f=f, bg=bg: E.tensor_scalar(gc_.t[:], g.t[:], bg.t[:, f:f + 1], 7.0, ALU.add, ALU.min), r=[g_g, bg], w=[gc_])
                OP("act", lambda E, gc_=gc_, sg_=sg_: E.activation(sg_.t[:], gc_.t[:], AF.Sigmoid, scale=1.702), r=[gc_], w=[sg_])
                OP("dve", lambda E, g=g_u, up_=up_, f=f, bg=bg: E.tensor_scalar(up_.t[:], g.t[:], bg.t[:, 8 + f:9 + f], 7.0, ALU.add, ALU.min), r=[g_u, bg], w=[up_])
                OP("pool", lambda E, up_=up_: E.tensor_scalar(up_.t[:], up_.t[:], -7.0, 1.0, ALU.max, ALU.add), r=[up_], w=[up_])
                OP("pool", lambda E, gc_=gc_, sg_=sg_: E.tensor_tensor(gc_.t[:], gc_.t[:], sg_.t[:], ALU.mult), r=[gc_, sg_], w=[gc_])
                OP("dve", lambda E, gc_=gc_, up_=up_, f=f, actT=actT: E.tensor_tensor(actT.t[:, f, :], gc_.t[:], up_.t[:], ALU.mult), r=[gc_, up_], w=[actT])

        def blk_down(L, blk):
            wdn, bd, actT = L["wdn"], L["bd"], L["actT"]
            for r in range(4):
                yb = ysb.next()
                for dh in range(2):
                    g = Grot.next()
                    MM(g, g.t[:], [(actT.t[:, fc, r * 128:(r + 1) * 128], wdn.t[:, fc, dh * 512:(dh + 1) * 512]) for fc in range(KC)], [actT, wdn])
                    OP("dve", lambda E, g=g, yb=yb, dh=dh, bd=bd: E.tensor_tensor(yb.t[:, dh * 512:(dh + 1) * 512], g.t[:], bd.t[:, dh * 512:(dh + 1) * 512], ALU.add),
                       r=[g, bd], w=[yb])
                row0 = blk * BLK + r * 128
                S.dma("sp", lambda E, yb=yb, row0=row0: E.dma_start(out=ys_h[row0:row0 + 128, :], in_=yb.t[:]), yb, reads=[yb], writes=[])

        cur = blk_loads(0)
        blk_transposes(cur)
        for blk in range(NBLK):
            nxt = blk_loads(blk + 1) if blk + 1 < NBLK else None
            blk_gu(cur)
            if nxt is not None:
                blk_transposes(nxt)
            blk_down(cur, blk)
            cur = nxt

        if STOP == 4:
            return finish()
        S.barrier()
        p3.close()
        open_stacks.remove(p3)

        p5 = ExitStack()
        open_stacks.append(p5)
        gfin_bc = S.sbuf("gfin_bc", [128, D], F32, p5)
        LOAD("sp", gfin_bc, gfin_bc.t[:], gfin_bc_d)
        x1r = Rot([S.sbuf(f"x1r{i}", [128, D], F32, p5) for i in range(2)])
        ygat = Rot([S.sbuf(f"ygat{i}", [128, D], F32, p5) for i in range(4)])
        accb = Rot([S.sbuf(f"accb{i}", [128, D], F32, p5) for i in range(2)])
        junk5 = S.sbuf("junk5", [128, D], BF16, p5)
        bct5 = S.sbuf("bct5", [128, D], F32, p5)
        M.update({"wadab": Rot([S.sbuf(f"wadab5{i}", [128, KC, 128], F32, p5) for i in range(2)]),
                  "badab": Rot([S.sbuf(f"badab5{i}", [128, 128], F32, p5) for i in range(2)]),
                  "condrep": S.sbuf("condrep5", [128, KC, 128], F32, p5), "bct": bct5, "off": 24})
        ss5 = S.sbuf("ss5", [128, 2], F32, p5)
        for b in range(NSEQ):
            mod_bc(b, range(24, 32))
            for it in range(SEQ // 128):
                tt = b * (SEQ // 128) + it
                xb = x1r.next()
                LOAD("sp", xb, xb.t[:], x1_h[tt * 128:(tt + 1) * 128, :], reads=[B_x1])
                acc = accb.next()
                for k in range(4):
                    col = tt * 4 + k
                    yk = ygat.next()
                    S.dma("pool", lambda E, yk=yk, col=col: E.indirect_dma_start(
                        out=yk.t[:, :], out_offset=None, in_=ys_h,
                        in_offset=bass.IndirectOffsetOnAxis(ap=dest_i.t[:, col:col + 1], axis=0)), yk, reads=[B_ys, dest_i], writes=[yk])
                    if k == 0:
                        OP("dve", lambda E, yk=yk, acc=acc, col=col: E.tensor_scalar(acc.t[:], yk.t[:], wts.t[:, col:col + 1], None, ALU.mult), r=[yk, wts], w=[acc])
                    else:
                        eng = "dve"
                        OP(eng, lambda E, yk=yk, acc=acc, col=col: E.scalar_tensor_tensor(acc.t[:], yk.t[:], wts.t[:, col:col + 1], acc.t[:], ALU.mult, ALU.add),
                           r=[yk, wts, acc], w=[acc])
                OP("dve", lambda E, acc=acc: E.tensor_tensor(acc.t[:], acc.t[:], bct5.t[:, GT2:GT2 + D], ALU.mult), r=[acc, bct5], w=[acc])
                OP("pool", lambda E, acc=acc, xb=xb: E.tensor_tensor(acc.t[:], acc.t[:], xb.t[:], ALU.add), r=[acc, xb], w=[acc])
                OP("act", lambda E, acc=acc: E.activation(junk5.t[:], acc.t[:], AF.Square, accum_out=ss5.t[:, 0:1]), r=[acc], w=[junk5, ss5])
                OP("act", lambda E: E.activation(ss5.t[:, 0:1], ss5.t[:, 0:1], AF.Sqrt, bias=epst.t[:, 0:1], scale=1.0 / D), r=[ss5, epst], w=[ss5])
                OP("dve", lambda E: E.reciprocal(ss5.t[:, 1:2], ss5.t[:, 0:1]), r=[ss5], w=[ss5])
                OP("dve", lambda E, acc=acc: E.scalar_tensor_tensor(acc.t[:], acc.t[:], ss5.t[:, 1:2], gfin_bc.t[:], ALU.mult, ALU.mult), r=[acc, ss5, gfin_bc], w=[acc])
                S.dma("sp", lambda E, acc=acc, tt=tt: E.dma_start(out=out_d[tt * 128:(tt + 1) * 128, :], in_=acc.t[:]), acc, reads=[acc], writes=[])
        return finish()
    return nc


def _prep_shared(inp):
    f = lambda a: np.ascontiguousarray(np.asarray(a, dtype=np.float32))
    rep = lambda v: f(np.broadcast_to(np.asarray(v, np.float32).reshape(1, -1), (128, np.asarray(v).size)))
    fm = lambda v: f(np.asarray(v, np.float32).reshape(-1, 128).T)
    l = 0
    b_ada = np.asarray(inp["b_ada"][l], np.float32)
    d = {
        "w_ada": f(inp["w_ada"][l]),
        "b_ada_fm": fm(b_ada[:2 * D]),
        "b_ada_bc": rep(b_ada[2 * D:]),
        "g_mix_fm": fm(inp["g_mix"][l]),
        "g_ffn_bc": rep(inp["g_ffn"][l]),
        "g_final_bc": rep(inp["g_final"]),
        "w_in": f(inp["w_in"][l]),
        "w_sconv_fm": f(np.asarray(inp["w_sconv"][l], np.float32).reshape(3, KC, 128).transpose(2, 1, 0).reshape(128, KC * 3)),
        "w_sconv_out": f(inp["w_sconv_out"][l]),
        "w_ssm_conv_fm": f(np.asarray(inp["w_ssm_conv"][l], np.float32).reshape(4, 24, 128).transpose(2, 1, 0).reshape(128, 96)),
        "b_ssm_conv_fm": fm(inp["b_ssm_conv"][l]),
        "dt_bias_bc": rep(inp["dt_bias"][l]),
        "a_log_bc": rep(inp["a_log"][l]),
        "d_skip_bc": rep(inp["d_skip"][l]),
        "g_ssm_bc": rep(inp["g_ssm_norm"][l]),
        "w_ssm_out": f(inp["w_ssm_out"][l]),
        "w_o": f(inp["w_o"][l]),
        "w_router": f(inp["w_router"][l]),
        "b_router_bc": rep(inp["b_router"][l]),
        "w_gu": f(np.asarray(inp["w_gu"][l], np.float32).reshape(NE * D, 2 * D)),
        "b_gu_l": f(np.asarray(inp["b_gu"][l], np.float32).reshape(NE, 16, 128).transpose(0, 2, 1).reshape(NE * 128, 16)),
        "w_down": f(np.asarray(inp["w_down"][l], np.float32).reshape(NE * D, D)),
        "b_down": f(inp["b_down"][l]),
    }
    return d


_NC_CACHE = {}
_STOP = 99
_JLIST = (0, 1)


def kernel(**inputs):
    x = np.asarray(inputs["x"], np.float32)
    c = np.asarray(inputs["c"], np.float32)
    bsz, seq, _ = x.shape
    ncores = 8
    nseq = bsz // ncores
    key = (nseq, seq)
    if key not in _NC_CACHE:
        _NC_CACHE[key] = build_nc(nseq, seq, _STOP)
    nc = _NC_CACHE[key]
    shared = _prep_shared(inputs)
    in_maps = []
    for i in range(ncores):
        m = dict(shared)
        m["x"] = np.ascontiguousarray(x[i * nseq:(i + 1) * nseq].reshape(nseq * seq, D))
        cc = c[i * nseq:(i + 1) * nseq]
        m["cT"] = np.ascontiguousarray(cc.reshape(nseq, KC, 128).transpose(2, 1, 0).reshape(128, KC * nseq))
        in_maps.append(m)
    res = run_bass_kernel_spmd(nc, in_maps, core_ids=list(range(ncores)))
    out = np.concatenate([np.asarray(r["out"]).reshape(nseq, seq, D) for r in res.results], axis=0)
    return out.astype(np.float32)
```

```python
import numpy as np
from contextlib import ExitStack
import concourse.bass as bass
import concourse.mybir as mybir
from concourse.bass_utils import run_bass_kernel_spmd

F32 = mybir.dt.float32
BF16 = mybir.dt.bfloat16
I32 = mybir.dt.int32
U32 = mybir.dt.uint32
ALU = mybir.AluOpType
AF = mybir.ActivationFunctionType
AX = mybir.AxisListType

ENGS = ("pe", "act", "dve", "pool", "sp")
SAME_ENGINE_SYNC = ("act", "dve", "pool")
SEM_EPOCH = 30000
LAZY = ("pe",)

D = 1024
KC = 8
DI = 2048
NH = 32
HP = 64
NG = 4
NS = 128
NCOL = 10272
C_SB, C_SC, C_SX, C_Z, C_XBC, C_DT, C_GA, C_GB = 0, 1024, 2048, 3072, 5120, 8192, 8224, 9248
NE = 32
TOPK = 4
BLK = 512
T = 256
EPS = 1e-6


class Buf:
    __slots__ = ("t", "w", "r", "sem", "semcnt", "name")

    def __init__(self, t, name=""):
        self.t = t
        self.w = None
        self.r = []
        self.sem = None
        self.semcnt = 0
        self.name = name


class Rot:
    def __init__(self, bufs):
        self.bufs = bufs
        self.i = 0

    def next(self):
        b = self.bufs[self.i % len(self.bufs)]
        self.i += 1
        return b


class Sched:
    def __init__(self, nc, stack):
        self.nc = nc
        self.stack = stack
        self.q = {e: [] for e in ENGS}
        self.nsem = 0
        self.dma_bufs = []
        self.esem = {}
        self.ecnt = {}
        self.allsems = {e: [] for e in ENGS}
        self.lastcell = {}
        for e in ENGS:
            self._new_epoch(e)
        self.waited = {e: {} for e in ENGS}

    def _new_sem(self, name):
        self.nsem += 1
        return self.stack.enter_context(self.nc.semaphore(f"{name}_{self.nsem}"))

    def _new_epoch(self, e):
        self.esem[e] = self._new_sem("e" + e)
        self.ecnt[e] = 0
        self.allsems[e].append(self.esem[e])

    def sbuf(self, name, shape, dt, stack=None):
        st = stack or self.stack
        return Buf(st.enter_context(self.nc.sbuf_tensor("s_" + name, list(shape), dt)), name)

    def psum(self, name, shape, dt):
        return Buf(self.stack.enter_context(self.nc.psum_tensor("p_" + name, list(shape), dt)), name)

    def _flush(self, e):
        cell = self.lastcell.get(e)
        if cell is None or cell["inc"]:
            return
        cell["inc"] = True
        self.ecnt[e] += 1
        self.lastcell[e] = None
        if self.ecnt[e] >= SEM_EPOCH:
            self._new_epoch(e)

    def _collect(self, eng, reads, writes):
        waits = {}

        def need(ev):
            if ev is None:
                return
            if ev[0] == "e":
                _, e2, sem, c = ev
                if e2 == eng and eng not in SAME_ENGINE_SYNC:
                    return
                if sem is self.esem[e2] and c > self.ecnt[e2]:
                    self._flush(e2)
                val = c
            else:
                sem = ev[1].sem
                val = ev[1].semcnt
            key = id(sem)
            if key not in waits or waits[key][1] < val:
                waits[key] = (sem, val)

        for b in reads:
            need(b.w)
        for b in writes:
            need(b.w)
            for ev in b.r:
                need(ev)
        out = []
        wd = self.waited[eng]
        for key, (sem, val) in waits.items():
            if wd.get(key, 0) >= val:
                continue
            wd[key] = val
            out.append((sem, val))
        return out

    def op(self, eng, fn, reads=(), writes=(), inc=True):
        waits = self._collect(eng, reads, writes)
        sem = self.esem[eng]
        cnt = self.ecnt[eng] + 1
        if eng in LAZY:
            cell = {"inc": False}
            self.lastcell[eng] = cell
        else:
            cell = {"inc": True}
            self.ecnt[eng] = cnt
            if cnt >= SEM_EPOCH:
                self._new_epoch(eng)

        def rec(E, waits=waits, fn=fn, sem=sem, cell=cell):
            for s, v in waits:
                E.wait_ge(s, v)
            ins = fn(E)
            if cell["inc"]:
                ins.then_inc(sem, 1)

        self.q[eng].append(rec)
        ev = ("e", eng, sem, cnt)
        for b in writes:
            b.w = ev
            b.r = []
        for b in reads:
            b.r.append(ev)

    def dma(self, eng, fn, owner, reads=(), writes=()):
        waits = self._collect(eng, reads, writes)
        if owner.sem is None:
            owner.sem = self._new_sem("d" + owner.name)
            self.dma_bufs.append(owner)
        owner.semcnt += 16
        sem = owner.sem

        def rec(E, waits=waits, fn=fn, sem=sem):
            for s, v in waits:
                E.wait_ge(s, v)
            fn(E).then_inc(sem, 16)

        self.q[eng].append(rec)
        ev = ("d", owner)
        for b in writes:
            b.w = ev
            b.r = []
        for b in reads:
            b.r.append(ev)

    def barrier(self):
        for e in ENGS:
            self._flush(e)
        for eng in ENGS:
            waits = []
            wd = self.waited[eng]
            for e2 in ENGS:
                if e2 == eng or self.ecnt[e2] == 0:
                    continue
                sem, val = self.esem[e2], self.ecnt[e2]
                if wd.get(id(sem), 0) < val:
                    wd[id(sem)] = val
                    waits.append((sem, val))
            for ob in self.dma_bufs:
                if wd.get(id(ob.sem), 0) < ob.semcnt:
                    wd[id(ob.sem)] = ob.semcnt
                    waits.append((ob.sem, ob.semcnt))

            def rec(E, waits=waits):
                for s, v in waits:
                    E.wait_ge(s, v)

            self.q[eng].append(rec)

    def emit(self):
        q = self.q
        with self.nc.Block() as block:
            @block.sync
            def _(E):
                for r in q["sp"]:
                    r(E)

            @block.tensor
            def _(E):
                for r in q["pe"]:
                    r(E)

            @block.scalar
            def _(E):
                for r in q["act"]:
                    r(E)

            @block.vector
            def _(E):
                for r in q["dve"]:
                    r(E)

            @block.gpsimd
            def _(E):
                for r in q["pool"]:
                    r(E)


def build_nc(NSEQ, SEQ, STOP=99):
    nc = bass.Bass("TRN2", target_bir_lowering=False)
    NT = NSEQ * SEQ
    NTT = NT // 128
    NTL = SEQ // T
    NROWS = NT * TOPK
    NBLK = NROWS // BLK + NE
    RTOT = NBLK * BLK

    def din(name, shape, dt=F32):
        return nc.dram_tensor(name, list(shape), dt, kind="ExternalInput").ap()

    def dscr(name, shape, dt):
        return nc.dram_tensor(name, list(shape), dt, kind="Internal").ap()

    x_d = din("x", [NT, D])
    cT_d = din("cT", [128, KC * NSEQ])
    wada_d = din("w_ada", [D, 6 * D])
    bada_fm_d = din("b_ada_fm", [128, 16])
    bada_bc_d = din("b_ada_bc", [128, 4 * D])
    gmix_fm_d = din("g_mix_fm", [128, KC])
    gffn_bc_d = din("g_ffn_bc", [128, D])
    gfin_bc_d = din("g_final_bc", [128, D])
    win_d = din("w_in", [D, NCOL])
    wsc_fm_d = din("w_sconv_fm", [128, KC * 3])
    wsco_d = din("w_sconv_out", [D, D])
    wxc_fm_d = din("w_ssm_conv_fm", [128, 24 * 4])
    bxc_fm_d = din("b_ssm_conv_fm", [128, 24])
    dtb_bc_d = din("dt_bias_bc", [128, NH])
    alog_bc_d = din("a_log_bc", [128, NH])
    dsk_bc_d = din("d_skip_bc", [128, NH])
    gssm_bc_d = din("g_ssm_bc", [128, DI])
    wsso_d = din("w_ssm_out", [DI, D])
    wo_d = din("w_o", [D, D])
    wr_d = din("w_router", [D, NE])
    br_bc_d = din("b_router_bc", [128, NE])
    wgu_d = din("w_gu", [NE * D, 2 * D])
    bgu_l_d = din("b_gu_l", [NE * 128, 16])
    wd_d = din("w_down", [NE * D, D])
    bd_d = din("b_down", [NE, D])
    out_d = nc.dram_tensor("out", [NT, D], F32, kind="ExternalOutput").ap()

    win_bf = dscr("win_bf", [D, NCOL], BF16)
    wsco_bf = dscr("wsco_bf", [D, D], BF16)
    wsso_bf = dscr("wsso_bf", [DI, D], BF16)
    wo_bf = dscr("wo_bf", [D, D], BF16)
    wg_bf = dscr("wg_bf", [NE * 128, KC * D], BF16)
    wu_bf = dscr("wu_bf", [NE * 128, KC * D], BF16)
    wd_bf = dscr("wd_bf", [NE * 128, KC * D], BF16)
    x1_h = dscr("x1_h", [NT, D], F32)
    h2_h = dscr("h2_h", [NT, D], BF16)
    xs_h = dscr("xs_h", [RTOT, D], BF16)
    ys_h = dscr("ys_h", [RTOT, D], F32)

    with ExitStack() as st:
        S = Sched(nc, st)
        def OP(eng, fn, r=(), w=()):
            S.op(eng, fn, reads=r, writes=w)

        def MM(outb, out_ap, pairs, reads):
            n = len(pairs)
            for i, (l, r_) in enumerate(pairs):
                S.op("pe", lambda E, l=l, r_=r_, i=i: E.matmul(out_ap, l, r_, start=(i == 0), stop=(i == n - 1)),
                     reads=reads, writes=[outb], inc=(i == n - 1))

        def TR(outb, out_ap, in_ap, idn, reads):
            S.op("pe", lambda E: E.transpose(out_ap, in_ap, idn.t[:]), reads=list(reads) + [idn], writes=[outb])

        def LOAD(eng, dst, dst_ap, src_ap, reads=()):
            S.dma(eng, lambda E: E.dma_start(out=dst_ap, in_=src_ap), dst, reads=reads, writes=[dst])

        open_stacks = []

        def finish():
            S.barrier()
            S.emit()
            for stx in reversed(open_stacks):
                stx.close()
            return nc

        B_win, B_wsco, B_wsso, B_wo = Buf(None, "winbf"), Buf(None, "wscobf"), Buf(None, "wssobf"), Buf(None, "wobf")
        B_wgu, B_wd = Buf(None, "wgubf"), Buf(None, "wdbf")
        B_x1, B_h2, B_xs, B_ys, B_out = Buf(None, "x1h"), Buf(None, "h2h"), Buf(None, "xsh"), Buf(None, "ysh"), Buf(None, "outh")

        for kc in range(KC):
            S.dma("pool", lambda E, kc=kc: E.dma_start(out=win_bf[kc * 128:(kc + 1) * 128, :], in_=win_d[kc * 128:(kc + 1) * 128, :]),
                  B_win, writes=[B_win])
        for (dst, src, bb, rows) in ((wsco_bf, wsco_d, B_wsco, D), (wsso_bf, wsso_d, B_wsso, DI), (wo_bf, wo_d, B_wo, D)):
            for r0 in range(0, rows, 512):
                S.dma("pool", lambda E, dst=dst, src=src, r0=r0: E.dma_start(out=dst[r0:r0 + 512, :], in_=src[r0:r0 + 512, :]),
                      bb, writes=[bb])

        G = [S.psum(f"G{i}", [128, 512], F32) for i in range(5)]
        ptb = S.psum("ptb", [128, 2048], BF16)
        ptB = S.psum("ptB", [128, 1024], BF16)
        Grot = Rot(G)

        io = S.sbuf("io", [128, 128], F32)
        ident = S.sbuf("ident", [128, 128], F32)
        identb = S.sbuf("identb", [128, 128], BF16)
        triu = S.sbuf("triu", [128, 128], F32)
        ones = S.sbuf("ones", [128, 128], F32)
        onesb = S.sbuf("onesb", [128, 128], BF16)
        tristb = S.sbuf("tristb", [128, 128], BF16)
        epst = S.sbuf("epst", [128, 1], F32)
        iota32 = S.sbuf("iota32", [128, NE], F32)
        OP("pool", lambda E: E.iota(io.t[:], pattern=[[1, 128]], base=0, channel_multiplier=-1,
                                    allow_small_or_imprecise_dtypes=True), w=[io])
        OP("pool", lambda E: E.iota(iota32.t[:], pattern=[[1, NE]], base=0, channel_multiplier=0,
                                    allow_small_or_imprecise_dtypes=True), w=[iota32])
        OP("dve", lambda E: E.tensor_single_scalar(ident.t[:], io.t[:], 0.0, ALU.is_equal), r=[io], w=[ident])
        OP("dve", lambda E: E.tensor_single_scalar(identb.t[:], io.t[:], 0.0, ALU.is_equal), r=[io], w=[identb])
        OP("dve", lambda E: E.tensor_single_scalar(triu.t[:], io.t[:], 0.0, ALU.is_ge), r=[io], w=[triu])
        OP("dve", lambda E: E.tensor_single_scalar(tristb.t[:], io.t[:], 0.0, ALU.is_gt), r=[io], w=[tristb])
        OP("dve", lambda E: E.memset(ones.t[:], 1.0), w=[ones])
        OP("dve", lambda E: E.memset(onesb.t[:], 1.0), w=[onesb])
        OP("dve", lambda E: E.memset(epst.t[:], EPS), w=[epst])

        def small(name, shape, src, dt=F32):
            b = S.sbuf(name, shape, dt)
            LOAD("sp", b, b.t[:], src)
            return b

        cT = small("cT", [128, KC * NSEQ], cT_d)
        bada_fm = small("bada_fm", [128, 16], bada_fm_d)
        gmix_fm = small("gmix_fm", [128, KC], gmix_fm_d)
        wsc_fm = small("wsc_fm", [128, KC * 3], wsc_fm_d)
        wxc_fm = small("wxc_fm", [128, 96], wxc_fm_d)
        bxc_fm = small("bxc_fm", [128, 24], bxc_fm_d)
        dtb_bc = small("dtb_bc", [128, NH], dtb_bc_d)
        a_bc = small("a_bc", [128, NH], alog_bc_d)
        dsk_bc = small("dsk_bc", [128, NH], dsk_bc_d)
        br_bc = small("br_bc", [128, NE], br_bc_d)
        wr_sb = S.sbuf("wr_sb", [128, KC, NE], F32)
        LOAD("sp", wr_sb, wr_sb.t[:], wr_d.rearrange("(kc p) n -> p kc n", p=128))
        wdt_sb = S.sbuf("wdt_sb", [128, KC, NH], BF16)
        OP("act", lambda E: E.activation(a_bc.t[:], a_bc.t[:], AF.Exp), r=[a_bc], w=[a_bc])
        OP("dve", lambda E: E.tensor_single_scalar(a_bc.t[:], a_bc.t[:], -1.0, ALU.mult), r=[a_bc], w=[a_bc])
        OP("act", lambda E: E.activation(cT.t[:], cT.t[:], AF.Silu), r=[cT], w=[cT])

        gs1 = S.sbuf("gs1", [128, KC * NSEQ], F32)
        sh1 = S.sbuf("sh1", [128, KC * NSEQ], F32)
        wts = S.sbuf("wts", [128, NTT * TOPK], F32)
        dest_i = S.sbuf("dest_i", [128, NTT * TOPK], I32)
        wrow = S.sbuf("wrow", [128, NBLK * KC], I32)
        brow = S.sbuf("brow", [128, NBLK], I32)
        erow = S.sbuf("erow", [128, NBLK], I32)
        plog = ExitStack()
        p1 = ExitStack()
        open_stacks.extend([plog, p1])
        logits = S.sbuf("logits", [128, NTT * NE], F32, plog)
        wadab = Rot([S.sbuf(f"wadab{i}", [128, KC, 128], F32, p1) for i in range(2)])
        badab = Rot([S.sbuf(f"badab{i}", [128, 128], F32, p1) for i in range(2)])
        condrep = S.sbuf("condrep", [128, KC, 128], F32, p1)
        bct = S.sbuf("bct", [128, 3 * D], F32, p1)
        gffn_bc = S.sbuf("gffn_bc", [128, D], F32, p1)
        LOAD("sp", gffn_bc, gffn_bc.t[:], gffn_bc_d)
        M = {"wadab": wadab, "badab": badab, "condrep": condrep, "bct": bct, "off": 0}
        wada_r = wada_d.rearrange("(kc p) n -> p kc n", p=128)

        for ch in range(16):
            wb = wadab.next()
            LOAD("sp", wb, wb.t[:], wada_r[:, :, ch * 128:(ch + 1) * 128])
            for jj in range(1):
                j = ch
                g = Grot.next()
                MM(g, g.t[:, 0:NSEQ], [(wb.t[:, kc, jj * 128:(jj + 1) * 128], cT.t[:, kc * NSEQ:(kc + 1) * NSEQ]) for kc in range(KC)], [wb, cT])
                if j < 8:
                    for b in range(NSEQ):
                        OP("act", lambda E, g=g, j=j, b=b: E.activation(sh1.t[:, j * NSEQ + b:j * NSEQ + b + 1], g.t[:, b:b + 1], AF.Identity,
                                                                         bias=bada_fm.t[:, j:j + 1], scale=1.0), r=[g, bada_fm], w=[sh1])
                else:
                    c = j - 8
                    for b in range(NSEQ):
                        OP("act", lambda E, g=g, j=j, b=b, c=c: E.activation(gs1.t[:, c * NSEQ + b:c * NSEQ + b + 1], g.t[:, b:b + 1], AF.Identity,
                                                                              bias=bada_fm.t[:, j:j + 1], scale=1.0), r=[g, bada_fm], w=[gs1])
                        OP("dve", lambda E, b=b, c=c: E.tensor_scalar(gs1.t[:, c * NSEQ + b:c * NSEQ + b + 1], gs1.t[:, c * NSEQ + b:c * NSEQ + b + 1],
                                                                     1.0, gmix_fm.t[:, c:c + 1], ALU.add, ALU.mult), r=[gs1, gmix_fm], w=[gs1])

        def mod_bc(b, chunks):
            wadab, badab, condrep, bct, off = M["wadab"], M["badab"], M["condrep"], M["bct"], M["off"]
            for kc in range(KC):
                OP("dve", lambda E, kc=kc: E.tensor_copy(condrep.t[:, kc, :], cT.t[:, kc * NSEQ + b:kc * NSEQ + b + 1].to_broadcast([128, 128])),
                   r=[cT], w=[condrep])
            for ch in chunks:
                wb = wadab.next()
                bb = badab.next()
                LOAD("sp", wb, wb.t[:], wada_r[:, :, 2 * D + ch * 128:2 * D + (ch + 1) * 128])
                LOAD("sp", bb, bb.t[:], bada_bc_d[:, ch * 128:(ch + 1) * 128])
                g = Grot.next()
                MM(g, g.t[:, 0:128], [(condrep.t[:, kc, :], wb.t[:, kc, :]) for kc in range(KC)], [wb, condrep])
                dsl = slice((ch - off) * 128, (ch - off + 1) * 128)
                OP("dve", lambda E, g=g, bb=bb, dsl=dsl: E.tensor_tensor(bct.t[:, dsl], g.t[:, 0:128], bb.t[:], ALU.add),
                   r=[g, bb], w=[bct])
                if 16 <= ch < 24:
                    OP("dve", lambda E, ch=ch, dsl=dsl: E.scalar_tensor_tensor(bct.t[:, dsl], bct.t[:, dsl], 1.0,
                                                                      gffn_bc.t[:, (ch - 16) * 128:(ch - 15) * 128], ALU.add, ALU.mult),
                       r=[bct, gffn_bc], w=[bct])

        GT1, SH2, GS2, GT2 = 0, D, 2 * D, 0
        if STOP == 0:
            return finish()

        gssm_bc = S.sbuf("gssm_bc", [128, DI], F32, p1)
        LOAD("sp", gssm_bc, gssm_bc.t[:], gssm_bc_d)
        LOAD("sp", wdt_sb, wdt_sb.t[:], win_bf.rearrange("(kc p) n -> p kc n", p=128)[:, :, C_DT:C_DT + NH], reads=[B_win])
        wst = Rot([S.sbuf(f"wst{i}", [128, KC, 512], BF16, p1) for i in range(4)])
        xr = Rot([S.sbuf(f"xr{i}", [128, D], F32, p1) for i in range(2)])
        junk = S.sbuf("junk", [128, D], BF16, p1)
        ss = S.sbuf("ss", [128, 4], F32, p1)
        rstd = S.sbuf("rstd", [128, 4], F32, p1)
        hT = S.sbuf("hT", [128, KC, T], BF16, p1)
        vT = S.sbuf("vT", [128, KC, T], BF16, p1)
        tmpA = Rot([S.sbuf(f"tmpA{i}", [128, T], F32, p1) for i in range(2)])
        tmpY = Rot([S.sbuf(f"tmpY{i}", [128, T], F32, p1) for i in range(2)])
        utmp = Rot([S.sbuf(f"utmp{i}", [128, T + 2], F32, p1) for i in range(2)])
        uhalo = S.sbuf("uhalo", [128, KC, 2], F32, p1)
        xraw = Rot([S.sbuf(f"xraw{i}", [128, T + 3], F32, p1) for i in range(2)])
        xhalo = S.sbuf("xhalo", [128, 24, 3], F32, p1)
        xsT = S.sbuf("xsT", [128, 16, T], BF16, p1)
        BT = S.sbuf("BT", [128, NG, T], BF16, p1)
        CT = S.sbuf("CT", [128, NG, T], BF16, p1)
        zs = S.sbuf("zs", [128, 2, DI], BF16, p1)
        dtt = S.sbuf("dtt", [128, 2, NH], F32, p1)
        dat = S.sbuf("dat", [128, 2, NH], F32, p1)
        dtmp = S.sbuf("dtmp", [128, NH], F32, p1)
        acum = S.sbuf("acum", [128, NH], F32, p1)
        dend = S.sbuf("dend", [128, NH], F32, p1)
        dchunk = S.sbuf("dchunk", [128, NH], F32, p1)
        eacum = S.sbuf("eacum", [128, NH], F32, p1)
        xs_tok = S.sbuf("xs_tok", [128, DI], BF16, p1)
        xdt = S.sbuf("xdt", [128, DI], BF16, p1)
        xdte = S.sbuf("xdte", [128, DI], BF16, p1)
        B_tok = S.sbuf("B_tok", [128, NG * NS], BF16, p1)
        CBm = S.sbuf("CBm", [128, NG, 128], F32, p1)
        seg = Rot([S.sbuf(f"seg{i}", [128, 4, 128], F32, p1) for i in range(2)])
        Sb = S.sbuf("Sb", [128, 8, 128], BF16, p1)
        st32 = S.sbuf("st32", [128, DI], F32, p1)
        stbf = S.sbuf("stbf", [128, DI], BF16, p1)
        yg = Rot([S.sbuf(f"yg{i}", [128, 512], F32, p1) for i in range(2)])
        yt2 = Rot([S.sbuf(f"yt2{i}", [128, 512], F32, p1) for i in range(1)])
        yn = Rot([S.sbuf(f"yn{i}", [128, 512], BF16, p1) for i in range(2)])
        ssg = S.sbuf("ssg", [128, 2], F32, p1)
        ynT = S.sbuf("ynT", [128, 16, T], BF16, p1)
        mpart = S.sbuf("mpart", [128, 4, T], F32, p1)
        mT = S.sbuf("mT", [128, KC, T], BF16, p1)
        h2 = S.sbuf("h2", [128, D], F32, p1)
        h2b = Rot([S.sbuf(f"h2b{i}", [128, D], BF16, p1) for i in range(1)])
        h2T = S.sbuf("h2T", [128, KC, 128], F32, p1)

        win_r = win_bf.rearrange("(kc p) n -> p kc n", p=128)
        wsco_r = wsco_bf.rearrange("(kc p) n -> p kc n", p=128)
        wsso_r = wsso_bf.rearrange("(kc p) n -> p kc n", p=128)
        wo_r = wo_bf.rearrange("(kc p) n -> p kc n", p=128)

        def wload(src_r, bb, c0, k0=0):
            w = wst.next()
            LOAD("sp", w, w.t[:], src_r[:, k0:k0 + KC, c0:c0 + 512], reads=[bb])
            return w

        def rms_stats(src, col, n):
            OP("act", lambda E: E.activation(junk.t[:, 0:n], src, AF.Square, accum_out=ss.t[:, col:col + 1]), r=[], w=[junk, ss])
            OP("act", lambda E: E.activation(ss.t[:, col:col + 1], ss.t[:, col:col + 1], AF.Sqrt, bias=epst.t[:, 0:1], scale=1.0 / n),
               r=[ss, epst], w=[ss])
            OP("dve", lambda E: E.reciprocal(rstd.t[:, col:col + 1], ss.t[:, col:col + 1]), r=[ss], w=[rstd])

        cast_jobs = []
        for e in range(NE):
            cast_jobs.append((wg_bf, wgu_d, e, 0, B_wgu))
            cast_jobs.append((wu_bf, wgu_d, e, D, B_wgu))
            cast_jobs.append((wd_bf, wd_d, e, 0, B_wd))

        def issue_casts(n):
            for _ in range(n):
                if not cast_jobs:
                    return
                dst, src, e, c0, bb = cast_jobs.pop(0)
                S.dma("pool", lambda E, dst=dst, src=src, e=e, c0=c0: E.dma_start(
                    out=dst[e * 128:(e + 1) * 128, :].rearrange("p (k n) -> p k n", n=D),
                    in_=src[e * D:(e + 1) * D, c0:c0 + D].rearrange("(k p) n -> p k n", p=128)), bb, writes=[bb])

        casts_per_tile = -(-len(cast_jobs) // (NSEQ * NTL))
        zt = S.sbuf("zt", [128, 2 * D], BF16, p1)
        OP("pool", lambda E: E.memset(zt.t[:], 0.0), w=[zt])
        B_xz = Buf(None, "xsz")
        zero_jobs = list(range(0, RTOT, 256))

        def issue_zero(n):
            for _ in range(n):
                if not zero_jobs:
                    return
                q0 = zero_jobs.pop(0)
                S.dma("sp", lambda E, q0=q0: E.dma_start(out=xs_h[q0:q0 + 256, :].rearrange("(p r) d -> p (r d)", r=2), in_=zt.t[:]),
                      B_xz, reads=[zt], writes=[])
            B_xz.w = ("d", B_xz)

        zeros_per_tile = -(-len(zero_jobs) // (NSEQ * NTL))
        for b in range(NSEQ):
            mod_bc(b, range(0, 24))
            OP("dve", lambda E: E.memset(st32.t[:], 0.0), w=[st32])
            OP("dve", lambda E: E.memset(stbf.t[:], 0.0), w=[stbf])
            OP("dve", lambda E: E.memset(uhalo.t[:], 0.0), w=[uhalo])
            OP("dve", lambda E: E.memset(xhalo.t[:], 0.0), w=[xhalo])
            for it in range(NTL):
                r0 = b * SEQ + it * T
                issue_casts(casts_per_tile)
                issue_zero(zeros_per_tile)
                xs_ = []
                for j in range(2):
                    xb = xr.next()
                    xs_.append(xb)
                    LOAD("sp", xb, xb.t[:], x_d[r0 + j * 128:r0 + (j + 1) * 128, :])
                    OP("act", lambda E, xb=xb, j=j: E.activation(junk.t[:], xb.t[:], AF.Square, accum_out=ss.t[:, j:j + 1]), r=[xb], w=[junk, ss])
                    OP("act", lambda E, j=j: E.activation(ss.t[:, j:j + 1], ss.t[:, j:j + 1], AF.Sqrt, bias=epst.t[:, 0:1], scale=1.0 / D),
                       r=[ss, epst], w=[ss])
                    OP("dve", lambda E, j=j: E.reciprocal(rstd.t[:, j:j + 1], ss.t[:, j:j + 1]), r=[ss], w=[rstd])
                    OP("pool", lambda E, xb=xb, j=j: E.tensor_scalar(xb.t[:], xb.t[:], rstd.t[:, j:j + 1], None, ALU.mult), r=[xb, rstd], w=[xb])
                for c in range(KC):
                    g = Grot.next()
                    for j in range(2):
                        TR(g, g.t[:, j * 128:(j + 1) * 128], xs_[j].t[:, c * 128:(c + 1) * 128], ident, [xs_[j]])
                    OP("act", lambda E, g=g, c=c, b=b: E.activation(hT.t[:, c, :], g.t[:, 0:T], AF.Identity,
                                                               bias=sh1.t[:, c * NSEQ + b:c * NSEQ + b + 1],
                                                               scale=gs1.t[:, c * NSEQ + b:c * NSEQ + b + 1]), r=[g, sh1, gs1], w=[hT])

                def proj_fm(w, cc):
                    g = Grot.next()
                    MM(g, g.t[:, 0:T], [(w.t[:, kc, cc * 128:(cc + 1) * 128], hT.t[:, kc, :]) for kc in range(KC)], [w, hT])
                    return g

                if STOP == 11:
                    return finish()
                for q in range(2):
                    w_sb = wload(win_r, B_win, C_SB + q * 512)
                    w_sx = wload(win_r, B_win, C_SX + q * 512)
                    w_sc = wload(win_r, B_win, C_SC + q * 512)
                    for cc in range(4):
                        c = q * 4 + cc
                        g_sb = proj_fm(w_sb, cc)
                        g_sx = proj_fm(w_sx, cc)
                        ta = tmpA.next()
                        OP("act", lambda E, ta=ta, g=g_sx: E.activation(ta.t[:], g.t[:, 0:T], AF.Copy), r=[g_sx], w=[ta])
                        u = utmp.next()
                        OP("pool", lambda E, u=u, c=c: E.tensor_copy(u.t[:, 0:2], uhalo.t[:, c, :]), r=[uhalo], w=[u])
                        OP("dve", lambda E, u=u, g=g_sb, ta=ta: E.tensor_tensor(u.t[:, 2:T + 2], g.t[:, 0:T], ta.t[:], ALU.mult), r=[g_sb, ta], w=[u])
                        OP("pool", lambda E, u=u, c=c: E.tensor_copy(uhalo.t[:, c, :], u.t[:, T:T + 2]), r=[u], w=[uhalo])
                        g_sc = proj_fm(w_sc, cc)
                        y = tmpY.next()
                        OP("pool", lambda E, y=y, u=u, c=c: E.tensor_scalar(y.t[:], u.t[:, 2:T + 2], wsc_fm.t[:, c * 3 + 2:c * 3 + 3], None, ALU.mult),
                           r=[u, wsc_fm], w=[y])
                        OP("dve", lambda E, y=y, u=u, c=c: E.scalar_tensor_tensor(y.t[:], u.t[:, 1:T + 1], wsc_fm.t[:, c * 3 + 1:c * 3 + 2], y.t[:], ALU.mult, ALU.add),
                           r=[u, wsc_fm, y], w=[y])
                        OP("dve", lambda E, y=y, u=u, c=c: E.scalar_tensor_tensor(y.t[:], u.t[:, 0:T], wsc_fm.t[:, c * 3:c * 3 + 1], y.t[:], ALU.mult, ALU.add),
                           r=[u, wsc_fm, y], w=[y])
                        OP("dve", lambda E, y=y, g=g_sc, c=c: E.tensor_tensor(vT.t[:, c, :], g.t[:, 0:T], y.t[:], ALU.mult), r=[g_sc, y], w=[vT])

                if STOP == 12:
                    return finish()
                for zq in range(4):
                    w_z = wload(win_r, B_win, C_Z + zq * 512)
                    for j in range(2):
                        g = Grot.next()
                        MM(g, g.t[:], [(hT.t[:, kc, j * 128:(j + 1) * 128], w_z.t[:, kc, :]) for kc in range(KC)], [w_z, hT])
                        OP("act", lambda E, g=g, j=j, zq=zq: E.activation(zs.t[:, j, zq * 512:(zq + 1) * 512], g.t[:], AF.Silu), r=[g], w=[zs])
                for j in range(2):
                    g = Grot.next()
                    MM(g, g.t[:, 0:NH], [(hT.t[:, kc, j * 128:(j + 1) * 128], wdt_sb.t[:, kc, :]) for kc in range(KC)], [wdt_sb, hT])
                    OP("dve", lambda E, g=g: E.tensor_tensor(dtmp.t[:], g.t[:, 0:NH], dtb_bc.t[:], ALU.add), r=[g, dtb_bc], w=[dtmp])
                    OP("act", lambda E: E.activation(dtmp.t[:], dtmp.t[:], AF.Exp), r=[dtmp], w=[dtmp])
                    OP("act", lambda E, j=j: E.activation(dtt.t[:, j, :], dtmp.t[:], AF.Ln, bias=1.0, scale=1.0), r=[dtmp], w=[dtt])
                    OP("dve", lambda E, j=j: E.tensor_tensor(dat.t[:, j, :], dtt.t[:, j, :], a_bc.t[:], ALU.mult), r=[dtt, a_bc], w=[dat])
                for xq in range(6):
                    w_x = wload(win_r, B_win, C_XBC + xq * 512)
                    for cc in range(4):
                        cx = xq * 4 + cc
                        g = proj_fm(w_x, cc)
                        xw = xraw.next()
                        OP("pool", lambda E, xw=xw, cx=cx: E.tensor_copy(xw.t[:, 0:3], xhalo.t[:, cx, :]), r=[xhalo], w=[xw])
                        OP("act", lambda E, xw=xw, g=g: E.activation(xw.t[:, 3:T + 3], g.t[:, 0:T], AF.Copy), r=[g], w=[xw])
                        OP("pool", lambda E, xw=xw, cx=cx: E.tensor_copy(xhalo.t[:, cx, :], xw.t[:, T:T + 3]), r=[xw], w=[xhalo])
                        y = tmpY.next()
                        OP("dve", lambda E, y=y, xw=xw, cx=cx: E.tensor_scalar(y.t[:], xw.t[:, 3:T + 3], wxc_fm.t[:, cx * 4 + 3:cx * 4 + 4], None, ALU.mult),
                           r=[xw, wxc_fm], w=[y])
                        for k in (2, 1, 0):
                            eng = "dve"
                            OP(eng, lambda E, y=y, xw=xw, cx=cx, k=k: E.scalar_tensor_tensor(y.t[:], xw.t[:, k:k + T], wxc_fm.t[:, cx * 4 + k:cx * 4 + k + 1], y.t[:],
                                                                                           ALU.mult, ALU.add), r=[xw, wxc_fm, y], w=[y])
                        if cx < 16:
                            dstb, dst = xsT, xsT.t[:, cx, :]
                        elif cx < 20:
                            dstb, dst = BT, BT.t[:, cx - 16, :]
                        else:
                            dstb, dst = CT, CT.t[:, cx - 20, :]
                        OP("act", lambda E, y=y, dst=dst, cx=cx: E.activation(dst, y.t[:], AF.Silu, bias=bxc_fm.t[:, cx:cx + 1], scale=1.0),
                           r=[y, bxc_fm], w=[dstb])

                if STOP == 13:
                    return finish()
                for jpos, j in enumerate(_JLIST):
                    SD = (STOP - 100 * jpos) if 141 + 100 * jpos <= STOP <= 148 + 100 * jpos else -1
                    js = slice(j * 128, (j + 1) * 128)
                    g0 = G[0]
                    MM(g0, g0.t[:, 0:NH], [(triu.t[:], dat.t[:, j, :])], [triu, dat])
                    MM(g0, g0.t[:, NH:2 * NH], [(ones.t[:], dat.t[:, j, :])], [ones, dat])
                    OP("dve", lambda E: E.tensor_copy(acum.t[:], g0.t[:, 0:NH]), r=[g0], w=[acum])
                    OP("dve", lambda E: E.tensor_tensor(dend.t[:], g0.t[:, NH:2 * NH], acum.t[:], ALU.subtract), r=[g0, acum], w=[dend])
                    OP("act", lambda E: E.activation(dend.t[:], dend.t[:], AF.Exp), r=[dend], w=[dend])
                    OP("act", lambda E: E.activation(dchunk.t[:], g0.t[:, NH:2 * NH], AF.Exp), r=[g0], w=[dchunk])
                    OP("act", lambda E: E.activation(eacum.t[:], acum.t[:], AF.Exp), r=[acum], w=[eacum])
                    if STOP == 240 and jpos == 1:
                        return finish()
                    for cx in range(16):
                        TR(ptb, ptb.t[:, cx * 128:(cx + 1) * 128], xsT.t[:, cx, js], identb, [xsT])
                    if STOP == 2401 and jpos == 1:
                        return finish()
                    for g_ in range(NG):
                        TR(ptB, ptB.t[:, g_ * 128:(g_ + 1) * 128], BT.t[:, g_, js], identb, [BT])
                    if STOP == 2402 and jpos == 1:
                        return finish()
                    OP("act", lambda E: E.activation(xs_tok.t[:], ptb.t[:], AF.Copy), r=[ptb], w=[xs_tok])
                    if STOP == 2403 and jpos == 1:
                        return finish()
                    OP("dve", lambda E, j=j: E.tensor_tensor(xdt.t[:].rearrange("p (h q) -> p h q", q=HP), xs_tok.t[:].rearrange("p (h q) -> p h q", q=HP),
                                                             dtt.t[:, j, :].unsqueeze(2).to_broadcast([128, NH, HP]), ALU.mult), r=[xs_tok, dtt], w=[xdt])
                    if STOP == 2404 and jpos == 1:
                        return finish()
                    OP("pool", lambda E: E.tensor_tensor(xdte.t[:].rearrange("p (h q) -> p h q", q=HP), xdt.t[:].rearrange("p (h q) -> p h q", q=HP),
                                                         dend.t[:].unsqueeze(2).to_broadcast([128, NH, HP]), ALU.mult), r=[xdt, dend], w=[xdte])
                    OP("act", lambda E: E.activation(B_tok.t[:], ptB.t[:, 0:NG * NS], AF.Copy), r=[ptB], w=[B_tok])
                    if SD == 141:
                        return finish()
                    g1 = G[1]
                    for g_ in range(NG):
                        MM(g1, g1.t[:, g_ * 128:(g_ + 1) * 128], [(BT.t[:, g_, js], CT.t[:, g_, js])], [BT, CT])
                    OP("dve", lambda E: E.tensor_tensor(CBm.t[:], g1.t[:].rearrange("p (g i) -> p g i", i=128),
                                                        triu.t[:].unsqueeze(1).to_broadcast([128, NG, 128]), ALU.mult), r=[g1, triu], w=[CBm])
                    if SD == 142:
                        return finish()
                    for g_ in range(NG):
                        gsl = slice(g_ * 512, (g_ + 1) * 512)
                        for half in range(2):
                            ga_ = G[2 + half]
                            for hh in range(4):
                                h = g_ * 8 + half * 4 + hh
                                MM(ga_, ga_.t[:, hh * 128:(hh + 1) * 128], [(dat.t[:, j, h:h + 1].to_broadcast([128, 128]), triu.t[:])], [dat, triu])
                            sg_ = seg.next()
                            h0 = g_ * 8 + half * 4
                            OP("dve", lambda E, ga_=ga_, sg_=sg_, h0=h0: E.tensor_tensor(sg_.t[:], ga_.t[:].rearrange("p (h i) -> p h i", i=128),
                                                                                       acum.t[:, h0:h0 + 4].unsqueeze(2).to_broadcast([128, 4, 128]), ALU.subtract),
                               r=[ga_, acum], w=[sg_])
                            OP("pool", lambda E, sg_=sg_: E.tensor_single_scalar(sg_.t[:], sg_.t[:], 0.0, ALU.min), r=[sg_], w=[sg_])
                            OP("act", lambda E, sg_=sg_: E.activation(sg_.t[:], sg_.t[:], AF.Exp), r=[sg_], w=[sg_])
                            OP("dve", lambda E, sg_=sg_, half=half, g_=g_: E.tensor_tensor(Sb.t[:, half * 4:half * 4 + 4, :], sg_.t[:],
                                                                                         CBm.t[:, g_:g_ + 1, :].to_broadcast([128, 4, 128]), ALU.mult),
                               r=[sg_, CBm], w=[Sb])
                        if SD == 143:
                            return finish()
                        g4 = G[4]
                        for h8 in range(8):
                            h = g_ * 8 + h8
                            MM(g4, g4.t[:, h8 * HP:(h8 + 1) * HP], [(Sb.t[:, h8, :], xdt.t[:, h * HP:(h + 1) * HP])], [Sb, xdt])
                        MM(g0, g0.t[:], [(CT.t[:, g_, js], stbf.t[:, gsl])], [CT, stbf])
                        y_ = yg.next()
                        OP("dve", lambda E, y_=y_, g_=g_: E.tensor_tensor(y_.t[:].rearrange("p (h q) -> p h q", q=HP), g0.t[:].rearrange("p (h q) -> p h q", q=HP),
                                                                         eacum.t[:, g_ * 8:(g_ + 1) * 8].unsqueeze(2).to_broadcast([128, 8, HP]), ALU.mult),
                           r=[g0, eacum], w=[y_])
                        OP("dve", lambda E, y_=y_: E.tensor_tensor(y_.t[:], g4.t[:], y_.t[:], ALU.add), r=[g4, y_], w=[y_])
                        t2 = yt2.next()
                        OP("pool", lambda E, t2=t2, g_=g_, gsl=gsl: E.tensor_tensor(t2.t[:].rearrange("p (h q) -> p h q", q=HP), xs_tok.t[:, gsl].rearrange("p (h q) -> p h q", q=HP),
                                                                                  dsk_bc.t[:, g_ * 8:(g_ + 1) * 8].unsqueeze(2).to_broadcast([128, 8, HP]), ALU.mult),
                           r=[xs_tok, dsk_bc], w=[t2])
                        OP("pool", lambda E, t2=t2, y_=y_: E.tensor_tensor(y_.t[:], y_.t[:], t2.t[:], ALU.add), r=[t2, y_], w=[y_])
                        if SD == 144:
                            return finish()
                        OP("dve", lambda E, y_=y_, j=j, gsl=gsl: E.tensor_tensor(y_.t[:], y_.t[:], zs.t[:, j, gsl], ALU.mult), r=[y_, zs], w=[y_])
                        OP("act", lambda E, y_=y_: E.activation(junk.t[:, 0:512], y_.t[:], AF.Square, accum_out=ssg.t[:, 0:1]), r=[y_], w=[junk, ssg])
                        OP("act", lambda E: E.activation(ssg.t[:, 0:1], ssg.t[:, 0:1], AF.Sqrt, bias=epst.t[:, 0:1], scale=1.0 / 512), r=[ssg, epst], w=[ssg])
                        OP("dve", lambda E: E.reciprocal(ssg.t[:, 1:2], ssg.t[:, 0:1]), r=[ssg], w=[ssg])
                        yn_ = yn.next()
                        OP("dve", lambda E, y_=y_, yn_=yn_, gsl=gsl: E.scalar_tensor_tensor(yn_.t[:], y_.t[:], ssg.t[:, 1:2], gssm_bc.t[:, gsl], ALU.mult, ALU.mult),
                           r=[y_, ssg, gssm_bc], w=[yn_])
                        for q in range(4):
                            TR(ptB, ptB.t[:, 512 + q * 128:512 + (q + 1) * 128], yn_.t[:, q * 128:(q + 1) * 128], identb, [yn_])
                        OP("act", lambda E, g_=g_, js=js: E.activation(ynT.t[:, g_ * 4:(g_ + 1) * 4, js], ptB.t[:, 512:1024].rearrange("p (q t) -> p q t", t=128), AF.Copy),
                           r=[ptB], w=[ynT])
                        if SD == 145:
                            return finish()
                        MM(g1, g1.t[:], [(B_tok.t[:, g_ * 128:(g_ + 1) * 128], xdte.t[:, gsl])], [B_tok, xdte])
                        OP("pool", lambda E, g_=g_, gsl=gsl: E.tensor_tensor(st32.t[:, gsl].rearrange("p (h q) -> p h q", q=HP), st32.t[:, gsl].rearrange("p (h q) -> p h q", q=HP),
                                                                           dchunk.t[:, g_ * 8:(g_ + 1) * 8].unsqueeze(2).to_broadcast([128, 8, HP]), ALU.mult),
                           r=[st32, dchunk], w=[st32])
                        OP("dve", lambda E, gsl=gsl: E.tensor_tensor(st32.t[:, gsl], g1.t[:], st32.t[:, gsl], ALU.add), r=[g1, st32], w=[st32])
                        OP("act", lambda E, gsl=gsl: E.activation(stbf.t[:, gsl], st32.t[:, gsl], AF.Copy), r=[st32], w=[stbf])
                        if SD == 146:
                            return finish()
                        if SD == 147 and g_ == 1:
                            return finish()
                    if SD == 148:
                        return finish()

                if STOP == 14:
                    return finish()
                for dq in range(2):
                    w_a = wload(wsco_r, B_wsco, dq * 512)
                    w_ga = wload(win_r, B_win, C_GA + dq * 512)
                    for dcc in range(4):
                        g_y = Grot.next()
                        MM(g_y, g_y.t[:, 0:T], [(w_a.t[:, kc, dcc * 128:(dcc + 1) * 128], vT.t[:, kc, :]) for kc in range(KC)], [w_a, vT])
                        g_g = proj_fm(w_ga, dcc)
                        ta = tmpA.next()
                        OP("act", lambda E, ta=ta, g=g_g: E.activation(ta.t[:], g.t[:, 0:T], AF.Sigmoid), r=[g_g], w=[ta])
                        OP("dve", lambda E, ta=ta, g=g_y, dcc=dcc: E.tensor_tensor(mpart.t[:, dcc, :], g.t[:, 0:T], ta.t[:], ALU.mult), r=[g_y, ta], w=[mpart])
                    w_b0 = wload(wsso_r, B_wsso, dq * 512, 0)
                    w_b1 = wload(wsso_r, B_wsso, dq * 512, 8)
                    w_gb = wload(win_r, B_win, C_GB + dq * 512)
                    for dcc in range(4):
                        g_y = Grot.next()
                        MM(g_y, g_y.t[:, 0:T], [(w_b0.t[:, kc, dcc * 128:(dcc + 1) * 128], ynT.t[:, kc, :]) for kc in range(KC)] +
                           [(w_b1.t[:, kc, dcc * 128:(dcc + 1) * 128], ynT.t[:, 8 + kc, :]) for kc in range(KC)], [w_b0, w_b1, ynT])
                        g_g = proj_fm(w_gb, dcc)
                        ta = tmpA.next()
                        OP("act", lambda E, ta=ta, g=g_g: E.activation(ta.t[:], g.t[:, 0:T], AF.Sigmoid), r=[g_g], w=[ta])
                        OP("dve", lambda E, ta=ta, g=g_y: E.tensor_tensor(ta.t[:], g.t[:, 0:T], ta.t[:], ALU.mult), r=[g_y, ta], w=[ta])
                        OP("pool", lambda E, ta=ta, dq=dq, dcc=dcc: E.tensor_tensor(mT.t[:, dq * 4 + dcc, :], ta.t[:], mpart.t[:, dcc, :], ALU.add),
                           r=[ta, mpart], w=[mT])

                if STOP == 15:
                    return finish()
                x1b = [xr.next(), xr.next()]
                for j in range(2):
                    LOAD("sp", x1b[j], x1b[j].t[:], x_d[r0 + j * 128:r0 + (j + 1) * 128, :], reads=[])
                for dh in range(2):
                    w_o_ = wload(wo_r, B_wo, dh * 512)
                    dsl = slice(dh * 512, (dh + 1) * 512)
                    for j in range(2):
                        g = Grot.next()
                        MM(g, g.t[:], [(mT.t[:, kc, j * 128:(j + 1) * 128], w_o_.t[:, kc, :]) for kc in range(KC)], [w_o_, mT])
                        t2 = yt2.next()
                        OP("dve", lambda E, g=g, t2=t2, dsl=dsl: E.tensor_tensor(t2.t[:], g.t[:], bct.t[:, GT1 + dsl.start:GT1 + dsl.stop], ALU.mult), r=[g, bct], w=[t2])
                        OP("pool", lambda E, t2=t2, xb_=x1b[j], dsl=dsl: E.tensor_tensor(xb_.t[:, dsl], xb_.t[:, dsl], t2.t[:], ALU.add), r=[t2, x1b[j]], w=[x1b[j]])
                for j in range(2):
                    tt = (r0 // 128) + j
                    xb = x1b[j]
                    S.dma("sp", lambda E, xb=xb, tt=tt: E.dma_start(out=x1_h[tt * 128:(tt + 1) * 128, :], in_=xb.t[:]), xb, reads=[xb], writes=[B_x1])
                    OP("act", lambda E, xb=xb: E.activation(junk.t[:], xb.t[:], AF.Square, accum_out=ss.t[:, 2:3]), r=[xb], w=[junk, ss])
                    OP("act", lambda E: E.activation(ss.t[:, 2:3], ss.t[:, 2:3], AF.Sqrt, bias=epst.t[:, 0:1], scale=1.0 / D), r=[ss, epst], w=[ss])
                    OP("dve", lambda E: E.reciprocal(rstd.t[:, 2:3], ss.t[:, 2:3]), r=[ss], w=[rstd])
                    OP("dve", lambda E, xb=xb: E.scalar_tensor_tensor(h2.t[:], xb.t[:], rstd.t[:, 2:3], bct.t[:, GS2:GS2 + D], ALU.mult, ALU.mult),
                       r=[xb, rstd, bct], w=[h2])
                    OP("pool", lambda E: E.tensor_tensor(h2.t[:], h2.t[:], bct.t[:, SH2:SH2 + D], ALU.add), r=[h2, bct], w=[h2])
                    hb = h2b.next()
                    OP("act", lambda E, hb=hb: E.activation(hb.t[:], h2.t[:], AF.Copy), r=[h2], w=[hb])
                    S.dma("sp", lambda E, hb=hb, tt=tt: E.dma_start(out=h2_h[tt * 128:(tt + 1) * 128, :], in_=hb.t[:]), hb, reads=[hb], writes=[B_h2])
                    for half in range(2):
                        g = Grot.next()
                        for c4 in range(4):
                            c = half * 4 + c4
                            TR(g, g.t[:, c4 * 128:(c4 + 1) * 128], h2.t[:, c * 128:(c + 1) * 128], ident, [h2])
                        OP("act", lambda E, g=g, half=half: E.activation(h2T.t[:, half * 4:half * 4 + 4, :], g.t[:].rearrange("p (c t) -> p c t", t=128), AF.Copy),
                           r=[g], w=[h2T])
                    g = Grot.next()
                    MM(g, g.t[:, 0:NE], [(h2T.t[:, kc, :], wr_sb.t[:, kc, :]) for kc in range(KC)], [h2T, wr_sb])
                    OP("dve", lambda E, g=g, tt=tt: E.tensor_tensor(logits.t[:, tt * NE:(tt + 1) * NE], g.t[:, 0:NE], br_bc.t[:], ALU.add), r=[g, br_bc], w=[logits])

        issue_casts(len(cast_jobs))
        issue_zero(len(zero_jobs))
        if STOP == 1:
            return finish()
        S.barrier()
        p1.close()
        open_stacks.remove(p1)

        p2a = ExitStack()
        open_stacks.append(p2a)
        top8 = S.sbuf("top8", [128, 8], F32, p2a)
        topi = S.sbuf("topi", [128, 8], U32, p2a)
        idxf = S.sbuf("idxf", [128, NTT * TOPK], F32, p2a)
        negm = S.sbuf("negm", [128, 1], F32, p2a)
        esum = S.sbuf("esum", [128, 2], F32, p2a)
        Mb = S.sbuf("Mb", [128, NE], BF16, p2a)
        rank = S.sbuf("rank", [128, NTT * NE], F32, p2a)
        runc = S.sbuf("runc", [128, NE], F32, p2a)
        ca = S.sbuf("ca", [128, NE], F32, p2a)
        cb = S.sbuf("cb", [128, NE], F32, p2a)
        ci = S.sbuf("ci", [128, NE], I32, p2a)
        padded = S.sbuf("padded", [128, NE], F32, p2a)
        pstart = S.sbuf("pstart", [128, NE], F32, p2a)
        base = S.sbuf("base", [128, NE], F32, p2a)
        tmpe = S.sbuf("tmpe", [128, NE], F32, p2a)
        destf = S.sbuf("destf", [128, NTT * TOPK], F32, p2a)
        bstart = S.sbuf("bstart", [128, NBLK], F32, p2a)
        be = S.sbuf("be", [128, NBLK], F32, p2a)
        pio = S.sbuf("pio", [128, KC], F32, p2a)
        wrow_f = S.sbuf("wrow_f", [128, NBLK, KC], F32, p2a)
        brow_f = S.sbuf("brow_f", [128, NBLK], F32, p2a)

        OP("dve", lambda E: E.memset(runc.t[:], 0.0), w=[runc])
        for tt in range(NTT):
            lg = logits.t[:, tt * NE:(tt + 1) * NE]
            OP("dve", lambda E, lg=lg: E.max(top8.t[:], lg), r=[logits], w=[top8])
            OP("dve", lambda E, lg=lg: E.max_index(topi.t[:], top8.t[:], lg), r=[logits, top8], w=[topi])
            OP("dve", lambda E, tt=tt: E.tensor_copy(idxf.t[:, tt * 4:tt * 4 + 4], topi.t[:, 0:4]), r=[topi], w=[idxf])
            OP("dve", lambda E: E.tensor_single_scalar(negm.t[:], top8.t[:, 0:1], -1.0, ALU.mult), r=[top8], w=[negm])
            OP("act", lambda E, tt=tt: E.activation(wts.t[:, tt * 4:tt * 4 + 4], top8.t[:, 0:4], AF.Exp, bias=negm.t[:, 0:1], scale=1.0,
                                                   accum_out=esum.t[:, 0:1]), r=[top8, negm], w=[wts, esum])
            OP("dve", lambda E: E.reciprocal(esum.t[:, 1:2], esum.t[:, 0:1]), r=[esum], w=[esum])
            OP("dve", lambda E, tt=tt: E.tensor_scalar(wts.t[:, tt * 4:tt * 4 + 4], wts.t[:, tt * 4:tt * 4 + 4], esum.t[:, 1:2], None, ALU.mult),
               r=[wts, esum], w=[wts])
            OP("dve", lambda E, tt=tt: E.tensor_scalar(Mb.t[:], iota32.t[:], idxf.t[:, tt * 4:tt * 4 + 1], None, ALU.is_equal), r=[iota32, idxf], w=[Mb])
            for k in range(1, 4):
                OP("dve", lambda E, tt=tt, k=k: E.scalar_tensor_tensor(Mb.t[:], iota32.t[:], idxf.t[:, tt * 4 + k:tt * 4 + k + 1], Mb.t[:], ALU.is_equal, ALU.add),
                   r=[iota32, idxf, Mb], w=[Mb])
            g = Grot.next()
            MM(g, g.t[:, 0:NE], [(tristb.t[:], Mb.t[:])], [tristb, Mb])
            MM(g, g.t[:, NE:2 * NE], [(onesb.t[:], Mb.t[:])], [onesb, Mb])
            OP("dve", lambda E, g=g, tt=tt: E.tensor_tensor(rank.t[:, tt * NE:(tt + 1) * NE], g.t[:, 0:NE], runc.t[:], ALU.add), r=[g, runc], w=[rank])
            OP("dve", lambda E, g=g: E.tensor_tensor(runc.t[:], g.t[:, NE:2 * NE], runc.t[:], ALU.add), r=[g, runc], w=[runc])
        sh = BLK.bit_length() - 1
        OP("dve", lambda E: E.tensor_copy(ci.t[:], runc.t[:]), r=[runc], w=[ci])
        OP("dve", lambda E: E.tensor_single_scalar(ci.t[:], ci.t[:], BLK - 1, ALU.add), r=[ci], w=[ci])
        OP("dve", lambda E: E.tensor_single_scalar(ci.t[:], ci.t[:], sh, ALU.arith_shift_right), r=[ci], w=[ci])
        OP("dve", lambda E: E.tensor_single_scalar(ci.t[:], ci.t[:], sh, ALU.logical_shift_left), r=[ci], w=[ci])
        OP("dve", lambda E: E.tensor_copy(padded.t[:], ci.t[:]), r=[ci], w=[padded])
        OP("dve", lambda E: E.tensor_copy(ca.t[:], padded.t[:]), r=[padded], w=[ca])
        src, dst = ca, cb
        s_ = 1
        while s_ < NE:
            OP("dve", lambda E, src=src, dst=dst, s_=s_: E.tensor_copy(dst.t[:, 0:s_], src.t[:, 0:s_]), r=[src], w=[dst])
            OP("dve", lambda E, src=src, dst=dst, s_=s_: E.tensor_tensor(dst.t[:, s_:NE], src.t[:, s_:NE], src.t[:, 0:NE - s_], ALU.add), r=[src], w=[dst])
            src, dst = dst, src
            s_ *= 2
        pend = src
        OP("dve", lambda E: E.tensor_tensor(pstart.t[:], pend.t[:], padded.t[:], ALU.subtract), r=[pend, padded], w=[pstart])
        for tt in range(NTT):
            OP("dve", lambda E, tt=tt: E.tensor_tensor(base.t[:], rank.t[:, tt * NE:(tt + 1) * NE], pstart.t[:], ALU.add), r=[rank, pstart], w=[base])
            for k in range(4):
                col = tt * 4 + k
                OP("dve", lambda E, col=col: E.scalar_tensor_tensor(tmpe.t[:], iota32.t[:], idxf.t[:, col:col + 1], base.t[:], ALU.is_equal, ALU.mult),
                   r=[iota32, idxf, base], w=[tmpe])
                OP("dve", lambda E, col=col: E.reduce_sum(destf.t[:, col:col + 1], tmpe.t[:], axis=AX.X), r=[tmpe], w=[destf])
        OP("dve", lambda E: E.tensor_copy(dest_i.t[:], destf.t[:]), r=[destf], w=[dest_i])
        OP("pool", lambda E: E.iota(bstart.t[:], pattern=[[BLK, NBLK]], base=0, channel_multiplier=0, allow_small_or_imprecise_dtypes=True), w=[bstart])
        OP("pool", lambda E: E.iota(pio.t[:], pattern=[[128, KC]], base=0, channel_multiplier=1, allow_small_or_imprecise_dtypes=True), w=[pio])
        OP("dve", lambda E: E.memset(be.t[:], 0.0), w=[be])
        for e in range(NE):
            OP("dve", lambda E, e=e: E.scalar_tensor_tensor(be.t[:], bstart.t[:], pend.t[:, e:e + 1], be.t[:], ALU.is_ge, ALU.add), r=[bstart, pend, be], w=[be])
        OP("dve", lambda E: E.tensor_single_scalar(be.t[:], be.t[:], float(NE - 1), ALU.min), r=[be], w=[be])
        for kc in range(KC):
            OP("dve", lambda E, kc=kc: E.tensor_scalar(wrow_f.t[:, :, kc], be.t[:], float(D), pio.t[:, kc:kc + 1], ALU.mult, ALU.add), r=[be, pio], w=[wrow_f])
        OP("dve", lambda E: E.tensor_copy(wrow.t[:], wrow_f.t[:].rearrange("p b k -> p (b k)")), r=[wrow_f], w=[wrow])
        OP("dve", lambda E: E.tensor_scalar(brow_f.t[:], be.t[:], 128.0, pio.t[:, 0:1], ALU.mult, ALU.add), r=[be, pio], w=[brow_f])
        OP("dve", lambda E: E.tensor_copy(brow.t[:], brow_f.t[:]), r=[brow_f], w=[brow])
        OP("dve", lambda E: E.tensor_copy(erow.t[:], be.t[:]), r=[be], w=[erow])

        if STOP == 2:
            return finish()
        S.barrier()
        p2a.close()
        plog.close()
        open_stacks.remove(p2a)
        open_stacks.remove(plog)
        p3 = ExitStack()
        open_stacks.append(p3)
        hrows = Rot([S.sbuf(f"hrows{i}", [128, D], BF16, p3) for i in range(6)])
        for tt in range(NTT):
            hb = hrows.next()
            LOAD("sp", hb, hb.t[:], h2_h[tt * 128:(tt + 1) * 128, :], reads=[B_h2])
            for k in range(4):
                col = tt * 4 + k
                S.dma("pool", lambda E, hb=hb, col=col: E.indirect_dma_start(
                    out=xs_h, out_offset=bass.IndirectOffsetOnAxis(ap=dest_i.t[:, col:col + 1], axis=0),
                    in_=hb.t[:, :], in_offset=None), B_xs, reads=[hb, dest_i, B_xz], writes=[])
        B_xs.w = ("d", B_xs)

        if STOP == 3:
            return finish()
        wunit = Rot([S.sbuf(f"wunit{i}", [128, KC, D], BF16, p3) for i in range(6)])
        bgu_sb = Rot([S.sbuf(f"bgu_sb{i}", [128, 16], F32, p3) for i in range(2)])
        bd_sb = Rot([S.sbuf(f"bd_sb{i}", [128, D], F32, p3) for i in range(2)])
        xrows = Rot([S.sbuf(f"xrows{i}", [128, 4, D], BF16, p3) for i in range(2)])
        gcl = Rot([S.sbuf(f"gcl{i}", [128, BLK], F32, p3) for i in range(2)])
        sgm = Rot([S.sbuf(f"sgm{i}", [128, BLK], F32, p3) for i in range(2)])
        upc = Rot([S.sbuf(f"upc{i}", [128, BLK], F32, p3) for i in range(2)])
        ysb = Rot([S.sbuf(f"ysb{i}", [128, D], F32, p3) for i in range(3)])
        xTs = Rot([S.sbuf(f"xT{i}", [128, KC, BLK], BF16, p3) for i in range(2)])
        actTs = Rot([S.sbuf(f"actT{i}", [128, KC, BLK], BF16, p3) for i in range(2)])

        def blk_loads(blk):
            wgg = wunit.next()
            wgup = wunit.next()
            wdn = wunit.next()
            bg = bgu_sb.next()
            bd = bd_sb.next()
            for (wdst, wsrc, bb) in ((wgg, wg_bf, B_wgu), (wgup, wu_bf, B_wgu), (wdn, wd_bf, B_wd)):
                S.dma("pool", lambda E, wdst=wdst, wsrc=wsrc, blk=blk: E.indirect_dma_start(
                    out=wdst.t[:].rearrange("p k n -> p (k n)"), out_offset=None, in_=wsrc,
                    in_offset=bass.IndirectOffsetOnAxis(ap=brow.t[:, blk:blk + 1], axis=0)), wdst, reads=[bb, brow], writes=[wdst])
            S.dma("pool", lambda E, bg=bg, blk=blk: E.indirect_dma_start(
                out=bg.t[:, :], out_offset=None, in_=bgu_l_d,
                in_offset=bass.IndirectOffsetOnAxis(ap=brow.t[:, blk:blk + 1], axis=0)), bg, reads=[brow], writes=[bg])
            S.dma("pool", lambda E, bd=bd, blk=blk: E.indirect_dma_start(
                out=bd.t[:, :], out_offset=None, in_=bd_d,
                in_offset=bass.IndirectOffsetOnAxis(ap=erow.t[:, blk:blk + 1], axis=0)), bd, reads=[erow], writes=[bd])
            xw = xrows.next()
            LOAD("sp", xw, xw.t[:], xs_h[blk * BLK:(blk + 1) * BLK, :].rearrange("(r p) d -> p r d", p=128), reads=[B_xs])
            return dict(wgg=wgg, wgup=wgup, wdn=wdn, bg=bg, bd=bd, xw=xw)

        def blk_transposes(L):
            xw = L["xw"]
            xT = xTs.next()
            for half in range(2):
                for c4 in range(4):
                    kc = half * 4 + c4
                    for r in range(4):
                        TR(ptb, ptb.t[:, c4 * 512 + r * 128:c4 * 512 + (r + 1) * 128], xw.t[:, r, kc * 128:(kc + 1) * 128], identb, [xw])
                OP("act", lambda E, half=half, xT=xT: E.activation(xT.t[:, half * 4:half * 4 + 4, :], ptb.t[:].rearrange("p (c t) -> p c t", t=BLK), AF.Copy),
                   r=[ptb], w=[xT])
            L["xT"] = xT

        def blk_gu(L):
            wgg, wgup, bg, xT = L["wgg"], L["wgup"], L["bg"], L["xT"]
            actT = actTs.next()
            L["actT"] = actT
            for f in range(KC):
                g_g = Grot.next()
                MM(g_g, g_g.t[:], [(wgg.t[:, kc, f * 128:(f + 1) * 128], xT.t[:, kc, :]) for kc in range(KC)], [wgg, xT])
                g_u = Grot.next()
                MM(g_u, g_u.t[:], [(wgup.t[:, kc, f * 128:(f + 1) * 128], xT.t[:, kc, :]) for kc in range(KC)], [wgup, xT])
                gc_, sg_, up_ = gcl.next(), sgm.next(), upc.next()
                OP("dve", lambda E, g=g_g, gc_=gc_, f=f, bg=bg: E.tensor_scalar(gc_.t[:], g.t[:], bg.t[:, f:f + 1], 7.0, ALU.add, ALU.min), r=[g_g, bg], w=[gc_])
                OP("act", lambda E, gc_=gc_, sg_=sg_: E.activation(sg_.t[:], gc_.t[:], AF.Sigmoid, scale=1.702), r=[gc_], w=[sg_])
                OP("dve", lambda E, g=g_u, up_=up_, f=f, bg=bg: E.tensor_scalar(up_.t[:], g.t[:], bg.t[:, 8 + f:9 + f], 7.0, ALU.add, ALU.min), r=[g_u, bg], w=[up_])
                OP("pool", lambda E, up_=up_: E.tensor_scalar(up_.t[:], up_.t[:], -7.0, 1.0, ALU.max, ALU.add), r=[up_], w=[up_])
                OP("pool", lambda E, gc_=gc_, sg_=sg_: E.tensor_tensor(gc_.t[:], gc_.t[:], sg_.t[:], ALU.mult), r=[gc_, sg_], w=[gc_])
                OP("dve", lambda E, gc_=gc_, up_=up_, f=f, actT=actT: E.tensor_tensor(actT.t[:, f, :], gc_.t[:], up_.t[:], ALU.mult), r=[gc_, up_], w=[actT])

        def blk_down(L, blk):
            wdn, bd, actT = L["wdn"], L["bd"], L["actT"]
            for r in range(4):
                yb = ysb.next()
                for dh in range(2):
                    g = Grot.next()
                    MM(g, g.t[:], [(actT.t[:, fc, r * 128:(r + 1) * 128], wdn.t[:, fc, dh * 512:(dh + 1) * 512]) for fc in range(KC)], [actT, wdn])
                    OP("dve", lambda E, g=g, yb=yb, dh=dh, bd=bd: E.tensor_tensor(yb.t[:, dh * 512:(dh + 1) * 512], g.t[:], bd.t[:, dh * 512:(dh + 1) * 512], ALU.add),
                       r=[g, bd], w=[yb])
                row0 = blk * BLK + r * 128
                S.dma("sp", lambda E, yb=yb, row0=row0: E.dma_start(out=ys_h[row0:row0 + 128, :], in_=yb.t[:]), yb, reads=[yb], writes=[])

        cur = blk_loads(0)
        blk_transposes(cur)
        for blk in range(NBLK):
            nxt = blk_loads(blk + 1) if blk + 1 < NBLK else None
            blk_gu(cur)
            if nxt is not None:
                blk_transposes(nxt)
            blk_down(cur, blk)
            cur = nxt

        if STOP == 4:
            return finish()
        S.barrier()
        p3.close()
        open_stacks.remove(p3)

        p5 = ExitStack()
        open_stacks.append(p5)
        gfin_bc = S.sbuf("gfin_bc", [128, D], F32, p5)
        LOAD("sp", gfin_bc, gfin_bc.t[:], gfin_bc_d)
        x1r = Rot([S.sbuf(f"x1r{i}", [128, D], F32, p5) for i in range(4)])
        ygat = Rot([S.sbuf(f"ygat{i}", [128, D], F32, p5) for i in range(12)])
        accb = Rot([S.sbuf(f"accb{i}", [128, D], F32, p5) for i in range(4)])
        junk5 = S.sbuf("junk5", [128, D], BF16, p5)
        bct5 = S.sbuf("bct5", [128, D], F32, p5)
        M.update({"wadab": Rot([S.sbuf(f"wadab5{i}", [128, KC, 128], F32, p5) for i in range(2)]),
                  "badab": Rot([S.sbuf(f"badab5{i}", [128, 128], F32, p5) for i in range(2)]),
                  "condrep": S.sbuf("condrep5", [128, KC, 128], F32, p5), "bct": bct5, "off": 24})
        ss5 = S.sbuf("ss5", [128, 2], F32, p5)
        for b in range(NSEQ):
            mod_bc(b, range(24, 32))
            for it in range(SEQ // 128):
                tt = b * (SEQ // 128) + it
                xb = x1r.next()
                LOAD("sp", xb, xb.t[:], x1_h[tt * 128:(tt + 1) * 128, :], reads=[B_x1])
                acc = accb.next()
                for k in range(4):
                    col = tt * 4 + k
                    yk = ygat.next()
                    S.dma("pool", lambda E, yk=yk, col=col: E.indirect_dma_start(
                        out=yk.t[:, :], out_offset=None, in_=ys_h,
                        in_offset=bass.IndirectOffsetOnAxis(ap=dest_i.t[:, col:col + 1], axis=0)), yk, reads=[B_ys, dest_i], writes=[yk])
                    if k == 0:
                        OP("dve", lambda E, yk=yk, acc=acc, col=col: E.tensor_scalar(acc.t[:], yk.t[:], wts.t[:, col:col + 1], None, ALU.mult), r=[yk, wts], w=[acc])
                    else:
                        eng = "dve"
                        OP(eng, lambda E, yk=yk, acc=acc, col=col: E.scalar_tensor_tensor(acc.t[:], yk.t[:], wts.t[:, col:col + 1], acc.t[:], ALU.mult, ALU.add),
                           r=[yk, wts, acc], w=[acc])
                OP("dve", lambda E, acc=acc: E.tensor_tensor(acc.t[:], acc.t[:], bct5.t[:, GT2:GT2 + D], ALU.mult), r=[acc, bct5], w=[acc])
                OP("pool", lambda E, acc=acc, xb=xb: E.tensor_tensor(acc.t[:], acc.t[:], xb.t[:], ALU.add), r=[acc, xb], w=[acc])
                OP("act", lambda E, acc=acc: E.activation(junk5.t[:], acc.t[:], AF.Square, accum_out=ss5.t[:, 0:1]), r=[acc], w=[junk5, ss5])
                OP("act", lambda E: E.activation(ss5.t[:, 0:1], ss5.t[:, 0:1], AF.Sqrt, bias=epst.t[:, 0:1], scale=1.0 / D), r=[ss5, epst], w=[ss5])
                OP("dve", lambda E: E.reciprocal(ss5.t[:, 1:2], ss5.t[:, 0:1]), r=[ss5], w=[ss5])
                OP("dve", lambda E, acc=acc: E.scalar_tensor_tensor(acc.t[:], acc.t[:], ss5.t[:, 1:2], gfin_bc.t[:], ALU.mult, ALU.mult), r=[acc, ss5, gfin_bc], w=[acc])
                S.dma("sp", lambda E, acc=acc, tt=tt: E.dma_start(out=out_d[tt * 128:(tt + 1) * 128, :], in_=acc.t[:]), acc, reads=[acc], writes=[])
        return finish()
    return nc


def _prep_shared(inp):
    f = lambda a: np.ascontiguousarray(np.asarray(a, dtype=np.float32))
    rep = lambda v: f(np.broadcast_to(np.asarray(v, np.float32).reshape(1, -1), (128, np.asarray(v).size)))
    fm = lambda v: f(np.asarray(v, np.float32).reshape(-1, 128).T)
    l = 0
    b_ada = np.asarray(inp["b_ada"][l], np.float32)
    d = {
        "w_ada": f(inp["w_ada"][l]),
        "b_ada_fm": fm(b_ada[:2 * D]),
        "b_ada_bc": rep(b_ada[2 * D:]),
        "g_mix_fm": fm(inp["g_mix"][l]),
        "g_ffn_bc": rep(inp["g_ffn"][l]),
        "g_final_bc": rep(inp["g_final"]),
        "w_in": f(inp["w_in"][l]),
        "w_sconv_fm": f(np.asarray(inp["w_sconv"][l], np.float32).reshape(3, KC, 128).transpose(2, 1, 0).reshape(128, KC * 3)),
        "w_sconv_out": f(inp["w_sconv_out"][l]),
        "w_ssm_conv_fm": f(np.asarray(inp["w_ssm_conv"][l], np.float32).reshape(4, 24, 128).transpose(2, 1, 0).reshape(128, 96)),
        "b_ssm_conv_fm": fm(inp["b_ssm_conv"][l]),
        "dt_bias_bc": rep(inp["dt_bias"][l]),
        "a_log_bc": rep(inp["a_log"][l]),
        "d_skip_bc": rep(inp["d_skip"][l]),
        "g_ssm_bc": rep(inp["g_ssm_norm"][l]),
        "w_ssm_out": f(inp["w_ssm_out"][l]),
        "w_o": f(inp["w_o"][l]),
        "w_router": f(inp["w_router"][l]),
        "b_router_bc": rep(inp["b_router"][l]),
        "w_gu": f(np.asarray(inp["w_gu"][l], np.float32).reshape(NE * D, 2 * D)),
        "b_gu_l": f(np.asarray(inp["b_gu"][l], np.float32).reshape(NE, 16, 128).transpose(0, 2, 1).reshape(NE * 128, 16)),
        "w_down": f(np.asarray(inp["w_down"][l], np.float32).reshape(NE * D, D)),
        "b_down": f(inp["b_down"][l]),
    }
    return d


_NC_CACHE = {}
_STOP = 99
_JLIST = (0, 1)


def kernel(**inputs):
    x = np.asarray(inputs["x"], np.float32)
    c = np.asarray(inputs["c"], np.float32)
    bsz, seq, _ = x.shape
    ncores = 8
    nseq = bsz // ncores
    key = (nseq, seq)
    if key not in _NC_CACHE:
        _NC_CACHE[key] = build_nc(nseq, seq, _STOP)
    nc = _NC_CACHE[key]
    shared = _prep_shared(inputs)
    in_maps = []
    for i in range(ncores):
        m = dict(shared)
        m["x"] = np.ascontiguousarray(x[i * nseq:(i + 1) * nseq].reshape(nseq * seq, D))
        cc = c[i * nseq:(i + 1) * nseq]
        m["cT"] = np.ascontiguousarray(cc.reshape(nseq, KC, 128).transpose(2, 1, 0).reshape(128, KC * nseq))
        in_maps.append(m)
    res = run_bass_kernel_spmd(nc, in_maps, core_ids=list(range(ncores)))
    out = np.concatenate([np.asarray(r["out"]).reshape(nseq, seq, D) for r in res.results], axis=0)
    return out.astype(np.float32)
```

```python
import numpy as np
from contextlib import ExitStack
import concourse.bass as bass
import concourse.mybir as mybir
from concourse.bass_utils import run_bass_kernel_spmd

F32 = mybir.dt.float32
BF16 = mybir.dt.bfloat16
I32 = mybir.dt.int32
U32 = mybir.dt.uint32
ALU = mybir.AluOpType
AF = mybir.ActivationFunctionType
AX = mybir.AxisListType

ENGS = ("pe", "act", "dve", "pool", "sp")
SAME_ENGINE_SYNC = ("act", "dve", "pool")
SEM_EPOCH = 30000
LAZY = ("pe",)

D = 1024
KC = 8
DI = 2048
NH = 32
HP = 64
NG = 4
NS = 128
NCOL = 10272
C_SB, C_SC, C_SX, C_Z, C_XBC, C_DT, C_GA, C_GB = 0, 1024, 2048, 3072, 5120, 8192, 8224, 9248
NE = 32
TOPK = 4
BLK = 512
T = 256
EPS = 1e-6


class Buf:
    __slots__ = ("t", "w", "r", "sem", "semcnt", "name")

    def __init__(self, t, name=""):
        self.t = t
        self.w = None
        self.r = []
        self.sem = None
        self.semcnt = 0
        self.name = name


class Rot:
    def __init__(self, bufs):
        self.bufs = bufs
        self.i = 0

    def next(self):
        b = self.bufs[self.i % len(self.bufs)]
        self.i += 1
        return b


class Sched:
    def __init__(self, nc, stack):
        self.nc = nc
        self.stack = stack
        self.q = {e: [] for e in ENGS}
        self.nsem = 0
        self.dma_bufs = []
        self.esem = {}
        self.ecnt = {}
        self.allsems = {e: [] for e in ENGS}
        self.lastcell = {}
        for e in ENGS:
            self._new_epoch(e)
        self.waited = {e: {} for e in ENGS}

    def _new_sem(self, name):
        self.nsem += 1
        return self.stack.enter_context(self.nc.semaphore(f"{name}_{self.nsem}"))

    def _new_epoch(self, e):
        self.esem[e] = self._new_sem("e" + e)
        self.ecnt[e] = 0
        self.allsems[e].append(self.esem[e])

    def sbuf(self, name, shape, dt, stack=None):
        st = stack or self.stack
        return Buf(st.enter_context(self.nc.sbuf_tensor("s_" + name, list(shape), dt)), name)

    def psum(self, name, shape, dt):
        return Buf(self.stack.enter_context(self.nc.psum_tensor("p_" + name, list(shape), dt)), name)

    def _flush(self, e):
        cell = self.lastcell.get(e)
        if cell is None or cell["inc"]:
            return
        cell["inc"] = True
        self.ecnt[e] += 1
        self.lastcell[e] = None
        if self.ecnt[e] >= SEM_EPOCH:
            self._new_epoch(e)

    def _collect(self, eng, reads, writes):
        waits = {}

        def need(ev):
            if ev is None:
                return
            if ev[0] == "e":
                _, e2, sem, c = ev
                if e2 == eng and eng not in SAME_ENGINE_SYNC:
                    return
                if sem is self.esem[e2] and c > self.ecnt[e2]:
                    self._flush(e2)
                val = c
            else:
                sem = ev[1].sem
                val = ev[1].semcnt
            key = id(sem)
            if key not in waits or waits[key][1] < val:
                waits[key] = (sem, val)

        for b in reads:
            need(b.w)
        for b in writes:
            need(b.w)
            for ev in b.r:
                need(ev)
        out = []
        wd = self.waited[eng]
        for key, (sem, val) in waits.items():
            if wd.get(key, 0) >= val:
                continue
            wd[key] = val
            out.append((sem, val))
        return out

    def op(self, eng, fn, reads=(), writes=(), inc=True):
        waits = self._collect(eng, reads, writes)
        sem = self.esem[eng]
        cnt = self.ecnt[eng] + 1
        if eng in LAZY:
            cell = {"inc": False}
            self.lastcell[eng] = cell
        else:
            cell = {"inc": True}
            self.ecnt[eng] = cnt
            if cnt >= SEM_EPOCH:
                self._new_epoch(eng)

        def rec(E, waits=waits, fn=fn, sem=sem, cell=cell):
            for s, v in waits:
                E.wait_ge(s, v)
            ins = fn(E)
            if cell["inc"]:
                ins.then_inc(sem, 1)

        self.q[eng].append(rec)
        ev = ("e", eng, sem, cnt)
        for b in writes:
            b.w = ev
            b.r = []
        for b in reads:
            b.r.append(ev)

    def dma(self, eng, fn, owner, reads=(), writes=()):
        waits = self._collect(eng, reads, writes)
        if owner.sem is None:
            owner.sem = self._new_sem("d" + owner.name)
            self.dma_bufs.append(owner)
        owner.semcnt += 16
        sem = owner.sem

        def rec(E, waits=waits, fn=fn, sem=sem):
            for s, v in waits:
                E.wait_ge(s, v)
            fn(E).then_inc(sem, 16)

        self.q[eng].append(rec)
        ev = ("d", owner)
        for b in writes:
            b.w = ev
            b.r = []
        for b in reads:
            b.r.append(ev)

    def barrier(self):
        for e in ENGS:
            self._flush(e)
        for eng in ENGS:
            waits = []
            wd = self.waited[eng]
            for e2 in ENGS:
                if e2 == eng or self.ecnt[e2] == 0:
                    continue
                sem, val = self.esem[e2], self.ecnt[e2]
                if wd.get(id(sem), 0) < val:
                    wd[id(sem)] = val
                    waits.append((sem, val))
            for ob in self.dma_bufs:
                if wd.get(id(ob.sem), 0) < ob.semcnt:
                    wd[id(ob.sem)] = ob.semcnt
                    waits.append((ob.sem, ob.semcnt))

            def rec(E, waits=waits):
                for s, v in waits:
                    E.wait_ge(s, v)

            self.q[eng].append(rec)

    def emit(self):
        q = self.q
        with self.nc.Block() as block:
            @block.sync
            def _(E):
                for r in q["sp"]:
                    r(E)

            @block.tensor
            def _(E):
                for r in q["pe"]:
                    r(E)

            @block.scalar
            def _(E):
                for r in q["act"]:
                    r(E)

            @block.vector
            def _(E):
                for r in q["dve"]:
                    r(E)

            @block.gpsimd
            def _(E):
                for r in q["pool"]:
                    r(E)


def build_nc(NSEQ, SEQ, STOP=99):
    nc = bass.Bass("TRN2", target_bir_lowering=False)
    NT = NSEQ * SEQ
    NTT = NT // 128
    NTL = SEQ // T
    NROWS = NT * TOPK
    NBLK = NROWS // BLK + NE
    RTOT = NBLK * BLK

    def din(name, shape, dt=F32):
        return nc.dram_tensor(name, list(shape), dt, kind="ExternalInput").ap()

    def dscr(name, shape, dt):
        return nc.dram_tensor(name, list(shape), dt, kind="Internal").ap()

    x_d = din("x", [NT, D])
    cT_d = din("cT", [128, KC * NSEQ])
    wada_d = din("w_ada", [D, 6 * D])
    bada_fm_d = din("b_ada_fm", [128, 16])
    bada_bc_d = din("b_ada_bc", [128, 4 * D])
    gmix_fm_d = din("g_mix_fm", [128, KC])
    gffn_bc_d = din("g_ffn_bc", [128, D])
    gfin_bc_d = din("g_final_bc", [128, D])
    win_d = din("w_in", [D, NCOL])
    wsc_fm_d = din("w_sconv_fm", [128, KC * 3])
    wsco_d = din("w_sconv_out", [D, D])
    wxc_fm_d = din("w_ssm_conv_fm", [128, 24 * 4])
    bxc_fm_d = din("b_ssm_conv_fm", [128, 24])
    dtb_bc_d = din("dt_bias_bc", [128, NH])
    alog_bc_d = din("a_log_bc", [128, NH])
    dsk_bc_d = din("d_skip_bc", [128, NH])
    gssm_bc_d = din("g_ssm_bc", [128, DI])
    wsso_d = din("w_ssm_out", [DI, D])
    wo_d = din("w_o", [D, D])
    wr_d = din("w_router", [D, NE])
    br_bc_d = din("b_router_bc", [128, NE])
    wgu_d = din("w_gu", [NE * D, 2 * D])
    bgu_l_d = din("b_gu_l", [NE * 128, 16])
    wd_d = din("w_down", [NE * D, D])
    bd_d = din("b_down", [NE, D])
    out_d = nc.dram_tensor("out", [NT, D], F32, kind="ExternalOutput").ap()

    win_bf = dscr("win_bf", [D, NCOL], BF16)
    wsco_bf = dscr("wsco_bf", [D, D], BF16)
    wsso_bf = dscr("wsso_bf", [DI, D], BF16)
    wo_bf = dscr("wo_bf", [D, D], BF16)
    wg_bf = dscr("wg_bf", [NE * 128, KC * D], BF16)
    wu_bf = dscr("wu_bf", [NE * 128, KC * D], BF16)
    wd_bf = dscr("wd_bf", [NE * 128, KC * D], BF16)
    x1_h = dscr("x1_h", [NT, D], F32)
    h2_h = dscr("h2_h", [NT, D], BF16)
    xs_h = dscr("xs_h", [RTOT, D], BF16)
    ys_h = dscr("ys_h", [RTOT, D], F32)

    with ExitStack() as st:
        S = Sched(nc, st)
        def OP(eng, fn, r=(), w=()):
            S.op(eng, fn, reads=r, writes=w)

        def MM(outb, out_ap, pairs, reads):
            n = len(pairs)
            for i, (l, r_) in enumerate(pairs):
                S.op("pe", lambda E, l=l, r_=r_, i=i: E.matmul(out_ap, l, r_, start=(i == 0), stop=(i == n - 1)),
                     reads=reads, writes=[outb], inc=(i == n - 1))

        def TR(outb, out_ap, in_ap, idn, reads):
            S.op("pe", lambda E: E.transpose(out_ap, in_ap, idn.t[:]), reads=list(reads) + [idn], writes=[outb])

        def LOAD(eng, dst, dst_ap, src_ap, reads=()):
            S.dma(eng, lambda E: E.dma_start(out=dst_ap, in_=src_ap), dst, reads=reads, writes=[dst])

        open_stacks = []

        def finish():
            S.barrier()
            S.emit()
            for stx in reversed(open_stacks):
                stx.close()
            return nc

        B_win, B_wsco, B_wsso, B_wo = Buf(None, "winbf"), Buf(None, "wscobf"), Buf(None, "wssobf"), Buf(None, "wobf")
        B_wgu, B_wd = Buf(None, "wgubf"), Buf(None, "wdbf")
        B_x1, B_h2, B_xs, B_ys, B_out = Buf(None, "x1h"), Buf(None, "h2h"), Buf(None, "xsh"), Buf(None, "ysh"), Buf(None, "outh")

        for kc in range(KC):
            S.dma("pool", lambda E, kc=kc: E.dma_start(out=win_bf[kc * 128:(kc + 1) * 128, :], in_=win_d[kc * 128:(kc + 1) * 128, :]),
                  B_win, writes=[B_win])
        for (dst, src, bb, rows) in ((wsco_bf, wsco_d, B_wsco, D), (wsso_bf, wsso_d, B_wsso, DI), (wo_bf, wo_d, B_wo, D)):
            for r0 in range(0, rows, 512):
                S.dma("pool", lambda E, dst=dst, src=src, r0=r0: E.dma_start(out=dst[r0:r0 + 512, :], in_=src[r0:r0 + 512, :]),
                      bb, writes=[bb])

        G = [S.psum(f"G{i}", [128, 512], F32) for i in range(5)]
        ptb = S.psum("ptb", [128, 2048], BF16)
        ptB = S.psum("ptB", [128, 1024], BF16)
        Grot = Rot(G)

        io = S.sbuf("io", [128, 128], F32)
        ident = S.sbuf("ident", [128, 128], F32)
        identb = S.sbuf("identb", [128, 128], BF16)
        triu = S.sbuf("triu", [128, 128], F32)
        ones = S.sbuf("ones", [128, 128], F32)
        onesb = S.sbuf("onesb", [128, 128], BF16)
        tristb = S.sbuf("tristb", [128, 128], BF16)
        epst = S.sbuf("epst", [128, 1], F32)
        iota32 = S.sbuf("iota32", [128, NE], F32)
        OP("pool", lambda E: E.iota(io.t[:], pattern=[[1, 128]], base=0, channel_multiplier=-1,
                                    allow_small_or_imprecise_dtypes=True), w=[io])
        OP("pool", lambda E: E.iota(iota32.t[:], pattern=[[1, NE]], base=0, channel_multiplier=0,
                                    allow_small_or_imprecise_dtypes=True), w=[iota32])
        OP("dve", lambda E: E.tensor_single_scalar(ident.t[:], io.t[:], 0.0, ALU.is_equal), r=[io], w=[ident])
        OP("dve", lambda E: E.tensor_single_scalar(identb.t[:], io.t[:], 0.0, ALU.is_equal), r=[io], w=[identb])
        OP("dve", lambda E: E.tensor_single_scalar(triu.t[:], io.t[:], 0.0, ALU.is_ge), r=[io], w=[triu])
        OP("dve", lambda E: E.tensor_single_scalar(tristb.t[:], io.t[:], 0.0, ALU.is_gt), r=[io], w=[tristb])
        OP("dve", lambda E: E.memset(ones.t[:], 1.0), w=[ones])
        OP("dve", lambda E: E.memset(onesb.t[:], 1.0), w=[onesb])
        OP("dve", lambda E: E.memset(epst.t[:], EPS), w=[epst])

        def small(name, shape, src, dt=F32):
            b = S.sbuf(name, shape, dt)
            LOAD("sp", b, b.t[:], src)
            return b

        cT = small("cT", [128, KC * NSEQ], cT_d)
        bada_fm = small("bada_fm", [128, 16], bada_fm_d)
        gmix_fm = small("gmix_fm", [128, KC], gmix_fm_d)
        wsc_fm = small("wsc_fm", [128, KC * 3], wsc_fm_d)
        wxc_fm = small("wxc_fm", [128, 96], wxc_fm_d)
        bxc_fm = small("bxc_fm", [128, 24], bxc_fm_d)
        dtb_bc = small("dtb_bc", [128, NH], dtb_bc_d)
        a_bc = small("a_bc", [128, NH], alog_bc_d)
        dsk_bc = small("dsk_bc", [128, NH], dsk_bc_d)
        br_bc = small("br_bc", [128, NE], br_bc_d)
        wr_sb = S.sbuf("wr_sb", [128, KC, NE], F32)
        LOAD("sp", wr_sb, wr_sb.t[:], wr_d.rearrange("(kc p) n -> p kc n", p=128))
        wdt_sb = S.sbuf("wdt_sb", [128, KC, NH], BF16)
        OP("act", lambda E: E.activation(a_bc.t[:], a_bc.t[:], AF.Exp), r=[a_bc], w=[a_bc])
        OP("dve", lambda E: E.tensor_single_scalar(a_bc.t[:], a_bc.t[:], -1.0, ALU.mult), r=[a_bc], w=[a_bc])
        OP("act", lambda E: E.activation(cT.t[:], cT.t[:], AF.Silu), r=[cT], w=[cT])

        gs1 = S.sbuf("gs1", [128, KC * NSEQ], F32)
        sh1 = S.sbuf("sh1", [128, KC * NSEQ], F32)
        wts = S.sbuf("wts", [128, NTT * TOPK], F32)
        dest_i = S.sbuf("dest_i", [128, NTT * TOPK], I32)
        wrow = S.sbuf("wrow", [128, NBLK * KC], I32)
        brow = S.sbuf("brow", [128, NBLK], I32)
        erow = S.sbuf("erow", [128, NBLK], I32)
        plog = ExitStack()
        p1 = ExitStack()
        open_stacks.extend([plog, p1])
        logits = S.sbuf("logits", [128, NTT * NE], F32, plog)
        wadab = Rot([S.sbuf(f"wadab{i}", [128, KC, 128], F32, p1) for i in range(2)])
        badab = Rot([S.sbuf(f"badab{i}", [128, 128], F32, p1) for i in range(2)])
        condrep = S.sbuf("condrep", [128, KC, 128], F32, p1)
        bct = S.sbuf("bct", [128, 3 * D], F32, p1)
        gffn_bc = S.sbuf("gffn_bc", [128, D], F32, p1)
        LOAD("sp", gffn_bc, gffn_bc.t[:], gffn_bc_d)
        M = {"wadab": wadab, "badab": badab, "condrep": condrep, "bct": bct, "off": 0}
        wada_r = wada_d.rearrange("(kc p) n -> p kc n", p=128)

        for ch in range(16):
            wb = wadab.next()
            LOAD("sp", wb, wb.t[:], wada_r[:, :, ch * 128:(ch + 1) * 128])
            for jj in range(1):
                j = ch
                g = Grot.next()
                MM(g, g.t[:, 0:NSEQ], [(wb.t[:, kc, jj * 128:(jj + 1) * 128], cT.t[:, kc * NSEQ:(kc + 1) * NSEQ]) for kc in range(KC)], [wb, cT])
                if j < 8:
                    for b in range(NSEQ):
                        OP("act", lambda E, g=g, j=j, b=b: E.activation(sh1.t[:, j * NSEQ + b:j * NSEQ + b + 1], g.t[:, b:b + 1], AF.Identity,
                                                                         bias=bada_fm.t[:, j:j + 1], scale=1.0), r=[g, bada_fm], w=[sh1])
                else:
                    c = j - 8
                    for b in range(NSEQ):
                        OP("act", lambda E, g=g, j=j, b=b, c=c: E.activation(gs1.t[:, c * NSEQ + b:c * NSEQ + b + 1], g.t[:, b:b + 1], AF.Identity,
                                                                              bias=bada_fm.t[:, j:j + 1], scale=1.0), r=[g, bada_fm], w=[gs1])
                        OP("dve", lambda E, b=b, c=c: E.tensor_scalar(gs1.t[:, c * NSEQ + b:c * NSEQ + b + 1], gs1.t[:, c * NSEQ + b:c * NSEQ + b + 1],
                                                                     1.0, gmix_fm.t[:, c:c + 1], ALU.add, ALU.mult), r=[gs1, gmix_fm], w=[gs1])

        def mod_bc(b, chunks):
            wadab, badab, condrep, bct, off = M["wadab"], M["badab"], M["condrep"], M["bct"], M["off"]
            for kc in range(KC):
                OP("dve", lambda E, kc=kc: E.tensor_copy(condrep.t[:, kc, :], cT.t[:, kc * NSEQ + b:kc * NSEQ + b + 1].to_broadcast([128, 128])),
                   r=[cT], w=[condrep])
            for ch in chunks:
                wb = wadab.next()
                bb = badab.next()
                LOAD("sp", wb, wb.t[:], wada_r[:, :, 2 * D + ch * 128:2 * D + (ch + 1) * 128])
                LOAD("sp", bb, bb.t[:], bada_bc_d[:, ch * 128:(ch + 1) * 128])
                g = Grot.next()
                MM(g, g.t[:, 0:128], [(condrep.t[:, kc, :], wb.t[:, kc, :]) for kc in range(KC)], [wb, condrep])
                dsl = slice((ch - off) * 128, (ch - off + 1) * 128)
                OP("dve", lambda E, g=g, bb=bb, dsl=dsl: E.tensor_tensor(bct.t[:, dsl], g.t[:, 0:128], bb.t[:], ALU.add),
                   r=[g, bb], w=[bct])
                if 16 <= ch < 24:
                    OP("dve", lambda E, ch=ch, dsl=dsl: E.scalar_tensor_tensor(bct.t[:, dsl], bct.t[:, dsl], 1.0,
                                                                      gffn_bc.t[:, (ch - 16) * 128:(ch - 15) * 128], ALU.add, ALU.mult),
                       r=[bct, gffn_bc], w=[bct])

        GT1, SH2, GS2, GT2 = 0, D, 2 * D, 0
        if STOP == 0:
            return finish()

        gssm_bc = S.sbuf("gssm_bc", [128, DI], F32, p1)
        LOAD("sp", gssm_bc, gssm_bc.t[:], gssm_bc_d)
        LOAD("sp", wdt_sb, wdt_sb.t[:], win_bf.rearrange("(kc p) n -> p kc n", p=128)[:, :, C_DT:C_DT + NH], reads=[B_win])
        wst = Rot([S.sbuf(f"wst{i}", [128, KC, 512], BF16, p1) for i in range(4)])
        xr = Rot([S.sbuf(f"xr{i}", [128, D], F32, p1) for i in range(2)])
        junk = S.sbuf("junk", [128, D], BF16, p1)
        ss = S.sbuf("ss", [128, 4], F32, p1)
        rstd = S.sbuf("rstd", [128, 4], F32, p1)
        hT = S.sbuf("hT", [128, KC, T], BF16, p1)
        vT = S.sbuf("vT", [128, KC, T], BF16, p1)
        tmpA = Rot([S.sbuf(f"tmpA{i}", [128, T], F32, p1) for i in range(2)])
        tmpY = Rot([S.sbuf(f"tmpY{i}", [128, T], F32, p1) for i in range(2)])
        utmp = Rot([S.sbuf(f"utmp{i}", [128, T + 2], F32, p1) for i in range(2)])
        uhalo = S.sbuf("uhalo", [128, KC, 2], F32, p1)
        xraw = Rot([S.sbuf(f"xraw{i}", [128, T + 3], F32, p1) for i in range(2)])
        xhalo = S.sbuf("xhalo", [128, 24, 3], F32, p1)
        xsT = S.sbuf("xsT", [128, 16, T], BF16, p1)
        BT = S.sbuf("BT", [128, NG, T], BF16, p1)
        CT = S.sbuf("CT", [128, NG, T], BF16, p1)
        zs = S.sbuf("zs", [128, 2, DI], BF16, p1)
        dtt = S.sbuf("dtt", [128, 2, NH], F32, p1)
        dat = S.sbuf("dat", [128, 2, NH], F32, p1)
        dtmp = S.sbuf("dtmp", [128, NH], F32, p1)
        acum = S.sbuf("acum", [128, NH], F32, p1)
        dend = S.sbuf("dend", [128, NH], F32, p1)
        dchunk = S.sbuf("dchunk", [128, NH], F32, p1)
        eacum = S.sbuf("eacum", [128, NH], F32, p1)
        xs_tok = S.sbuf("xs_tok", [128, DI], BF16, p1)
        xdt = S.sbuf("xdt", [128, DI], BF16, p1)
        xdte = S.sbuf("xdte", [128, DI], BF16, p1)
        B_tok = S.sbuf("B_tok", [128, NG * NS], BF16, p1)
        CBm = S.sbuf("CBm", [128, NG, 128], F32, p1)
        seg = Rot([S.sbuf(f"seg{i}", [128, 4, 128], F32, p1) for i in range(2)])
        Sb = S.sbuf("Sb", [128, 8, 128], BF16, p1)
        st32 = S.sbuf("st32", [128, DI], F32, p1)
        stbf = S.sbuf("stbf", [128, DI], BF16, p1)
        yg = Rot([S.sbuf(f"yg{i}", [128, 512], F32, p1) for i in range(2)])
        yt2 = Rot([S.sbuf(f"yt2{i}", [128, 512], F32, p1) for i in range(1)])
        yn = Rot([S.sbuf(f"yn{i}", [128, 512], BF16, p1) for i in range(2)])
        ssg = S.sbuf("ssg", [128, 2], F32, p1)
        ynT = S.sbuf("ynT", [128, 16, T], BF16, p1)
        mpart = S.sbuf("mpart", [128, 4, T], F32, p1)
        mT = S.sbuf("mT", [128, KC, T], BF16, p1)
        h2 = S.sbuf("h2", [128, D], F32, p1)
        h2b = Rot([S.sbuf(f"h2b{i}", [128, D], BF16, p1) for i in range(1)])
        h2T = S.sbuf("h2T", [128, KC, 128], F32, p1)

        win_r = win_bf.rearrange("(kc p) n -> p kc n", p=128)
        wsco_r = wsco_bf.rearrange("(kc p) n -> p kc n", p=128)
        wsso_r = wsso_bf.rearrange("(kc p) n -> p kc n", p=128)
        wo_r = wo_bf.rearrange("(kc p) n -> p kc n", p=128)

        def wload(src_r, bb, c0, k0=0):
            w = wst.next()
            LOAD("sp", w, w.t[:], src_r[:, k0:k0 + KC, c0:c0 + 512], reads=[bb])
            return w

        def rms_stats(src, col, n):
            OP("act", lambda E: E.activation(junk.t[:, 0:n], src, AF.Square, accum_out=ss.t[:, col:col + 1]), r=[], w=[junk, ss])
            OP("act", lambda E: E.activation(ss.t[:, col:col + 1], ss.t[:, col:col + 1], AF.Sqrt, bias=epst.t[:, 0:1], scale=1.0 / n),
               r=[ss, epst], w=[ss])
            OP("dve", lambda E: E.reciprocal(rstd.t[:, col:col + 1], ss.t[:, col:col + 1]), r=[ss], w=[rstd])

        cast_jobs = []
        for e in range(NE):
            cast_jobs.append((wg_bf, wgu_d, e, 0, B_wgu))
            cast_jobs.append((wu_bf, wgu_d, e, D, B_wgu))
            cast_jobs.append((wd_bf, wd_d, e, 0, B_wd))

        def issue_casts(n):
            for _ in range(n):
                if not cast_jobs:
                    return
                dst, src, e, c0, bb = cast_jobs.pop(0)
                S.dma("pool", lambda E, dst=dst, src=src, e=e, c0=c0: E.dma_start(
                    out=dst[e * 128:(e + 1) * 128, :].rearrange("p (k n) -> p k n", n=D),
                    in_=src[e * D:(e + 1) * D, c0:c0 + D].rearrange("(k p) n -> p k n", p=128)), bb, writes=[bb])

        casts_per_tile = -(-len(cast_jobs) // (NSEQ * NTL))
        zt = S.sbuf("zt", [128, 2 * D], BF16, p1)
        OP("pool", lambda E: E.memset(zt.t[:], 0.0), w=[zt])
        B_xz = Buf(None, "xsz")
        zero_jobs = list(range(0, RTOT, 256))

        def issue_zero(n):
            for _ in range(n):
                if not zero_jobs:
                    return
                q0 = zero_jobs.pop(0)
                S.dma("sp", lambda E, q0=q0: E.dma_start(out=xs_h[q0:q0 + 256, :].rearrange("(p r) d -> p (r d)", r=2), in_=zt.t[:]),
                      B_xz, reads=[zt], writes=[])
            B_xz.w = ("d", B_xz)

        zeros_per_tile = -(-len(zero_jobs) // (NSEQ * NTL))
        for b in range(NSEQ):
            mod_bc(b, range(0, 24))
            OP("dve", lambda E: E.memset(st32.t[:], 0.0), w=[st32])
            OP("dve", lambda E: E.memset(stbf.t[:], 0.0), w=[stbf])
            OP("dve", lambda E: E.memset(uhalo.t[:], 0.0), w=[uhalo])
            OP("dve", lambda E: E.memset(xhalo.t[:], 0.0), w=[xhalo])
            for it in range(NTL):
                r0 = b * SEQ + it * T
                xs_ = []
                for j in range(2):
                    xb = xr.next()
                    xs_.append(xb)
                    LOAD("sp", xb, xb.t[:], x_d[r0 + j * 128:r0 + (j + 1) * 128, :])
                    OP("act", lambda E, xb=xb, j=j: E.activation(junk.t[:], xb.t[:], AF.Square, accum_out=ss.t[:, j:j + 1]), r=[xb], w=[junk, ss])
                    OP("act", lambda E, j=j: E.activation(ss.t[:, j:j + 1], ss.t[:, j:j + 1], AF.Sqrt, bias=epst.t[:, 0:1], scale=1.0 / D),
                       r=[ss, epst], w=[ss])
                    OP("dve", lambda E, j=j: E.reciprocal(rstd.t[:, j:j + 1], ss.t[:, j:j + 1]), r=[ss], w=[rstd])
                    OP("pool", lambda E, xb=xb, j=j: E.tensor_scalar(xb.t[:], xb.t[:], rstd.t[:, j:j + 1], None, ALU.mult), r=[xb, rstd], w=[xb])
                for c in range(KC):
                    g = Grot.next()
                    for j in range(2):
                        TR(g, g.t[:, j * 128:(j + 1) * 128], xs_[j].t[:, c * 128:(c + 1) * 128], ident, [xs_[j]])
                    OP("act", lambda E, g=g, c=c, b=b: E.activation(hT.t[:, c, :], g.t[:, 0:T], AF.Identity,
                                                               bias=sh1.t[:, c * NSEQ + b:c * NSEQ + b + 1],
                                                               scale=gs1.t[:, c * NSEQ + b:c * NSEQ + b + 1]), r=[g, sh1, gs1], w=[hT])

                def proj_fm(w, cc):
                    g = Grot.next()
                    MM(g, g.t[:, 0:T], [(w.t[:, kc, cc * 128:(cc + 1) * 128], hT.t[:, kc, :]) for kc in range(KC)], [w, hT])
                    return g

                if STOP == 11:
                    return finish()
                for q in range(2):
                    w_sb = wload(win_r, B_win, C_SB + q * 512)
                    w_sx = wload(win_r, B_win, C_SX + q * 512)
                    w_sc = wload(win_r, B_win, C_SC + q * 512)
                    for cc in range(4):
                        c = q * 4 + cc
                        g_sb = proj_fm(w_sb, cc)
                        g_sx = proj_fm(w_sx, cc)
                        ta = tmpA.next()
                        OP("act", lambda E, ta=ta, g=g_sx: E.activation(ta.t[:], g.t[:, 0:T], AF.Copy), r=[g_sx], w=[ta])
                        u = utmp.next()
                        OP("pool", lambda E, u=u, c=c: E.tensor_copy(u.t[:, 0:2], uhalo.t[:, c, :]), r=[uhalo], w=[u])
                        OP("dve", lambda E, u=u, g=g_sb, ta=ta: E.tensor_tensor(u.t[:, 2:T + 2], g.t[:, 0:T], ta.t[:], ALU.mult), r=[g_sb, ta], w=[u])
                        OP("pool", lambda E, u=u, c=c: E.tensor_copy(uhalo.t[:, c, :], u.t[:, T:T + 2]), r=[u], w=[uhalo])
                        g_sc = proj_fm(w_sc, cc)
                        y = tmpY.next()
                        OP("pool", lambda E, y=y, u=u, c=c: E.tensor_scalar(y.t[:], u.t[:, 2:T + 2], wsc_fm.t[:, c * 3 + 2:c * 3 + 3], None, ALU.mult),
                           r=[u, wsc_fm], w=[y])
                        OP("dve", lambda E, y=y, u=u, c=c: E.scalar_tensor_tensor(y.t[:], u.t[:, 1:T + 1], wsc_fm.t[:, c * 3 + 1:c * 3 + 2], y.t[:], ALU.mult, ALU.add),
                           r=[u, wsc_fm, y], w=[y])
                        OP("dve", lambda E, y=y, u=u, c=c: E.scalar_tensor_tensor(y.t[:], u.t[:, 0:T], wsc_fm.t[:, c * 3:c * 3 + 1], y.t[:], ALU.mult, ALU.add),
                           r=[u, wsc_fm, y], w=[y])
                        OP("dve", lambda E, y=y, g=g_sc, c=c: E.tensor_tensor(vT.t[:, c, :], g.t[:, 0:T], y.t[:], ALU.mult), r=[g_sc, y], w=[vT])

                if STOP == 12:
                    return finish()
                for zq in range(4):
                    w_z = wload(win_r, B_win, C_Z + zq * 512)
                    for j in range(2):
                        g = Grot.next()
                        MM(g, g.t[:], [(hT.t[:, kc, j * 128:(j + 1) * 128], w_z.t[:, kc, :]) for kc in range(KC)], [w_z, hT])
                        OP("act", lambda E, g=g, j=j, zq=zq: E.activation(zs.t[:, j, zq * 512:(zq + 1) * 512], g.t[:], AF.Silu), r=[g], w=[zs])
                for j in range(2):
                    g = Grot.next()
                    MM(g, g.t[:, 0:NH], [(hT.t[:, kc, j * 128:(j + 1) * 128], wdt_sb.t[:, kc, :]) for kc in range(KC)], [wdt_sb, hT])
                    OP("dve", lambda E, g=g: E.tensor_tensor(dtmp.t[:], g.t[:, 0:NH], dtb_bc.t[:], ALU.add), r=[g, dtb_bc], w=[dtmp])
                    OP("act", lambda E: E.activation(dtmp.t[:], dtmp.t[:], AF.Exp), r=[dtmp], w=[dtmp])
                    OP("act", lambda E, j=j: E.activation(dtt.t[:, j, :], dtmp.t[:], AF.Ln, bias=1.0, scale=1.0), r=[dtmp], w=[dtt])
                    OP("dve", lambda E, j=j: E.tensor_tensor(dat.t[:, j, :], dtt.t[:, j, :], a_bc.t[:], ALU.mult), r=[dtt, a_bc], w=[dat])
                for xq in range(6):
                    w_x = wload(win_r, B_win, C_XBC + xq * 512)
                    for cc in range(4):
                        cx = xq * 4 + cc
                        g = proj_fm(w_x, cc)
                        xw = xraw.next()
                        OP("pool", lambda E, xw=xw, cx=cx: E.tensor_copy(xw.t[:, 0:3], xhalo.t[:, cx, :]), r=[xhalo], w=[xw])
                        OP("act", lambda E, xw=xw, g=g: E.activation(xw.t[:, 3:T + 3], g.t[:, 0:T], AF.Copy), r=[g], w=[xw])
                        OP("pool", lambda E, xw=xw, cx=cx: E.tensor_copy(xhalo.t[:, cx, :], xw.t[:, T:T + 3]), r=[xw], w=[xhalo])
                        y = tmpY.next()
                        OP("dve", lambda E, y=y, xw=xw, cx=cx: E.tensor_scalar(y.t[:], xw.t[:, 3:T + 3], wxc_fm.t[:, cx * 4 + 3:cx * 4 + 4], None, ALU.mult),
                           r=[xw, wxc_fm], w=[y])
                        for k in (2, 1, 0):
                            eng = "dve"
                            OP(eng, lambda E, y=y, xw=xw, cx=cx, k=k: E.scalar_tensor_tensor(y.t[:], xw.t[:, k:k + T], wxc_fm.t[:, cx * 4 + k:cx * 4 + k + 1], y.t[:],
                                                                                           ALU.mult, ALU.add), r=[xw, wxc_fm, y], w=[y])
                        if cx < 16:
                            dstb, dst = xsT, xsT.t[:, cx, :]
                        elif cx < 20:
                            dstb, dst = BT, BT.t[:, cx - 16, :]
                        else:
                            dstb, dst = CT, CT.t[:, cx - 20, :]
                        OP("act", lambda E, y=y, dst=dst, cx=cx: E.activation(dst, y.t[:], AF.Silu, bias=bxc_fm.t[:, cx:cx + 1], scale=1.0),
                           r=[y, bxc_fm], w=[dstb])

                if STOP == 13:
                    return finish()
                issue_zero(zeros_per_tile)
                for jpos, j in enumerate(_JLIST):
                    SD = (STOP - 100 * jpos) if 141 + 100 * jpos <= STOP <= 148 + 100 * jpos else -1
                    js = slice(j * 128, (j + 1) * 128)
                    g0 = G[0]
                    MM(g0, g0.t[:, 0:NH], [(triu.t[:], dat.t[:, j, :])], [triu, dat])
                    MM(g0, g0.t[:, NH:2 * NH], [(ones.t[:], dat.t[:, j, :])], [ones, dat])
                    OP("dve", lambda E: E.tensor_copy(acum.t[:], g0.t[:, 0:NH]), r=[g0], w=[acum])
                    OP("dve", lambda E: E.tensor_tensor(dend.t[:], g0.t[:, NH:2 * NH], acum.t[:], ALU.subtract), r=[g0, acum], w=[dend])
                    OP("act", lambda E: E.activation(dend.t[:], dend.t[:], AF.Exp), r=[dend], w=[dend])
                    OP("act", lambda E: E.activation(dchunk.t[:], g0.t[:, NH:2 * NH], AF.Exp), r=[g0], w=[dchunk])
                    OP("act", lambda E: E.activation(eacum.t[:], acum.t[:], AF.Exp), r=[acum], w=[eacum])
                    if STOP == 240 and jpos == 1:
                        return finish()
                    for cx in range(16):
                        TR(ptb, ptb.t[:, cx * 128:(cx + 1) * 128], xsT.t[:, cx, js], identb, [xsT])
                    if STOP == 2401 and jpos == 1:
                        return finish()
                    for g_ in range(NG):
                        TR(ptB, ptB.t[:, g_ * 128:(g_ + 1) * 128], BT.t[:, g_, js], identb, [BT])
                    if STOP == 2402 and jpos == 1:
                        return finish()
                    OP("act", lambda E: E.activation(xs_tok.t[:], ptb.t[:], AF.Copy), r=[ptb], w=[xs_tok])
                    if STOP == 2403 and jpos == 1:
                        return finish()
                    OP("dve", lambda E, j=j: E.tensor_tensor(xdt.t[:].rearrange("p (h q) -> p h q", q=HP), xs_tok.t[:].rearrange("p (h q) -> p h q", q=HP),
                                                             dtt.t[:, j, :].unsqueeze(2).to_broadcast([128, NH, HP]), ALU.mult), r=[xs_tok, dtt], w=[xdt])
                    if STOP == 2404 and jpos == 1:
                        return finish()
                    OP("dve", lambda E: E.tensor_tensor(xdte.t[:].rearrange("p (h q) -> p h q", q=HP), xdt.t[:].rearrange("p (h q) -> p h q", q=HP),
                                                         dend.t[:].unsqueeze(2).to_broadcast([128, NH, HP]), ALU.mult), r=[xdt, dend], w=[xdte])
                    OP("act", lambda E: E.activation(B_tok.t[:], ptB.t[:, 0:NG * NS], AF.Copy), r=[ptB], w=[B_tok])
                    if SD == 141:
                        return finish()
                    g1 = G[1]
                    for g_ in range(NG):
                        MM(g1, g1.t[:, g_ * 128:(g_ + 1) * 128], [(BT.t[:, g_, js], CT.t[:, g_, js])], [BT, CT])
                    OP("dve", lambda E: E.tensor_tensor(CBm.t[:], g1.t[:].rearrange("p (g i) -> p g i", i=128),
                                                        triu.t[:].unsqueeze(1).to_broadcast([128, NG, 128]), ALU.mult), r=[g1, triu], w=[CBm])
                    if SD == 142:
                        return finish()
                    for g_ in range(NG):
                        gsl = slice(g_ * 512, (g_ + 1) * 512)
                        for half in range(2):
                            ga_ = G[2 + half]
                            for hh in range(4):
                                h = g_ * 8 + half * 4 + hh
                                MM(ga_, ga_.t[:, hh * 128:(hh + 1) * 128], [(dat.t[:, j, h:h + 1].to_broadcast([128, 128]), triu.t[:])], [dat, triu])
                            sg_ = seg.next()
                            h0 = g_ * 8 + half * 4
                            OP("dve", lambda E, ga_=ga_, sg_=sg_, h0=h0: E.tensor_tensor(sg_.t[:], ga_.t[:].rearrange("p (h i) -> p h i", i=128),
                                                                                       acum.t[:, h0:h0 + 4].unsqueeze(2).to_broadcast([128, 4, 128]), ALU.subtract),
                               r=[ga_, acum], w=[sg_])
                            OP("act", lambda E, sg_=sg_: E.activation(sg_.t[:], sg_.t[:], AF.Relu, scale=-1.0), r=[sg_], w=[sg_])
                            OP("act", lambda E, sg_=sg_: E.activation(sg_.t[:], sg_.t[:], AF.Exp, scale=-1.0), r=[sg_], w=[sg_])
                            OP("dve", lambda E, sg_=sg_, half=half, g_=g_: E.tensor_tensor(Sb.t[:, half * 4:half * 4 + 4, :], sg_.t[:],
                                                                                         CBm.t[:, g_:g_ + 1, :].to_broadcast([128, 4, 128]), ALU.mult),
                               r=[sg_, CBm], w=[Sb])
                        if SD == 143:
                            return finish()
                        g4 = G[4]
                        for h8 in range(8):
                            h = g_ * 8 + h8
                            MM(g4, g4.t[:, h8 * HP:(h8 + 1) * HP], [(Sb.t[:, h8, :], xdt.t[:, h * HP:(h + 1) * HP])], [Sb, xdt])
                        MM(g0, g0.t[:], [(CT.t[:, g_, js], stbf.t[:, gsl])], [CT, stbf])
                        y_ = yg.next()
                        OP("dve", lambda E, y_=y_, g_=g_: E.tensor_tensor(y_.t[:].rearrange("p (h q) -> p h q", q=HP), g0.t[:].rearrange("p (h q) -> p h q", q=HP),
                                                                         eacum.t[:, g_ * 8:(g_ + 1) * 8].unsqueeze(2).to_broadcast([128, 8, HP]), ALU.mult),
                           r=[g0, eacum], w=[y_])
                        OP("dve", lambda E, y_=y_: E.tensor_tensor(y_.t[:], g4.t[:], y_.t[:], ALU.add), r=[g4, y_], w=[y_])
                        t2 = yt2.next()
                        OP("dve", lambda E, t2=t2, g_=g_, gsl=gsl: E.tensor_tensor(t2.t[:].rearrange("p (h q) -> p h q", q=HP), xs_tok.t[:, gsl].rearrange("p (h q) -> p h q", q=HP),
                                                                                  dsk_bc.t[:, g_ * 8:(g_ + 1) * 8].unsqueeze(2).to_broadcast([128, 8, HP]), ALU.mult),
                           r=[xs_tok, dsk_bc], w=[t2])
                        OP("dve", lambda E, t2=t2, y_=y_: E.tensor_tensor(y_.t[:], y_.t[:], t2.t[:], ALU.add), r=[t2, y_], w=[y_])
                        if SD == 144:
                            return finish()
                        OP("dve", lambda E, y_=y_, j=j, gsl=gsl: E.tensor_tensor(y_.t[:], y_.t[:], zs.t[:, j, gsl], ALU.mult), r=[y_, zs], w=[y_])
                        OP("act", lambda E, y_=y_: E.activation(junk.t[:, 0:512], y_.t[:], AF.Square, accum_out=ssg.t[:, 0:1]), r=[y_], w=[junk, ssg])
                        OP("act", lambda E: E.activation(ssg.t[:, 0:1], ssg.t[:, 0:1], AF.Sqrt, bias=epst.t[:, 0:1], scale=1.0 / 512), r=[ssg, epst], w=[ssg])
                        OP("dve", lambda E: E.reciprocal(ssg.t[:, 1:2], ssg.t[:, 0:1]), r=[ssg], w=[ssg])
                        yn_ = yn.next()
                        OP("dve", lambda E, y_=y_, yn_=yn_, gsl=gsl: E.scalar_tensor_tensor(yn_.t[:], y_.t[:], ssg.t[:, 1:2], gssm_bc.t[:, gsl], ALU.mult, ALU.mult),
                           r=[y_, ssg, gssm_bc], w=[yn_])
                        for q in range(4):
                            TR(ptB, ptB.t[:, 512 + q * 128:512 + (q + 1) * 128], yn_.t[:, q * 128:(q + 1) * 128], identb, [yn_])
                        OP("act", lambda E, g_=g_, js=js: E.activation(ynT.t[:, g_ * 4:(g_ + 1) * 4, js], ptB.t[:, 512:1024].rearrange("p (q t) -> p q t", t=128), AF.Copy),
                           r=[ptB], w=[ynT])
                        if SD == 145:
                            return finish()
                        MM(g1, g1.t[:], [(B_tok.t[:, g_ * 128:(g_ + 1) * 128], xdte.t[:, gsl])], [B_tok, xdte])
                        OP("dve", lambda E, g_=g_, gsl=gsl: E.tensor_tensor(st32.t[:, gsl].rearrange("p (h q) -> p h q", q=HP), st32.t[:, gsl].rearrange("p (h q) -> p h q", q=HP),
                                                                           dchunk.t[:, g_ * 8:(g_ + 1) * 8].unsqueeze(2).to_broadcast([128, 8, HP]), ALU.mult),
                           r=[st32, dchunk], w=[st32])
                        OP("dve", lambda E, gsl=gsl: E.tensor_tensor(st32.t[:, gsl], g1.t[:], st32.t[:, gsl], ALU.add), r=[g1, st32], w=[st32])
                        OP("act", lambda E, gsl=gsl: E.activation(stbf.t[:, gsl], st32.t[:, gsl], AF.Copy), r=[st32], w=[stbf])
                        if SD == 146:
                            return finish()
                        if SD == 147 and g_ == 1:
                            return finish()
                    if SD == 148:
                        return finish()

                if STOP == 14:
                    return finish()
                issue_casts(casts_per_tile)
                for dq in range(2):
                    w_a = wload(wsco_r, B_wsco, dq * 512)
                    w_ga = wload(win_r, B_win, C_GA + dq * 512)
                    for dcc in range(4):
                        g_y = Grot.next()
                        MM(g_y, g_y.t[:, 0:T], [(w_a.t[:, kc, dcc * 128:(dcc + 1) * 128], vT.t[:, kc, :]) for kc in range(KC)], [w_a, vT])
                        g_g = proj_fm(w_ga, dcc)
                        ta = tmpA.next()
                        OP("act", lambda E, ta=ta, g=g_g: E.activation(ta.t[:], g.t[:, 0:T], AF.Sigmoid), r=[g_g], w=[ta])
                        OP("dve", lambda E, ta=ta, g=g_y, dcc=dcc: E.tensor_tensor(mpart.t[:, dcc, :], g.t[:, 0:T], ta.t[:], ALU.mult), r=[g_y, ta], w=[mpart])
                    w_b0 = wload(wsso_r, B_wsso, dq * 512, 0)
                    w_b1 = wload(wsso_r, B_wsso, dq * 512, 8)
                    w_gb = wload(win_r, B_win, C_GB + dq * 512)
                    for dcc in range(4):
                        g_y = Grot.next()
                        MM(g_y, g_y.t[:, 0:T], [(w_b0.t[:, kc, dcc * 128:(dcc + 1) * 128], ynT.t[:, kc, :]) for kc in range(KC)] +
                           [(w_b1.t[:, kc, dcc * 128:(dcc + 1) * 128], ynT.t[:, 8 + kc, :]) for kc in range(KC)], [w_b0, w_b1, ynT])
                        g_g = proj_fm(w_gb, dcc)
                        ta = tmpA.next()
                        OP("act", lambda E, ta=ta, g=g_g: E.activation(ta.t[:], g.t[:, 0:T], AF.Sigmoid), r=[g_g], w=[ta])
                        OP("dve", lambda E, ta=ta, g=g_y: E.tensor_tensor(ta.t[:], g.t[:, 0:T], ta.t[:], ALU.mult), r=[g_y, ta], w=[ta])
                        OP("pool", lambda E, ta=ta, dq=dq, dcc=dcc: E.tensor_tensor(mT.t[:, dq * 4 + dcc, :], ta.t[:], mpart.t[:, dcc, :], ALU.add),
                           r=[ta, mpart], w=[mT])

                if STOP == 15:
                    return finish()
                x1b = [xr.next(), xr.next()]
                for j in range(2):
                    LOAD("sp", x1b[j], x1b[j].t[:], x_d[r0 + j * 128:r0 + (j + 1) * 128, :], reads=[])
                for dh in range(2):
                    w_o_ = wload(wo_r, B_wo, dh * 512)
                    dsl = slice(dh * 512, (dh + 1) * 512)
                    for j in range(2):
                        g = Grot.next()
                        MM(g, g.t[:], [(mT.t[:, kc, j * 128:(j + 1) * 128], w_o_.t[:, kc, :]) for kc in range(KC)], [w_o_, mT])
                        t2 = yt2.next()
                        OP("dve", lambda E, g=g, t2=t2, dsl=dsl: E.tensor_tensor(t2.t[:], g.t[:], bct.t[:, GT1 + dsl.start:GT1 + dsl.stop], ALU.mult), r=[g, bct], w=[t2])
                        OP("pool", lambda E, t2=t2, xb_=x1b[j], dsl=dsl: E.tensor_tensor(xb_.t[:, dsl], xb_.t[:, dsl], t2.t[:], ALU.add), r=[t2, x1b[j]], w=[x1b[j]])
                for j in range(2):
                    tt = (r0 // 128) + j
                    xb = x1b[j]
                    S.dma("sp", lambda E, xb=xb, tt=tt: E.dma_start(out=x1_h[tt * 128:(tt + 1) * 128, :], in_=xb.t[:]), xb, reads=[xb], writes=[B_x1])
                    OP("act", lambda E, xb=xb: E.activation(junk.t[:], xb.t[:], AF.Square, accum_out=ss.t[:, 2:3]), r=[xb], w=[junk, ss])
                    OP("act", lambda E: E.activation(ss.t[:, 2:3], ss.t[:, 2:3], AF.Sqrt, bias=epst.t[:, 0:1], scale=1.0 / D), r=[ss, epst], w=[ss])
                    OP("dve", lambda E: E.reciprocal(rstd.t[:, 2:3], ss.t[:, 2:3]), r=[ss], w=[rstd])
                    OP("dve", lambda E, xb=xb: E.scalar_tensor_tensor(h2.t[:], xb.t[:], rstd.t[:, 2:3], bct.t[:, GS2:GS2 + D], ALU.mult, ALU.mult),
                       r=[xb, rstd, bct], w=[h2])
                    OP("pool", lambda E: E.tensor_tensor(h2.t[:], h2.t[:], bct.t[:, SH2:SH2 + D], ALU.add), r=[h2, bct], w=[h2])
                    hb = h2b.next()
                    OP("act", lambda E, hb=hb: E.activation(hb.t[:], h2.t[:], AF.Copy), r=[h2], w=[hb])
                    S.dma("sp", lambda E, hb=hb, tt=tt: E.dma_start(out=h2_h[tt * 128:(tt + 1) * 128, :], in_=hb.t[:]), hb, reads=[hb], writes=[B_h2])
                    for half in range(2):
                        g = Grot.next()
                        for c4 in range(4):
                            c = half * 4 + c4
                            TR(g, g.t[:, c4 * 128:(c4 + 1) * 128], h2.t[:, c * 128:(c + 1) * 128], ident, [h2])
                        OP("act", lambda E, g=g, half=half: E.activation(h2T.t[:, half * 4:half * 4 + 4, :], g.t[:].rearrange("p (c t) -> p c t", t=128), AF.Copy),
                           r=[g], w=[h2T])
                    g = Grot.next()
                    MM(g, g.t[:, 0:NE], [(h2T.t[:, kc, :], wr_sb.t[:, kc, :]) for kc in range(KC)], [h2T, wr_sb])
                    OP("dve", lambda E, g=g, tt=tt: E.tensor_tensor(logits.t[:, tt * NE:(tt + 1) * NE], g.t[:, 0:NE], br_bc.t[:], ALU.add), r=[g, br_bc], w=[logits])

        issue_casts(len(cast_jobs))
        issue_zero(len(zero_jobs))
        if STOP == 1:
            return finish()
        S.barrier()
        p1.close()
        open_stacks.remove(p1)

        p2a = ExitStack()
        open_stacks.append(p2a)
        top8 = S.sbuf("top8", [128, 8], F32, p2a)
        topi = S.sbuf("topi", [128, 8], U32, p2a)
        idxf = S.sbuf("idxf", [128, NTT * TOPK], F32, p2a)
        negm = S.sbuf("negm", [128, 1], F32, p2a)
        esum = S.sbuf("esum", [128, 2], F32, p2a)
        Mb = S.sbuf("Mb", [128, NE], BF16, p2a)
        rank = S.sbuf("rank", [128, NTT * NE], F32, p2a)
        runc = S.sbuf("runc", [128, NE], F32, p2a)
        ca = S.sbuf("ca", [128, NE], F32, p2a)
        cb = S.sbuf("cb", [128, NE], F32, p2a)
        ci = S.sbuf("ci", [128, NE], I32, p2a)
        padded = S.sbuf("padded", [128, NE], F32, p2a)
        pstart = S.sbuf("pstart", [128, NE], F32, p2a)
        base = S.sbuf("base", [128, NE], F32, p2a)
        tmpe = S.sbuf("tmpe", [128, NE], F32, p2a)
        destf = S.sbuf("destf", [128, NTT * TOPK], F32, p2a)
        bstart = S.sbuf("bstart", [128, NBLK], F32, p2a)
        be = S.sbuf("be", [128, NBLK], F32, p2a)
        pio = S.sbuf("pio", [128, KC], F32, p2a)
        wrow_f = S.sbuf("wrow_f", [128, NBLK, KC], F32, p2a)
        brow_f = S.sbuf("brow_f", [128, NBLK], F32, p2a)

        OP("dve", lambda E: E.memset(runc.t[:], 0.0), w=[runc])
        for tt in range(NTT):
            lg = logits.t[:, tt * NE:(tt + 1) * NE]
            OP("dve", lambda E, lg=lg: E.max(top8.t[:], lg), r=[logits], w=[top8])
            OP("dve", lambda E, lg=lg: E.max_index(topi.t[:], top8.t[:], lg), r=[logits, top8], w=[topi])
            OP("dve", lambda E, tt=tt: E.tensor_copy(idxf.t[:, tt * 4:tt * 4 + 4], topi.t[:, 0:4]), r=[topi], w=[idxf])
            OP("dve", lambda E: E.tensor_single_scalar(negm.t[:], top8.t[:, 0:1], -1.0, ALU.mult), r=[top8], w=[negm])
            OP("act", lambda E, tt=tt: E.activation(wts.t[:, tt * 4:tt * 4 + 4], top8.t[:, 0:4], AF.Exp, bias=negm.t[:, 0:1], scale=1.0,
                                                   accum_out=esum.t[:, 0:1]), r=[top8, negm], w=[wts, esum])
            OP("dve", lambda E: E.reciprocal(esum.t[:, 1:2], esum.t[:, 0:1]), r=[esum], w=[esum])
            OP("dve", lambda E, tt=tt: E.tensor_scalar(wts.t[:, tt * 4:tt * 4 + 4], wts.t[:, tt * 4:tt * 4 + 4], esum.t[:, 1:2], None, ALU.mult),
               r=[wts, esum], w=[wts])
            OP("dve", lambda E, tt=tt: E.tensor_scalar(Mb.t[:], iota32.t[:], idxf.t[:, tt * 4:tt * 4 + 1], None, ALU.is_equal), r=[iota32, idxf], w=[Mb])
            for k in range(1, 4):
                OP("dve", lambda E, tt=tt, k=k: E.scalar_tensor_tensor(Mb.t[:], iota32.t[:], idxf.t[:, tt * 4 + k:tt * 4 + k + 1], Mb.t[:], ALU.is_equal, ALU.add),
                   r=[iota32, idxf, Mb], w=[Mb])
            g = Grot.next()
            MM(g, g.t[:, 0:NE], [(tristb.t[:], Mb.t[:])], [tristb, Mb])
            MM(g, g.t[:, NE:2 * NE], [(onesb.t[:], Mb.t[:])], [onesb, Mb])
            OP("dve", lambda E, g=g, tt=tt: E.tensor_tensor(rank.t[:, tt * NE:(tt + 1) * NE], g.t[:, 0:NE], runc.t[:], ALU.add), r=[g, runc], w=[rank])
            OP("dve", lambda E, g=g: E.tensor_tensor(runc.t[:], g.t[:, NE:2 * NE], runc.t[:], ALU.add), r=[g, runc], w=[runc])
        sh = BLK.bit_length() - 1
        OP("dve", lambda E: E.tensor_copy(ci.t[:], runc.t[:]), r=[runc], w=[ci])
        OP("dve", lambda E: E.tensor_single_scalar(ci.t[:], ci.t[:], BLK - 1, ALU.add), r=[ci], w=[ci])
        OP("dve", lambda E: E.tensor_single_scalar(ci.t[:], ci.t[:], sh, ALU.arith_shift_right), r=[ci], w=[ci])
        OP("dve", lambda E: E.tensor_single_scalar(ci.t[:], ci.t[:], sh, ALU.logical_shift_left), r=[ci], w=[ci])
        OP("dve", lambda E: E.tensor_copy(padded.t[:], ci.t[:]), r=[ci], w=[padded])
        OP("dve", lambda E: E.tensor_copy(ca.t[:], padded.t[:]), r=[padded], w=[ca])
        src, dst = ca, cb
        s_ = 1
        while s_ < NE:
            OP("dve", lambda E, src=src, dst=dst, s_=s_: E.tensor_copy(dst.t[:, 0:s_], src.t[:, 0:s_]), r=[src], w=[dst])
            OP("dve", lambda E, src=src, dst=dst, s_=s_: E.tensor_tensor(dst.t[:, s_:NE], src.t[:, s_:NE], src.t[:, 0:NE - s_], ALU.add), r=[src], w=[dst])
            src, dst = dst, src
            s_ *= 2
        pend = src
        OP("dve", lambda E: E.tensor_tensor(pstart.t[:], pend.t[:], padded.t[:], ALU.subtract), r=[pend, padded], w=[pstart])
        for tt in range(NTT):
            OP("dve", lambda E, tt=tt: E.tensor_tensor(base.t[:], rank.t[:, tt * NE:(tt + 1) * NE], pstart.t[:], ALU.add), r=[rank, pstart], w=[base])
            for k in range(4):
                col = tt * 4 + k
                OP("dve", lambda E, col=col: E.scalar_tensor_tensor(tmpe.t[:], iota32.t[:], idxf.t[:, col:col + 1], base.t[:], ALU.is_equal, ALU.mult),
                   r=[iota32, idxf, base], w=[tmpe])
                OP("dve", lambda E, col=col: E.reduce_sum(destf.t[:, col:col + 1], tmpe.t[:], axis=AX.X), r=[tmpe], w=[destf])
        OP("dve", lambda E: E.tensor_copy(dest_i.t[:], destf.t[:]), r=[destf], w=[dest_i])
        OP("pool", lambda E: E.iota(bstart.t[:], pattern=[[BLK, NBLK]], base=0, channel_multiplier=0, allow_small_or_imprecise_dtypes=True), w=[bstart])
        OP("pool", lambda E: E.iota(pio.t[:], pattern=[[128, KC]], base=0, channel_multiplier=1, allow_small_or_imprecise_dtypes=True), w=[pio])
        OP("dve", lambda E: E.memset(be.t[:], 0.0), w=[be])
        for e in range(NE):
            OP("dve", lambda E, e=e: E.scalar_tensor_tensor(be.t[:], bstart.t[:], pend.t[:, e:e + 1], be.t[:], ALU.is_ge, ALU.add), r=[bstart, pend, be], w=[be])
        OP("dve", lambda E: E.tensor_single_scalar(be.t[:], be.t[:], float(NE - 1), ALU.min), r=[be], w=[be])
        for kc in range(KC):
            OP("dve", lambda E, kc=kc: E.tensor_scalar(wrow_f.t[:, :, kc], be.t[:], float(D), pio.t[:, kc:kc + 1], ALU.mult, ALU.add), r=[be, pio], w=[wrow_f])
        OP("dve", lambda E: E.tensor_copy(wrow.t[:], wrow_f.t[:].rearrange("p b k -> p (b k)")), r=[wrow_f], w=[wrow])
        OP("dve", lambda E: E.tensor_scalar(brow_f.t[:], be.t[:], 128.0, pio.t[:, 0:1], ALU.mult, ALU.add), r=[be, pio], w=[brow_f])
        OP("dve", lambda E: E.tensor_copy(brow.t[:], brow_f.t[:]), r=[brow_f], w=[brow])
        OP("dve", lambda E: E.tensor_copy(erow.t[:], be.t[:]), r=[be], w=[erow])

        if STOP == 2:
            return finish()
        S.barrier()
        p2a.close()
        plog.close()
        open_stacks.remove(p2a)
        open_stacks.remove(plog)
        p3 = ExitStack()
        open_stacks.append(p3)
        hrows = Rot([S.sbuf(f"hrows{i}", [128, D], BF16, p3) for i in range(6)])
        for tt in range(NTT):
            hb = hrows.next()
            LOAD("sp", hb, hb.t[:], h2_h[tt * 128:(tt + 1) * 128, :], reads=[B_h2])
            for k in range(4):
                col = tt * 4 + k
                S.dma("pool", lambda E, hb=hb, col=col: E.indirect_dma_start(
                    out=xs_h, out_offset=bass.IndirectOffsetOnAxis(ap=dest_i.t[:, col:col + 1], axis=0),
                    in_=hb.t[:, :], in_offset=None), B_xs, reads=[hb, dest_i, B_xz], writes=[])
        B_xs.w = ("d", B_xs)

        if STOP == 3:
            return finish()
        wunit = Rot([S.sbuf(f"wunit{i}", [128, KC, D], BF16, p3) for i in range(6)])
        bgu_sb = Rot([S.sbuf(f"bgu_sb{i}", [128, 16], F32, p3) for i in range(2)])
        bd_sb = Rot([S.sbuf(f"bd_sb{i}", [128, D], F32, p3) for i in range(2)])
        xrows = Rot([S.sbuf(f"xrows{i}", [128, 4, D], BF16, p3) for i in range(2)])
        gcl = Rot([S.sbuf(f"gcl{i}", [128, BLK], F32, p3) for i in range(2)])
        sgm = Rot([S.sbuf(f"sgm{i}", [128, BLK], F32, p3) for i in range(2)])
        upc = Rot([S.sbuf(f"upc{i}", [128, BLK], F32, p3) for i in range(2)])
        ysb = Rot([S.sbuf(f"ysb{i}", [128, D], F32, p3) for i in range(3)])
        xTs = Rot([S.sbuf(f"xT{i}", [128, KC, BLK], BF16, p3) for i in range(2)])
        actTs = Rot([S.sbuf(f"actT{i}", [128, KC, BLK], BF16, p3) for i in range(2)])

        def blk_loads(blk):
            wgg = wunit.next()
            wgup = wunit.next()
            wdn = wunit.next()
            bg = bgu_sb.next()
            bd = bd_sb.next()
            for (wdst, wsrc, bb) in ((wgg, wg_bf, B_wgu), (wgup, wu_bf, B_wgu), (wdn, wd_bf, B_wd)):
                S.dma("pool", lambda E, wdst=wdst, wsrc=wsrc, blk=blk: E.indirect_dma_start(
                    out=wdst.t[:].rearrange("p k n -> p (k n)"), out_offset=None, in_=wsrc,
                    in_offset=bass.IndirectOffsetOnAxis(ap=brow.t[:, blk:blk + 1], axis=0)), wdst, reads=[bb, brow], writes=[wdst])
            S.dma("pool", lambda E, bg=bg, blk=blk: E.indirect_dma_start(
                out=bg.t[:, :], out_offset=None, in_=bgu_l_d,
                in_offset=bass.IndirectOffsetOnAxis(ap=brow.t[:, blk:blk + 1], axis=0)), bg, reads=[brow], writes=[bg])
            S.dma("pool", lambda E, bd=bd, blk=blk: E.indirect_dma_start(
                out=bd.t[:, :], out_offset=None, in_=bd_d,
                in_offset=bass.IndirectOffsetOnAxis(ap=erow.t[:, blk:blk + 1], axis=0)), bd, reads=[erow], writes=[bd])
            xw = xrows.next()
            LOAD("sp", xw, xw.t[:], xs_h[blk * BLK:(blk + 1) * BLK, :].rearrange("(r p) d -> p r d", p=128), reads=[B_xs])
            return dict(wgg=wgg, wgup=wgup, wdn=wdn, bg=bg, bd=bd, xw=xw)

        def blk_transposes(L):
            xw = L["xw"]
            xT = xTs.next()
            for half in range(2):
                for c4 in range(4):
                    kc = half * 4 + c4
                    for r in range(4):
                        TR(ptb, ptb.t[:, c4 * 512 + r * 128:c4 * 512 + (r + 1) * 128], xw.t[:, r, kc * 128:(kc + 1) * 128], identb, [xw])
                OP("act", lambda E, half=half, xT=xT: E.activation(xT.t[:, half * 4:half * 4 + 4, :], ptb.t[:].rearrange("p (c t) -> p c t", t=BLK), AF.Copy),
                   r=[ptb], w=[xT])
            L["xT"] = xT

        def blk_gu(L):
            wgg, wgup, bg, xT = L["wgg"], L["wgup"], L["bg"], L["xT"]
            actT = actTs.next()
            L["actT"] = actT
            for f in range(KC):
                g_g = Grot.next()
                MM(g_g, g_g.t[:], [(wgg.t[:, kc, f * 128:(f + 1) * 128], xT.t[:, kc, :]) for kc in range(KC)], [wgg, xT])
                g_u = Grot.next()
                MM(g_u, g_u.t[:], [(wgup.t[:, kc, f * 128:(f + 1) * 128], xT.t[:, kc, :]) for kc in range(KC)], [wgup, xT])
                gc_, sg_, up_ = gcl.next(), sgm.next(), upc.next()
                OP("dve", lambda E, g=g_g, gc_=gc_, f=f, bg=bg: E.tensor_scalar(gc_.t[:], g.t[:], bg.t[:, f:f + 1], 7.0, ALU.add, ALU.min), r=[g_g, bg], w=[gc_])
                OP("act", lambda E, gc_=gc_, sg_=sg_: E.activation(sg_.t[:], gc_.t[:], AF.Sigmoid, scale=1.702), r=[gc_], w=[sg_])
                OP("dve", lambda E, g=g_u, up_=up_, f=f, bg=bg: E.tensor_scalar(up_.t[:], g.t[:], bg.t[:, 8 + f:9 + f], 7.0, ALU.add, ALU.min), r=[g_u, bg], w=[up_])
                OP("pool", lambda E, up_=up_: E.tensor_scalar(up_.t[:], up_.t[:], -7.0, 1.0, ALU.max, ALU.add), r=[up_], w=[up_])
                OP("pool", lambda E, gc_=gc_, sg_=sg_: E.tensor_tensor(gc_.t[:], gc_.t[:], sg_.t[:], ALU.mult), r=[gc_, sg_], w=[gc_])
                OP("dve", lambda E, gc_=gc_, up_=up_, f=f, actT=actT: E.tensor_tensor(actT.t[:, f, :], gc_.t[:], up_.t[:], ALU.mult), r=[gc_, up_], w=[actT])

        def blk_down(L, blk):
            wdn, bd, actT = L["wdn"], L["bd"], L["actT"]
            for r in range(4):
                yb = ysb.next()
                for dh in range(2):
                    g = Grot.next()
                    MM(g, g.t[:], [(actT.t[:, fc, r * 128:(r + 1) * 128], wdn.t[:, fc, dh * 512:(dh + 1) * 512]) for fc in range(KC)], [actT, wdn])
                    OP("dve", lambda E, g=g, yb=yb, dh=dh, bd=bd: E.tensor_tensor(yb.t[:, dh * 512:(dh + 1) * 512], g.t[:], bd.t[:, dh * 512:(dh + 1) * 512], ALU.add),
                       r=[g, bd], w=[yb])
                row0 = blk * BLK + r * 128
                S.dma("sp", lambda E, yb=yb, row0=row0: E.dma_start(out=ys_h[row0:row0 + 128, :], in_=yb.t[:]), yb, reads=[yb], writes=[])

        cur = blk_loads(0)
        blk_transposes(cur)
        for blk in range(NBLK):
            nxt = blk_loads(blk + 1) if blk + 1 < NBLK else None
            blk_gu(cur)
            if nxt is not None:
                blk_transposes(nxt)
            blk_down(cur, blk)
            cur = nxt

        if STOP == 4:
            return finish()
        S.barrier()
        p3.close()
        open_stacks.remove(p3)

        p5 = ExitStack()
        open_stacks.append(p5)
        gfin_bc = S.sbuf("gfin_bc", [128, D], F32, p5)
        LOAD("sp", gfin_bc, gfin_bc.t[:], gfin_bc_d)
        x1r = Rot([S.sbuf(f"x1r{i}", [128, D], F32, p5) for i in range(4)])
        ygat = Rot([S.sbuf(f"ygat{i}", [128, D], F32, p5) for i in range(12)])
        accb = Rot([S.sbuf(f"accb{i}", [128, D], F32, p5) for i in range(4)])
        junk5 = S.sbuf("junk5", [128, D], BF16, p5)
        bct5 = S.sbuf("bct5", [128, D], F32, p5)
        M.update({"wadab": Rot([S.sbuf(f"wadab5{i}", [128, KC, 128], F32, p5) for i in range(2)]),
                  "badab": Rot([S.sbuf(f"badab5{i}", [128, 128], F32, p5) for i in range(2)]),
                  "condrep": S.sbuf("condrep5", [128, KC, 128], F32, p5), "bct": bct5, "off": 24})
        ss5 = S.sbuf("ss5", [128, 2], F32, p5)
        for b in range(NSEQ):
            mod_bc(b, range(24, 32))
            for it in range(SEQ // 128):
                tt = b * (SEQ // 128) + it
                xb = x1r.next()
                LOAD("sp", xb, xb.t[:], x1_h[tt * 128:(tt + 1) * 128, :], reads=[B_x1])
                acc = accb.next()
                for k in range(4):
                    col = tt * 4 + k
                    yk = ygat.next()
                    S.dma("pool", lambda E, yk=yk, col=col: E.indirect_dma_start(
                        out=yk.t[:, :], out_offset=None, in_=ys_h,
                        in_offset=bass.IndirectOffsetOnAxis(ap=dest_i.t[:, col:col + 1], axis=0)), yk, reads=[B_ys, dest_i], writes=[yk])
                    if k == 0:
                        OP("dve", lambda E, yk=yk, acc=acc, col=col: E.tensor_scalar(acc.t[:], yk.t[:], wts.t[:, col:col + 1], None, ALU.mult), r=[yk, wts], w=[acc])
                    else:
                        eng = "dve"
                        OP(eng, lambda E, yk=yk, acc=acc, col=col: E.scalar_tensor_tensor(acc.t[:], yk.t[:], wts.t[:, col:col + 1], acc.t[:], ALU.mult, ALU.add),
                           r=[yk, wts, acc], w=[acc])
                OP("dve", lambda E, acc=acc: E.tensor_tensor(acc.t[:], acc.t[:], bct5.t[:, GT2:GT2 + D], ALU.mult), r=[acc, bct5], w=[acc])
                OP("pool", lambda E, acc=acc, xb=xb: E.tensor_tensor(acc.t[:], acc.t[:], xb.t[:], ALU.add), r=[acc, xb], w=[acc])
                OP("act", lambda E, acc=acc: E.activation(junk5.t[:], acc.t[:], AF.Square, accum_out=ss5.t[:, 0:1]), r=[acc], w=[junk5, ss5])
                OP("act", lambda E: E.activation(ss5.t[:, 0:1], ss5.t[:, 0:1], AF.Sqrt, bias=epst.t[:, 0:1], scale=1.0 / D), r=[ss5, epst], w=[ss5])
                OP("dve", lambda E: E.reciprocal(ss5.t[:, 1:2], ss5.t[:, 0:1]), r=[ss5], w=[ss5])
                OP("dve", lambda E, acc=acc: E.scalar_tensor_tensor(acc.t[:], acc.t[:], ss5.t[:, 1:2], gfin_bc.t[:], ALU.mult, ALU.mult), r=[acc, ss5, gfin_bc], w=[acc])
                S.dma("sp", lambda E, acc=acc, tt=tt: E.dma_start(out=out_d[tt * 128:(tt + 1) * 128, :], in_=acc.t[:]), acc, reads=[acc], writes=[])
        return finish()
    return nc


def _prep_shared(inp):
    f = lambda a: np.ascontiguousarray(np.asarray(a, dtype=np.float32))
    rep = lambda v: f(np.broadcast_to(np.asarray(v, np.float32).reshape(1, -1), (128, np.asarray(v).size)))
    fm = lambda v: f(np.asarray(v, np.float32).reshape(-1, 128).T)
    l = 0
    b_ada = np.asarray(inp["b_ada"][l], np.float32)
    d = {
        "w_ada": f(inp["w_ada"][l]),
        "b_ada_fm": fm(b_ada[:2 * D]),
        "b_ada_bc": rep(b_ada[2 * D:]),
        "g_mix_fm": fm(inp["g_mix"][l]),
        "g_ffn_bc": rep(inp["g_ffn"][l]),
        "g_final_bc": rep(inp["g_final"]),
        "w_in": f(inp["w_in"][l]),
        "w_sconv_fm": f(np.asarray(inp["w_sconv"][l], np.float32).reshape(3, KC, 128).transpose(2, 1, 0).reshape(128, KC * 3)),
        "w_sconv_out": f(inp["w_sconv_out"][l]),
        "w_ssm_conv_fm": f(np.asarray(inp["w_ssm_conv"][l], np.float32).reshape(4, 24, 128).transpose(2, 1, 0).reshape(128, 96)),
        "b_ssm_conv_fm": fm(inp["b_ssm_conv"][l]),
        "dt_bias_bc": rep(inp["dt_bias"][l]),
        "a_log_bc": rep(inp["a_log"][l]),
        "d_skip_bc": rep(inp["d_skip"][l]),
        "g_ssm_bc": rep(inp["g_ssm_norm"][l]),
        "w_ssm_out": f(inp["w_ssm_out"][l]),
        "w_o": f(inp["w_o"][l]),
        "w_router": f(inp["w_router"][l]),
        "b_router_bc": rep(inp["b_router"][l]),
        "w_gu": f(np.asarray(inp["w_gu"][l], np.float32).reshape(NE * D, 2 * D)),
        "b_gu_l": f(np.asarray(inp["b_gu"][l], np.float32).reshape(NE, 16, 128).transpose(0, 2, 1).reshape(NE * 128, 16)),
        "w_down": f(np.asarray(inp["w_down"][l], np.float32).reshape(NE * D, D)),
        "b_down": f(inp["b_down"][l]),
    }
    return d


_NC_CACHE = {}
_STOP = 99
_JLIST = (0, 1)


def kernel(**inputs):
    x = np.asarray(inputs["x"], np.float32)
    c = np.asarray(inputs["c"], np.float32)
    bsz, seq, _ = x.shape
    ncores = 8
    nseq = bsz // ncores
    key = (nseq, seq)
    if key not in _NC_CACHE:
        _NC_CACHE[key] = build_nc(nseq, seq, _STOP)
    nc = _NC_CACHE[key]
    shared = _prep_shared(inputs)
    in_maps = []
    for i in range(ncores):
        m = dict(shared)
        m["x"] = np.ascontiguousarray(x[i * nseq:(i + 1) * nseq].reshape(nseq * seq, D))
        cc = c[i * nseq:(i + 1) * nseq]
        m["cT"] = np.ascontiguousarray(cc.reshape(nseq, KC, 128).transpose(2, 1, 0).reshape(128, KC * nseq))
        in_maps.append(m)
    res = run_bass_kernel_spmd(nc, in_maps, core_ids=list(range(ncores)))
    out = np.concatenate([np.asarray(r["out"]).reshape(nseq, seq, D) for r in res.results], axis=0)
    return out.astype(np.float32)
```

```python
import numpy as np
from contextlib import ExitStack
import concourse.bass as bass
import concourse.mybir as mybir
from concourse.bass_utils import run_bass_kernel_spmd

F32 = mybir.dt.float32
BF16 = mybir.dt.bfloat16
I32 = mybir.dt.int32
U32 = mybir.dt.uint32
ALU = mybir.AluOpType
AF = mybir.ActivationFunctionType
AX = mybir.AxisListType

ENGS = ("pe", "act", "dve", "pool", "sp")
SAME_ENGINE_SYNC = ("act", "dve", "pool")
SEM_EPOCH = 30000
LAZY = ("pe",)

D = 1024
KC = 8
DI = 2048
NH = 32
HP = 64
NG = 4
NS = 128
NCOL = 10272
C_SB, C_SC, C_SX, C_Z, C_XBC, C_DT, C_GA, C_GB = 0, 1024, 2048, 3072, 5120, 8192, 8224, 9248
NE = 32
TOPK = 4
BLK = 512
T = 256
EPS = 1e-6


class Buf:
    __slots__ = ("t", "w", "r", "sem", "semcnt", "name")

    def __init__(self, t, name=""):
        self.t = t
        self.w = None
        self.r = []
        self.sem = None
        self.semcnt = 0
        self.name = name


class Rot:
    def __init__(self, bufs):
        self.bufs = bufs
        self.i = 0

    def next(self):
        b = self.bufs[self.i % len(self.bufs)]
        self.i += 1
        return b


class Sched:
    def __init__(self, nc, stack):
        self.nc = nc
        self.stack = stack
        self.q = {e: [] for e in ENGS}
        self.nsem = 0
        self.dma_bufs = []
        self.esem = {}
        self.ecnt = {}
        self.allsems = {e: [] for e in ENGS}
        self.lastcell = {}
        for e in ENGS:
            self._new_epoch(e)
        self.waited = {e: {} for e in ENGS}

    def _new_sem(self, name):
        self.nsem += 1
        return self.stack.enter_context(self.nc.semaphore(f"{name}_{self.nsem}"))

    def _new_epoch(self, e):
        self.esem[e] = self._new_sem("e" + e)
        self.ecnt[e] = 0
        self.allsems[e].append(self.esem[e])

    def sbuf(self, name, shape, dt, stack=None):
        st = stack or self.stack
        return Buf(st.enter_context(self.nc.sbuf_tensor("s_" + name, list(shape), dt)), name)

    def psum(self, name, shape, dt):
        return Buf(self.stack.enter_context(self.nc.psum_tensor("p_" + name, list(shape), dt)), name)

    def _flush(self, e):
        cell = self.lastcell.get(e)
        if cell is None or cell["inc"]:
            return
        cell["inc"] = True
        self.ecnt[e] += 1
        self.lastcell[e] = None
        if self.ecnt[e] >= SEM_EPOCH:
            self._new_epoch(e)

    def _collect(self, eng, reads, writes):
        waits = {}

        def need(ev):
            if ev is None:
                return
            if ev[0] == "e":
                _, e2, sem, c = ev
                if e2 == eng and eng not in SAME_ENGINE_SYNC:
                    return
                if sem is self.esem[e2] and c > self.ecnt[e2]:
                    self._flush(e2)
                val = c
            else:
                sem = ev[1].sem
                val = ev[1].semcnt
            key = id(sem)
            if key not in waits or waits[key][1] < val:
                waits[key] = (sem, val)

        for b in reads:
            need(b.w)
        for b in writes:
            need(b.w)
            for ev in b.r:
                need(ev)
        out = []
        wd = self.waited[eng]
        for key, (sem, val) in waits.items():
            if wd.get(key, 0) >= val:
                continue
            wd[key] = val
            out.append((sem, val))
        return out

    def op(self, eng, fn, reads=(), writes=(), inc=True):
        waits = self._collect(eng, reads, writes)
        sem = self.esem[eng]
        cnt = self.ecnt[eng] + 1
        if eng in LAZY:
            cell = {"inc": False}
            self.lastcell[eng] = cell
        else:
            cell = {"inc": True}
            self.ecnt[eng] = cnt
            if cnt >= SEM_EPOCH:
                self._new_epoch(eng)

        def rec(E, waits=waits, fn=fn, sem=sem, cell=cell):
            for s, v in waits:
                E.wait_ge(s, v)
            ins = fn(E)
            if cell["inc"]:
                ins.then_inc(sem, 1)

        self.q[eng].append(rec)
        ev = ("e", eng, sem, cnt)
        for b in writes:
            b.w = ev
            b.r = []
        for b in reads:
            b.r.append(ev)

    def dma(self, eng, fn, owner, reads=(), writes=()):
        waits = self._collect(eng, reads, writes)
        if owner.sem is None:
            owner.sem = self._new_sem("d" + owner.name)
            self.dma_bufs.append(owner)
        owner.semcnt += 16
        sem = owner.sem

        def rec(E, waits=waits, fn=fn, sem=sem):
            for s, v in waits:
                E.wait_ge(s, v)
            fn(E).then_inc(sem, 16)

        self.q[eng].append(rec)
        ev = ("d", owner)
        for b in writes:
            b.w = ev
            b.r = []
        for b in reads:
            b.r.append(ev)

    def barrier(self):
        for e in ENGS:
            self._flush(e)
        for eng in ENGS:
            waits = []
            wd = self.waited[eng]
            for e2 in ENGS:
                if e2 == eng or self.ecnt[e2] == 0:
                    continue
                sem, val = self.esem[e2], self.ecnt[e2]
                if wd.get(id(sem), 0) < val:
                    wd[id(sem)] = val
                    waits.append((sem, val))
            for ob in self.dma_bufs:
                if wd.get(id(ob.sem), 0) < ob.semcnt:
                    wd[id(ob.sem)] = ob.semcnt
                    waits.append((ob.sem, ob.semcnt))

            def rec(E, waits=waits):
                for s, v in waits:
                    E.wait_ge(s, v)

            self.q[eng].append(rec)

    def emit(self):
        q = self.q
        with self.nc.Block() as block:
            @block.sync
            def _(E):
                for r in q["sp"]:
                    r(E)

            @block.tensor
            def _(E):
                for r in q["pe"]:
                    r(E)

            @block.scalar
            def _(E):
                for r in q["act"]:
                    r(E)

            @block.vector
            def _(E):
                for r in q["dve"]:
                    r(E)

            @block.gpsimd
            def _(E):
                for r in q["pool"]:
                    r(E)


def build_nc(NSEQ, SEQ, STOP=99):
    nc = bass.Bass("TRN2", target_bir_lowering=False)
    NT = NSEQ * SEQ
    NTT = NT // 128
    NTL = SEQ // T
    NROWS = NT * TOPK
    NBLK = NROWS // BLK + NE
    RTOT = NBLK * BLK

    def din(name, shape, dt=F32):
        return nc.dram_tensor(name, list(shape), dt, kind="ExternalInput").ap()

    def dscr(name, shape, dt):
        return nc.dram_tensor(name, list(shape), dt, kind="Internal").ap()

    x_d = din("x", [NT, D])
    cT_d = din("cT", [128, KC * NSEQ])
    wada_d = din("w_ada", [D, 6 * D])
    bada_fm_d = din("b_ada_fm", [128, 16])
    bada_bc_d = din("b_ada_bc", [128, 4 * D])
    gmix_fm_d = din("g_mix_fm", [128, KC])
    gffn_bc_d = din("g_ffn_bc", [128, D])
    gfin_bc_d = din("g_final_bc", [128, D])
    win_d = din("w_in", [D, NCOL])
    wsc_fm_d = din("w_sconv_fm", [128, KC * 3])
    wsco_d = din("w_sconv_out", [D, D])
    wxc_fm_d = din("w_ssm_conv_fm", [128, 24 * 4])
    bxc_fm_d = din("b_ssm_conv_fm", [128, 24])
    dtb_bc_d = din("dt_bias_bc", [128, NH])
    alog_bc_d = din("a_log_bc", [128, NH])
    dsk_bc_d = din("d_skip_bc", [128, NH])
    gssm_bc_d = din("g_ssm_bc", [128, DI])
    wsso_d = din("w_ssm_out", [DI, D])
    wo_d = din("w_o", [D, D])
    wr_d = din("w_router", [D, NE])
    br_bc_d = din("b_router_bc", [128, NE])
    wgu_d = din("w_gu", [NE * D, 2 * D])
    bgu_l_d = din("b_gu_l", [NE * 128, 16])
    wd_d = din("w_down", [NE * D, D])
    bd_d = din("b_down", [NE, D])
    out_d = nc.dram_tensor("out", [NT, D], F32, kind="ExternalOutput").ap()

    win_bf = dscr("win_bf", [D, NCOL], BF16)
    wsco_bf = dscr("wsco_bf", [D, D], BF16)
    wsso_bf = dscr("wsso_bf", [DI, D], BF16)
    wo_bf = dscr("wo_bf", [D, D], BF16)
    wg_bf = dscr("wg_bf", [NE * 128, KC * D], BF16)
    wu_bf = dscr("wu_bf", [NE * 128, KC * D], BF16)
    wd_bf = dscr("wd_bf", [NE * 128, KC * D], BF16)
    x1_h = dscr("x1_h", [NT, D], F32)
    h2_h = dscr("h2_h", [NT, D], BF16)
    xs_h = dscr("xs_h", [RTOT, D], BF16)
    ys_h = dscr("ys_h", [RTOT, D], F32)

    with ExitStack() as st:
        S = Sched(nc, st)
        def OP(eng, fn, r=(), w=()):
            S.op(eng, fn, reads=r, writes=w)

        def MM(outb, out_ap, pairs, reads):
            n = len(pairs)
            for i, (l, r_) in enumerate(pairs):
                S.op("pe", lambda E, l=l, r_=r_, i=i: E.matmul(out_ap, l, r_, start=(i == 0), stop=(i == n - 1)),
                     reads=reads, writes=[outb], inc=(i == n - 1))

        def TR(outb, out_ap, in_ap, idn, reads):
            S.op("pe", lambda E: E.transpose(out_ap, in_ap, idn.t[:]), reads=list(reads) + [idn], writes=[outb])

        def LOAD(eng, dst, dst_ap, src_ap, reads=()):
            S.dma(eng, lambda E: E.dma_start(out=dst_ap, in_=src_ap), dst, reads=reads, writes=[dst])

        open_stacks = []

        def finish():
            S.barrier()
            S.emit()
            for stx in reversed(open_stacks):
                stx.close()
            return nc

        B_win, B_wsco, B_wsso, B_wo = Buf(None, "winbf"), Buf(None, "wscobf"), Buf(None, "wssobf"), Buf(None, "wobf")
        B_wgu, B_wd = Buf(None, "wgubf"), Buf(None, "wdbf")
        B_x1, B_h2, B_xs, B_ys, B_out = Buf(None, "x1h"), Buf(None, "h2h"), Buf(None, "xsh"), Buf(None, "ysh"), Buf(None, "outh")

        for kc in range(KC):
            S.dma("pool", lambda E, kc=kc: E.dma_start(out=win_bf[kc * 128:(kc + 1) * 128, :], in_=win_d[kc * 128:(kc + 1) * 128, :]),
                  B_win, writes=[B_win])
        for (dst, src, bb, rows) in ((wsco_bf, wsco_d, B_wsco, D), (wsso_bf, wsso_d, B_wsso, DI), (wo_bf, wo_d, B_wo, D)):
            for r0 in range(0, rows, 512):
                S.dma("pool", lambda E, dst=dst, src=src, r0=r0: E.dma_start(out=dst[r0:r0 + 512, :], in_=src[r0:r0 + 512, :]),
                      bb, writes=[bb])

        G = [S.psum(f"G{i}", [128, 512], F32) for i in range(5)]
        ptb = S.psum("ptb", [128, 2048], BF16)
        ptB = S.psum("ptB", [128, 1024], BF16)
        Grot = Rot(G)

        io = S.sbuf("io", [128, 128], F32)
        ident = S.sbuf("ident", [128, 128], F32)
        identb = S.sbuf("identb", [128, 128], BF16)
        triu = S.sbuf("triu", [128, 128], F32)
        ones = S.sbuf("ones", [128, 128], F32)
        onesb = S.sbuf("onesb", [128, 128], BF16)
        tristb = S.sbuf("tristb", [128, 128], BF16)
        epst = S.sbuf("epst", [128, 1], F32)
        iota32 = S.sbuf("iota32", [128, NE], F32)
        OP("pool", lambda E: E.iota(io.t[:], pattern=[[1, 128]], base=0, channel_multiplier=-1,
                                    allow_small_or_imprecise_dtypes=True), w=[io])
        OP("pool", lambda E: E.iota(iota32.t[:], pattern=[[1, NE]], base=0, channel_multiplier=0,
                                    allow_small_or_imprecise_dtypes=True), w=[iota32])
        OP("dve", lambda E: E.tensor_single_scalar(ident.t[:], io.t[:], 0.0, ALU.is_equal), r=[io], w=[ident])
        OP("dve", lambda E: E.tensor_single_scalar(identb.t[:], io.t[:], 0.0, ALU.is_equal), r=[io], w=[identb])
        OP("dve", lambda E: E.tensor_single_scalar(triu.t[:], io.t[:], 0.0, ALU.is_ge), r=[io], w=[triu])
        OP("dve", lambda E: E.tensor_single_scalar(tristb.t[:], io.t[:], 0.0, ALU.is_gt), r=[io], w=[tristb])
        OP("dve", lambda E: E.memset(ones.t[:], 1.0), w=[ones])
        OP("dve", lambda E: E.memset(onesb.t[:], 1.0), w=[onesb])
        OP("dve", lambda E: E.memset(epst.t[:], EPS), w=[epst])

        def small(name, shape, src, dt=F32):
            b = S.sbuf(name, shape, dt)
            LOAD("sp", b, b.t[:], src)
            return b

        cT = small("cT", [128, KC * NSEQ], cT_d)
        bada_fm = small("bada_fm", [128, 16], bada_fm_d)
        gmix_fm = small("gmix_fm", [128, KC], gmix_fm_d)
        wsc_fm = small("wsc_fm", [128, KC * 3], wsc_fm_d)
        wxc_fm = small("wxc_fm", [128, 96], wxc_fm_d)
        bxc_fm = small("bxc_fm", [128, 24], bxc_fm_d)
        dtb_bc = small("dtb_bc", [128, NH], dtb_bc_d)
        a_bc = small("a_bc", [128, NH], alog_bc_d)
        dsk_bc = small("dsk_bc", [128, NH], dsk_bc_d)
        br_bc = small("br_bc", [128, NE], br_bc_d)
        wr_sb = S.sbuf("wr_sb", [128, KC, NE], F32)
        LOAD("sp", wr_sb, wr_sb.t[:], wr_d.rearrange("(kc p) n -> p kc n", p=128))
        wdt_sb = S.sbuf("wdt_sb", [128, KC, NH], BF16)
        OP("act", lambda E: E.activation(a_bc.t[:], a_bc.t[:], AF.Exp), r=[a_bc], w=[a_bc])
        OP("dve", lambda E: E.tensor_single_scalar(a_bc.t[:], a_bc.t[:], -1.0, ALU.mult), r=[a_bc], w=[a_bc])
        OP("act", lambda E: E.activation(cT.t[:], cT.t[:], AF.Silu), r=[cT], w=[cT])

        gs1 = S.sbuf("gs1", [128, KC * NSEQ], F32)
        sh1 = S.sbuf("sh1", [128, KC * NSEQ], F32)
        wts = S.sbuf("wts", [128, NTT * TOPK], F32)
        dest_i = S.sbuf("dest_i", [128, NTT * TOPK], I32)
        wrow = S.sbuf("wrow", [128, NBLK * KC], I32)
        brow = S.sbuf("brow", [128, NBLK], I32)
        erow = S.sbuf("erow", [128, NBLK], I32)
        plog = ExitStack()
        p1 = ExitStack()
        open_stacks.extend([plog, p1])
        logits = S.sbuf("logits", [128, NTT * NE], F32, plog)
        wadab = Rot([S.sbuf(f"wadab{i}", [128, KC, 128], F32, p1) for i in range(2)])
        badab = Rot([S.sbuf(f"badab{i}", [128, 128], F32, p1) for i in range(2)])
        condrep = S.sbuf("condrep", [128, KC, 128], F32, p1)
        bct = S.sbuf("bct", [128, 3 * D], F32, p1)
        gffn_bc = S.sbuf("gffn_bc", [128, D], F32, p1)
        LOAD("sp", gffn_bc, gffn_bc.t[:], gffn_bc_d)
        M = {"wadab": wadab, "badab": badab, "condrep": condrep, "bct": bct, "off": 0}
        wada_r = wada_d.rearrange("(kc p) n -> p kc n", p=128)

        for ch in range(16):
            wb = wadab.next()
            LOAD("sp", wb, wb.t[:], wada_r[:, :, ch * 128:(ch + 1) * 128])
            for jj in range(1):
                j = ch
                g = Grot.next()
                MM(g, g.t[:, 0:NSEQ], [(wb.t[:, kc, jj * 128:(jj + 1) * 128], cT.t[:, kc * NSEQ:(kc + 1) * NSEQ]) for kc in range(KC)], [wb, cT])
                if j < 8:
                    for b in range(NSEQ):
                        OP("act", lambda E, g=g, j=j, b=b: E.activation(sh1.t[:, j * NSEQ + b:j * NSEQ + b + 1], g.t[:, b:b + 1], AF.Identity,
                                                                         bias=bada_fm.t[:, j:j + 1], scale=1.0), r=[g, bada_fm], w=[sh1])
                else:
                    c = j - 8
                    for b in range(NSEQ):
                        OP("act", lambda E, g=g, j=j, b=b, c=c: E.activation(gs1.t[:, c * NSEQ + b:c * NSEQ + b + 1], g.t[:, b:b + 1], AF.Identity,
                                                                              bias=bada_fm.t[:, j:j + 1], scale=1.0), r=[g, bada_fm], w=[gs1])
                        OP("dve", lambda E, b=b, c=c: E.tensor_scalar(gs1.t[:, c * NSEQ + b:c * NSEQ + b + 1], gs1.t[:, c * NSEQ + b:c * NSEQ + b + 1],
                                                                     1.0, gmix_fm.t[:, c:c + 1], ALU.add, ALU.mult), r=[gs1, gmix_fm], w=[gs1])

        def mod_bc(b, chunks):
            wadab, badab, condrep, bct, off = M["wadab"], M["badab"], M["condrep"], M["bct"], M["off"]
            for kc in range(KC):
                OP("dve", lambda E, kc=kc: E.tensor_copy(condrep.t[:, kc, :], cT.t[:, kc * NSEQ + b:kc * NSEQ + b + 1].to_broadcast([128, 128])),
                   r=[cT], w=[condrep])
            for ch in chunks:
                wb = wadab.next()
                bb = badab.next()
                LOAD("sp", wb, wb.t[:], wada_r[:, :, 2 * D + ch * 128:2 * D + (ch + 1) * 128])
                LOAD("sp", bb, bb.t[:], bada_bc_d[:, ch * 128:(ch + 1) * 128])
                g = Grot.next()
                MM(g, g.t[:, 0:128], [(condrep.t[:, kc, :], wb.t[:, kc, :]) for kc in range(KC)], [wb, condrep])
                dsl = slice((ch - off) * 128, (ch - off + 1) * 128)
                OP("dve", lambda E, g=g, bb=bb, dsl=dsl: E.tensor_tensor(bct.t[:, dsl], g.t[:, 0:128], bb.t[:], ALU.add),
                   r=[g, bb], w=[bct])
                if 16 <= ch < 24:
                    OP("dve", lambda E, ch=ch, dsl=dsl: E.scalar_tensor_tensor(bct.t[:, dsl], bct.t[:, dsl], 1.0,
                                                                      gffn_bc.t[:, (ch - 16) * 128:(ch - 15) * 128], ALU.add, ALU.mult),
                       r=[bct, gffn_bc], w=[bct])

        GT1, SH2, GS2, GT2 = 0, D, 2 * D, 0
        if STOP == 0:
            return finish()

        gssm_bc = S.sbuf("gssm_bc", [128, DI], F32, p1)
        LOAD("sp", gssm_bc, gssm_bc.t[:], gssm_bc_d)
        LOAD("sp", wdt_sb, wdt_sb.t[:], win_bf.rearrange("(kc p) n -> p kc n", p=128)[:, :, C_DT:C_DT + NH], reads=[B_win])
        wst = Rot([S.sbuf(f"wst{i}", [128, KC, 512], BF16, p1) for i in range(4)])
        xr = Rot([S.sbuf(f"xr{i}", [128, D], F32, p1) for i in range(2)])
        junk = S.sbuf("junk", [128, D], BF16, p1)
        ss = S.sbuf("ss", [128, 4], F32, p1)
        rstd = S.sbuf("rstd", [128, 4], F32, p1)
        hT = S.sbuf("hT", [128, KC, T], BF16, p1)
        vT = S.sbuf("vT", [128, KC, T], BF16, p1)
        tmpA = Rot([S.sbuf(f"tmpA{i}", [128, T], F32, p1) for i in range(2)])
        tmpY = Rot([S.sbuf(f"tmpY{i}", [128, T], F32, p1) for i in range(2)])
        utmp = Rot([S.sbuf(f"utmp{i}", [128, T + 2], F32, p1) for i in range(2)])
        uhalo = S.sbuf("uhalo", [128, KC, 2], F32, p1)
        xraw = Rot([S.sbuf(f"xraw{i}", [128, T + 3], F32, p1) for i in range(2)])
        xhalo = S.sbuf("xhalo", [128, 24, 3], F32, p1)
        xsT = S.sbuf("xsT", [128, 16, T], BF16, p1)
        BT = S.sbuf("BT", [128, NG, T], BF16, p1)
        CT = S.sbuf("CT", [128, NG, T], BF16, p1)
        zs = S.sbuf("zs", [128, 2, DI], BF16, p1)
        dtt = S.sbuf("dtt", [128, 2, NH], F32, p1)
        dat = S.sbuf("dat", [128, 2, NH], F32, p1)
        dtmp = S.sbuf("dtmp", [128, NH], F32, p1)
        acum = S.sbuf("acum", [128, NH], F32, p1)
        dend = S.sbuf("dend", [128, NH], F32, p1)
        dchunk = S.sbuf("dchunk", [128, NH], F32, p1)
        eacum = S.sbuf("eacum", [128, NH], F32, p1)
        xs_tok = S.sbuf("xs_tok", [128, DI], BF16, p1)
        xdt = S.sbuf("xdt", [128, DI], BF16, p1)
        xdte = S.sbuf("xdte", [128, DI], BF16, p1)
        B_tok = S.sbuf("B_tok", [128, NG * NS], BF16, p1)
        CBm = S.sbuf("CBm", [128, NG, 128], F32, p1)
        seg = Rot([S.sbuf(f"seg{i}", [128, 4, 128], F32, p1) for i in range(2)])
        Sb = S.sbuf("Sb", [128, 8, 128], BF16, p1)
        st32 = S.sbuf("st32", [128, DI], F32, p1)
        stbf = S.sbuf("stbf", [128, DI], BF16, p1)
        yg = Rot([S.sbuf(f"yg{i}", [128, 512], F32, p1) for i in range(2)])
        yt2 = Rot([S.sbuf(f"yt2{i}", [128, 512], F32, p1) for i in range(1)])
        yn = Rot([S.sbuf(f"yn{i}", [128, 512], BF16, p1) for i in range(2)])
        ssg = S.sbuf("ssg", [128, 2], F32, p1)
        ynT = S.sbuf("ynT", [128, 16, T], BF16, p1)
        mpart = S.sbuf("mpart", [128, 4, T], F32, p1)
        mT = S.sbuf("mT", [128, KC, T], BF16, p1)
        h2 = S.sbuf("h2", [128, D], F32, p1)
        h2b = Rot([S.sbuf(f"h2b{i}", [128, D], BF16, p1) for i in range(1)])
        h2T = S.sbuf("h2T", [128, KC, 128], F32, p1)

        win_r = win_bf.rearrange("(kc p) n -> p kc n", p=128)
        wsco_r = wsco_bf.rearrange("(kc p) n -> p kc n", p=128)
        wsso_r = wsso_bf.rearrange("(kc p) n -> p kc n", p=128)
        wo_r = wo_bf.rearrange("(kc p) n -> p kc n", p=128)

        def wload(src_r, bb, c0, k0=0):
            w = wst.next()
            LOAD("sp", w, w.t[:], src_r[:, k0:k0 + KC, c0:c0 + 512], reads=[bb])
            return w

        def rms_stats(src, col, n):
            OP("act", lambda E: E.activation(junk.t[:, 0:n], src, AF.Square, accum_out=ss.t[:, col:col + 1]), r=[], w=[junk, ss])
            OP("act", lambda E: E.activation(ss.t[:, col:col + 1], ss.t[:, col:col + 1], AF.Sqrt, bias=epst.t[:, 0:1], scale=1.0 / n),
               r=[ss, epst], w=[ss])
            OP("dve", lambda E: E.reciprocal(rstd.t[:, col:col + 1], ss.t[:, col:col + 1]), r=[ss], w=[rstd])

        cast_jobs = []
        for e in range(NE):
            cast_jobs.append((wg_bf, wgu_d, e, 0, B_wgu))
            cast_jobs.append((wu_bf, wgu_d, e, D, B_wgu))
            cast_jobs.append((wd_bf, wd_d, e, 0, B_wd))

        def issue_casts(n):
            for _ in range(n):
                if not cast_jobs:
                    return
                dst, src, e, c0, bb = cast_jobs.pop(0)
                S.dma("pool", lambda E, dst=dst, src=src, e=e, c0=c0: E.dma_start(
                    out=dst[e * 128:(e + 1) * 128, :].rearrange("p (k n) -> p k n", n=D),
                    in_=src[e * D:(e + 1) * D, c0:c0 + D].rearrange("(k p) n -> p k n", p=128)), bb, writes=[bb])

        casts_per_tile = -(-len(cast_jobs) // (NSEQ * NTL))
        zt = S.sbuf("zt", [128, 2 * D], BF16, p1)
        OP("pool", lambda E: E.memset(zt.t[:], 0.0), w=[zt])
        B_xz = Buf(None, "xsz")
        zero_jobs = list(range(0, RTOT, 256))

        def issue_zero(n):
            for _ in range(n):
                if not zero_jobs:
                    return
                q0 = zero_jobs.pop(0)
                S.dma("sp", lambda E, q0=q0: E.dma_start(out=xs_h[q0:q0 + 256, :].rearrange("(p r) d -> p (r d)", r=2), in_=zt.t[:]),
                      B_xz, reads=[zt], writes=[])
            B_xz.w = ("d", B_xz)

        zeros_per_tile = -(-len(zero_jobs) // (NSEQ * NTL))
        for b in range(NSEQ):
            mod_bc(b, range(0, 24))
            OP("dve", lambda E: E.memset(st32.t[:], 0.0), w=[st32])
            OP("dve", lambda E: E.memset(stbf.t[:], 0.0), w=[stbf])
            OP("dve", lambda E: E.memset(uhalo.t[:], 0.0), w=[uhalo])
            OP("dve", lambda E: E.memset(xhalo.t[:], 0.0), w=[xhalo])
            for it in range(NTL):
                r0 = b * SEQ + it * T
                xs_ = []
                for j in range(2):
                    xb = xr.next()
                    xs_.append(xb)
                    LOAD("sp", xb, xb.t[:], x_d[r0 + j * 128:r0 + (j + 1) * 128, :])
                    OP("act", lambda E, xb=xb, j=j: E.activation(junk.t[:], xb.t[:], AF.Square, accum_out=ss.t[:, j:j + 1]), r=[xb], w=[junk, ss])
                    OP("act", lambda E, j=j: E.activation(ss.t[:, j:j + 1], ss.t[:, j:j + 1], AF.Sqrt, bias=epst.t[:, 0:1], scale=1.0 / D),
                       r=[ss, epst], w=[ss])
                    OP("dve", lambda E, j=j: E.reciprocal(rstd.t[:, j:j + 1], ss.t[:, j:j + 1]), r=[ss], w=[rstd])
                    OP("dve", lambda E, xb=xb, j=j: E.tensor_scalar(xb.t[:], xb.t[:], rstd.t[:, j:j + 1], None, ALU.mult), r=[xb, rstd], w=[xb])
                for c in range(KC):
                    g = Grot.next()
                    for j in range(2):
                        TR(g, g.t[:, j * 128:(j + 1) * 128], xs_[j].t[:, c * 128:(c + 1) * 128], ident, [xs_[j]])
                    OP("act", lambda E, g=g, c=c, b=b: E.activation(hT.t[:, c, :], g.t[:, 0:T], AF.Identity,
                                                               bias=sh1.t[:, c * NSEQ + b:c * NSEQ + b + 1],
                                                               scale=gs1.t[:, c * NSEQ + b:c * NSEQ + b + 1]), r=[g, sh1, gs1], w=[hT])

                def proj_fm(w, cc):
                    g = Grot.next()
                    MM(g, g.t[:, 0:T], [(w.t[:, kc, cc * 128:(cc + 1) * 128], hT.t[:, kc, :]) for kc in range(KC)], [w, hT])
                    return g

                if STOP == 11:
                    return finish()
                for q in range(2):
                    w_sb = wload(win_r, B_win, C_SB + q * 512)
                    w_sx = wload(win_r, B_win, C_SX + q * 512)
                    w_sc = wload(win_r, B_win, C_SC + q * 512)
                    for cc in range(4):
                        c = q * 4 + cc
                        g_sb = proj_fm(w_sb, cc)
                        g_sx = proj_fm(w_sx, cc)
                        ta = tmpA.next()
                        OP("act", lambda E, ta=ta, g=g_sx: E.activation(ta.t[:], g.t[:, 0:T], AF.Copy), r=[g_sx], w=[ta])
                        u = utmp.next()
                        OP("pool", lambda E, u=u, c=c: E.tensor_copy(u.t[:, 0:2], uhalo.t[:, c, :]), r=[uhalo], w=[u])
                        OP("dve", lambda E, u=u, g=g_sb, ta=ta: E.tensor_tensor(u.t[:, 2:T + 2], g.t[:, 0:T], ta.t[:], ALU.mult), r=[g_sb, ta], w=[u])
                        OP("pool", lambda E, u=u, c=c: E.tensor_copy(uhalo.t[:, c, :], u.t[:, T:T + 2]), r=[u], w=[uhalo])
                        g_sc = proj_fm(w_sc, cc)
                        y = tmpY.next()
                        OP("pool", lambda E, y=y, u=u, c=c: E.tensor_scalar(y.t[:], u.t[:, 2:T + 2], wsc_fm.t[:, c * 3 + 2:c * 3 + 3], None, ALU.mult),
                           r=[u, wsc_fm], w=[y])
                        OP("dve", lambda E, y=y, u=u, c=c: E.scalar_tensor_tensor(y.t[:], u.t[:, 1:T + 1], wsc_fm.t[:, c * 3 + 1:c * 3 + 2], y.t[:], ALU.mult, ALU.add),
                           r=[u, wsc_fm, y], w=[y])
                        OP("dve", lambda E, y=y, u=u, c=c: E.scalar_tensor_tensor(y.t[:], u.t[:, 0:T], wsc_fm.t[:, c * 3:c * 3 + 1], y.t[:], ALU.mult, ALU.add),
                           r=[u, wsc_fm, y], w=[y])
                        OP("dve", lambda E, y=y, g=g_sc, c=c: E.tensor_tensor(vT.t[:, c, :], g.t[:, 0:T], y.t[:], ALU.mult), r=[g_sc, y], w=[vT])

                if STOP == 12:
                    return finish()
                for zq in range(4):
                    w_z = wload(win_r, B_win, C_Z + zq * 512)
                    for j in range(2):
                        g = Grot.next()
                        MM(g, g.t[:], [(hT.t[:, kc, j * 128:(j + 1) * 128], w_z.t[:, kc, :]) for kc in range(KC)], [w_z, hT])
                        OP("act", lambda E, g=g, j=j, zq=zq: E.activation(zs.t[:, j, zq * 512:(zq + 1) * 512], g.t[:], AF.Silu), r=[g], w=[zs])
                for j in range(2):
                    g = Grot.next()
                    MM(g, g.t[:, 0:NH], [(hT.t[:, kc, j * 128:(j + 1) * 128], wdt_sb.t[:, kc, :]) for kc in range(KC)], [wdt_sb, hT])
                    OP("dve", lambda E, g=g: E.tensor_tensor(dtmp.t[:], g.t[:, 0:NH], dtb_bc.t[:], ALU.add), r=[g, dtb_bc], w=[dtmp])
                    OP("act", lambda E: E.activation(dtmp.t[:], dtmp.t[:], AF.Exp), r=[dtmp], w=[dtmp])
                    OP("act", lambda E, j=j: E.activation(dtt.t[:, j, :], dtmp.t[:], AF.Ln, bias=1.0, scale=1.0), r=[dtmp], w=[dtt])
                    OP("dve", lambda E, j=j: E.tensor_tensor(dat.t[:, j, :], dtt.t[:, j, :], a_bc.t[:], ALU.mult), r=[dtt, a_bc], w=[dat])
                for xq in range(6):
                    w_x = wload(win_r, B_win, C_XBC + xq * 512)
                    for cc in range(4):
                        cx = xq * 4 + cc
                        g = proj_fm(w_x, cc)
                        xw = xraw.next()
                        OP("pool", lambda E, xw=xw, cx=cx: E.tensor_copy(xw.t[:, 0:3], xhalo.t[:, cx, :]), r=[xhalo], w=[xw])
                        OP("act", lambda E, xw=xw, g=g: E.activation(xw.t[:, 3:T + 3], g.t[:, 0:T], AF.Copy), r=[g], w=[xw])
                        OP("pool", lambda E, xw=xw, cx=cx: E.tensor_copy(xhalo.t[:, cx, :], xw.t[:, T:T + 3]), r=[xw], w=[xhalo])
                        y = tmpY.next()
                        OP("dve", lambda E, y=y, xw=xw, cx=cx: E.tensor_scalar(y.t[:], xw.t[:, 3:T + 3], wxc_fm.t[:, cx * 4 + 3:cx * 4 + 4], None, ALU.mult),
                           r=[xw, wxc_fm], w=[y])
                        for k in (2, 1, 0):
                            eng = "dve"
                            OP(eng, lambda E, y=y, xw=xw, cx=cx, k=k: E.scalar_tensor_tensor(y.t[:], xw.t[:, k:k + T], wxc_fm.t[:, cx * 4 + k:cx * 4 + k + 1], y.t[:],
                                                                                           ALU.mult, ALU.add), r=[xw, wxc_fm, y], w=[y])
                        if cx < 16:
                            dstb, dst = xsT, xsT.t[:, cx, :]
                        elif cx < 20:
                            dstb, dst = BT, BT.t[:, cx - 16, :]
                        else:
                            dstb, dst = CT, CT.t[:, cx - 20, :]
                        OP("act", lambda E, y=y, dst=dst, cx=cx: E.activation(dst, y.t[:], AF.Silu, bias=bxc_fm.t[:, cx:cx + 1], scale=1.0),
                           r=[y, bxc_fm], w=[dstb])

                if STOP == 13:
                    return finish()
                issue_zero(zeros_per_tile)
                for jpos, j in enumerate(_JLIST):
                    SD = (STOP - 100 * jpos) if 141 + 100 * jpos <= STOP <= 148 + 100 * jpos else -1
                    js = slice(j * 128, (j + 1) * 128)
                    g0 = G[0]
                    MM(g0, g0.t[:, 0:NH], [(triu.t[:], dat.t[:, j, :])], [triu, dat])
                    MM(g0, g0.t[:, NH:2 * NH], [(ones.t[:], dat.t[:, j, :])], [ones, dat])
                    OP("dve", lambda E: E.tensor_copy(acum.t[:], g0.t[:, 0:NH]), r=[g0], w=[acum])
                    OP("dve", lambda E: E.tensor_tensor(dend.t[:], g0.t[:, NH:2 * NH], acum.t[:], ALU.subtract), r=[g0, acum], w=[dend])
                    OP("act", lambda E: E.activation(dend.t[:], dend.t[:], AF.Exp), r=[dend], w=[dend])
                    OP("act", lambda E: E.activation(dchunk.t[:], g0.t[:, NH:2 * NH], AF.Exp), r=[g0], w=[dchunk])
                    OP("act", lambda E: E.activation(eacum.t[:], acum.t[:], AF.Exp), r=[acum], w=[eacum])
                    if STOP == 240 and jpos == 1:
                        return finish()
                    for cx in range(16):
                        TR(ptb, ptb.t[:, cx * 128:(cx + 1) * 128], xsT.t[:, cx, js], identb, [xsT])
                    if STOP == 2401 and jpos == 1:
                        return finish()
                    for g_ in range(NG):
                        TR(ptB, ptB.t[:, g_ * 128:(g_ + 1) * 128], BT.t[:, g_, js], identb, [BT])
                    if STOP == 2402 and jpos == 1:
                        return finish()
                    OP("act", lambda E: E.activation(xs_tok.t[:], ptb.t[:], AF.Copy), r=[ptb], w=[xs_tok])
                    if STOP == 2403 and jpos == 1:
                        return finish()
                    OP("dve", lambda E, j=j: E.tensor_tensor(xdt.t[:].rearrange("p (h q) -> p h q", q=HP), xs_tok.t[:].rearrange("p (h q) -> p h q", q=HP),
                                                             dtt.t[:, j, :].unsqueeze(2).to_broadcast([128, NH, HP]), ALU.mult), r=[xs_tok, dtt], w=[xdt])
                    if STOP == 2404 and jpos == 1:
                        return finish()
                    OP("dve", lambda E: E.tensor_tensor(xdte.t[:].rearrange("p (h q) -> p h q", q=HP), xdt.t[:].rearrange("p (h q) -> p h q", q=HP),
                                                         dend.t[:].unsqueeze(2).to_broadcast([128, NH, HP]), ALU.mult), r=[xdt, dend], w=[xdte])
                    OP("act", lambda E: E.activation(B_tok.t[:], ptB.t[:, 0:NG * NS], AF.Copy), r=[ptB], w=[B_tok])
                    if SD == 141:
                        return finish()
                    g1 = G[1]
                    for g_ in range(NG):
                        MM(g1, g1.t[:, g_ * 128:(g_ + 1) * 128], [(BT.t[:, g_, js], CT.t[:, g_, js])], [BT, CT])
                    OP("dve", lambda E: E.tensor_tensor(CBm.t[:], g1.t[:].rearrange("p (g i) -> p g i", i=128),
                                                        triu.t[:].unsqueeze(1).to_broadcast([128, NG, 128]), ALU.mult), r=[g1, triu], w=[CBm])
                    if SD == 142:
                        return finish()
                    for g_ in range(NG):
                        gsl = slice(g_ * 512, (g_ + 1) * 512)
                        for half in range(2):
                            ga_ = G[2 + half]
                            for hh in range(4):
                                h = g_ * 8 + half * 4 + hh
                                MM(ga_, ga_.t[:, hh * 128:(hh + 1) * 128], [(dat.t[:, j, h:h + 1].to_broadcast([128, 128]), triu.t[:])], [dat, triu])
                            sg_ = seg.next()
                            h0 = g_ * 8 + half * 4
                            OP("dve", lambda E, ga_=ga_, sg_=sg_, h0=h0: E.tensor_tensor(sg_.t[:], ga_.t[:].rearrange("p (h i) -> p h i", i=128),
                                                                                       acum.t[:, h0:h0 + 4].unsqueeze(2).to_broadcast([128, 4, 128]), ALU.subtract),
                               r=[ga_, acum], w=[sg_])
                            OP("act", lambda E, sg_=sg_: E.activation(sg_.t[:], sg_.t[:], AF.Relu, scale=-1.0), r=[sg_], w=[sg_])
                            OP("act", lambda E, sg_=sg_: E.activation(sg_.t[:], sg_.t[:], AF.Exp, scale=-1.0), r=[sg_], w=[sg_])
                            OP("dve", lambda E, sg_=sg_, half=half, g_=g_: E.tensor_tensor(Sb.t[:, half * 4:half * 4 + 4, :], sg_.t[:],
                                                                                         CBm.t[:, g_:g_ + 1, :].to_broadcast([128, 4, 128]), ALU.mult),
                               r=[sg_, CBm], w=[Sb])
                        if SD == 143:
                            return finish()
                        g4 = G[4]
                        for h8 in range(8):
                            h = g_ * 8 + h8
                            MM(g4, g4.t[:, h8 * HP:(h8 + 1) * HP], [(Sb.t[:, h8, :], xdt.t[:, h * HP:(h + 1) * HP])], [Sb, xdt])
                        MM(g0, g0.t[:], [(CT.t[:, g_, js], stbf.t[:, gsl])], [CT, stbf])
                        y_ = yg.next()
                        OP("dve", lambda E, y_=y_, g_=g_: E.tensor_tensor(y_.t[:].rearrange("p (h q) -> p h q", q=HP), g0.t[:].rearrange("p (h q) -> p h q", q=HP),
                                                                         eacum.t[:, g_ * 8:(g_ + 1) * 8].unsqueeze(2).to_broadcast([128, 8, HP]), ALU.mult),
                           r=[g0, eacum], w=[y_])
                        OP("dve", lambda E, y_=y_: E.tensor_tensor(y_.t[:], g4.t[:], y_.t[:], ALU.add), r=[g4, y_], w=[y_])
                        t2 = yt2.next()
                        OP("dve", lambda E, t2=t2, g_=g_, gsl=gsl: E.tensor_tensor(t2.t[:].rearrange("p (h q) -> p h q", q=HP), xs_tok.t[:, gsl].rearrange("p (h q) -> p h q", q=HP),
                                                                                  dsk_bc.t[:, g_ * 8:(g_ + 1) * 8].unsqueeze(2).to_broadcast([128, 8, HP]), ALU.mult),
                           r=[xs_tok, dsk_bc], w=[t2])
                        OP("dve", lambda E, t2=t2, y_=y_: E.tensor_tensor(y_.t[:], y_.t[:], t2.t[:], ALU.add), r=[t2, y_], w=[y_])
                        if SD == 144:
                            return finish()
                        OP("dve", lambda E, y_=y_, j=j, gsl=gsl: E.tensor_tensor(y_.t[:], y_.t[:], zs.t[:, j, gsl], ALU.mult), r=[y_, zs], w=[y_])
                        OP("act", lambda E, y_=y_: E.activation(junk.t[:, 0:512], y_.t[:], AF.Square, accum_out=ssg.t[:, 0:1]), r=[y_], w=[junk, ssg])
                        OP("act", lambda E: E.activation(ssg.t[:, 0:1], ssg.t[:, 0:1], AF.Sqrt, bias=epst.t[:, 0:1], scale=1.0 / 512), r=[ssg, epst], w=[ssg])
                        OP("dve", lambda E: E.reciprocal(ssg.t[:, 1:2], ssg.t[:, 0:1]), r=[ssg], w=[ssg])
                        yn_ = yn.next()
                        OP("dve", lambda E, y_=y_, yn_=yn_, gsl=gsl: E.scalar_tensor_tensor(yn_.t[:], y_.t[:], ssg.t[:, 1:2], gssm_bc.t[:, gsl], ALU.mult, ALU.mult),
                           r=[y_, ssg, gssm_bc], w=[yn_])
                        for q in range(4):
                            TR(ptB, ptB.t[:, 512 + q * 128:512 + (q + 1) * 128], yn_.t[:, q * 128:(q + 1) * 128], identb, [yn_])
                        OP("act", lambda E, g_=g_, js=js: E.activation(ynT.t[:, g_ * 4:(g_ + 1) * 4, js], ptB.t[:, 512:1024].rearrange("p (q t) -> p q t", t=128), AF.Copy),
                           r=[ptB], w=[ynT])
                        if SD == 145:
                            return finish()
                        MM(g1, g1.t[:], [(B_tok.t[:, g_ * 128:(g_ + 1) * 128], xdte.t[:, gsl])], [B_tok, xdte])
                        OP("dve", lambda E, g_=g_, gsl=gsl: E.tensor_tensor(st32.t[:, gsl].rearrange("p (h q) -> p h q", q=HP), st32.t[:, gsl].rearrange("p (h q) -> p h q", q=HP),
                                                                           dchunk.t[:, g_ * 8:(g_ + 1) * 8].unsqueeze(2).to_broadcast([128, 8, HP]), ALU.mult),
                           r=[st32, dchunk], w=[st32])
                        OP("dve", lambda E, gsl=gsl: E.tensor_tensor(st32.t[:, gsl], g1.t[:], st32.t[:, gsl], ALU.add), r=[g1, st32], w=[st32])
                        OP("act", lambda E, gsl=gsl: E.activation(stbf.t[:, gsl], st32.t[:, gsl], AF.Copy), r=[st32], w=[stbf])
                        if SD == 146:
                            return finish()
                        if SD == 147 and g_ == 1:
                            return finish()
                    if SD == 148:
                        return finish()

                if STOP == 14:
                    return finish()
                issue_casts(casts_per_tile)
                for dq in range(2):
                    w_a = wload(wsco_r, B_wsco, dq * 512)
                    w_ga = wload(win_r, B_win, C_GA + dq * 512)
                    for dcc in range(4):
                        g_y = Grot.next()
                        MM(g_y, g_y.t[:, 0:T], [(w_a.t[:, kc, dcc * 128:(dcc + 1) * 128], vT.t[:, kc, :]) for kc in range(KC)], [w_a, vT])
                        g_g = proj_fm(w_ga, dcc)
                        ta = tmpA.next()
                        OP("act", lambda E, ta=ta, g=g_g: E.activation(ta.t[:], g.t[:, 0:T], AF.Sigmoid), r=[g_g], w=[ta])
                        OP("dve", lambda E, ta=ta, g=g_y, dcc=dcc: E.tensor_tensor(mpart.t[:, dcc, :], g.t[:, 0:T], ta.t[:], ALU.mult), r=[g_y, ta], w=[mpart])
                    w_b0 = wload(wsso_r, B_wsso, dq * 512, 0)
                    w_b1 = wload(wsso_r, B_wsso, dq * 512, 8)
                    w_gb = wload(win_r, B_win, C_GB + dq * 512)
                    for dcc in range(4):
                        g_y = Grot.next()
                        MM(g_y, g_y.t[:, 0:T], [(w_b0.t[:, kc, dcc * 128:(dcc + 1) * 128], ynT.t[:, kc, :]) for kc in range(KC)] +
                           [(w_b1.t[:, kc, dcc * 128:(dcc + 1) * 128], ynT.t[:, 8 + kc, :]) for kc in range(KC)], [w_b0, w_b1, ynT])
                        g_g = proj_fm(w_gb, dcc)
                        ta = tmpA.next()
                        OP("act", lambda E, ta=ta, g=g_g: E.activation(ta.t[:], g.t[:, 0:T], AF.Sigmoid), r=[g_g], w=[ta])
                        OP("dve", lambda E, ta=ta, g=g_y: E.tensor_tensor(ta.t[:], g.t[:, 0:T], ta.t[:], ALU.mult), r=[g_y, ta], w=[ta])
                        OP("dve", lambda E, ta=ta, dq=dq, dcc=dcc: E.tensor_tensor(mT.t[:, dq * 4 + dcc, :], ta.t[:], mpart.t[:, dcc, :], ALU.add),
                           r=[ta, mpart], w=[mT])

                if STOP == 15:
                    return finish()
                x1b = [xr.next(), xr.next()]
                for j in range(2):
                    LOAD("sp", x1b[j], x1b[j].t[:], x_d[r0 + j * 128:r0 + (j + 1) * 128, :], reads=[])
                for dh in range(2):
                    w_o_ = wload(wo_r, B_wo, dh * 512)
                    dsl = slice(dh * 512, (dh + 1) * 512)
                    for j in range(2):
                        g = Grot.next()
                        MM(g, g.t[:], [(mT.t[:, kc, j * 128:(j + 1) * 128], w_o_.t[:, kc, :]) for kc in range(KC)], [w_o_, mT])
                        t2 = yt2.next()
                        OP("dve", lambda E, g=g, t2=t2, dsl=dsl: E.tensor_tensor(t2.t[:], g.t[:], bct.t[:, GT1 + dsl.start:GT1 + dsl.stop], ALU.mult), r=[g, bct], w=[t2])
                        OP("dve", lambda E, t2=t2, xb_=x1b[j], dsl=dsl: E.tensor_tensor(xb_.t[:, dsl], xb_.t[:, dsl], t2.t[:], ALU.add), r=[t2, x1b[j]], w=[x1b[j]])
                for j in range(2):
                    tt = (r0 // 128) + j
                    xb = x1b[j]
                    S.dma("sp", lambda E, xb=xb, tt=tt: E.dma_start(out=x1_h[tt * 128:(tt + 1) * 128, :], in_=xb.t[:]), xb, reads=[xb], writes=[B_x1])
                    OP("act", lambda E, xb=xb: E.activation(junk.t[:], xb.t[:], AF.Square, accum_out=ss.t[:, 2:3]), r=[xb], w=[junk, ss])
                    OP("act", lambda E: E.activation(ss.t[:, 2:3], ss.t[:, 2:3], AF.Sqrt, bias=epst.t[:, 0:1], scale=1.0 / D), r=[ss, epst], w=[ss])
                    OP("dve", lambda E: E.reciprocal(rstd.t[:, 2:3], ss.t[:, 2:3]), r=[ss], w=[rstd])
                    OP("dve", lambda E, xb=xb: E.scalar_tensor_tensor(h2.t[:], xb.t[:], rstd.t[:, 2:3], bct.t[:, GS2:GS2 + D], ALU.mult, ALU.mult),
                       r=[xb, rstd, bct], w=[h2])
                    OP("dve", lambda E: E.tensor_tensor(h2.t[:], h2.t[:], bct.t[:, SH2:SH2 + D], ALU.add), r=[h2, bct], w=[h2])
                    hb = h2b.next()
                    OP("act", lambda E, hb=hb: E.activation(hb.t[:], h2.t[:], AF.Copy), r=[h2], w=[hb])
                    S.dma("sp", lambda E, hb=hb, tt=tt: E.dma_start(out=h2_h[tt * 128:(tt + 1) * 128, :], in_=hb.t[:]), hb, reads=[hb], writes=[B_h2])
                    for half in range(2):
                        g = Grot.next()
                        for c4 in range(4):
                            c = half * 4 + c4
                            TR(g, g.t[:, c4 * 128:(c4 + 1) * 128], h2.t[:, c * 128:(c + 1) * 128], ident, [h2])
                        OP("act", lambda E, g=g, half=half: E.activation(h2T.t[:, half * 4:half * 4 + 4, :], g.t[:].rearrange("p (c t) -> p c t", t=128), AF.Copy),
                           r=[g], w=[h2T])
                    g = Grot.next()
                    MM(g, g.t[:, 0:NE], [(h2T.t[:, kc, :], wr_sb.t[:, kc, :]) for kc in range(KC)], [h2T, wr_sb])
                    OP("dve", lambda E, g=g, tt=tt: E.tensor_tensor(logits.t[:, tt * NE:(tt + 1) * NE], g.t[:, 0:NE], br_bc.t[:], ALU.add), r=[g, br_bc], w=[logits])

        issue_casts(len(cast_jobs))
        issue_zero(len(zero_jobs))
        if STOP == 1:
            return finish()
        S.barrier()
        p1.close()
        open_stacks.remove(p1)

        p2a = ExitStack()
        open_stacks.append(p2a)
        top8 = S.sbuf("top8", [128, 8], F32, p2a)
        topi = S.sbuf("topi", [128, 8], U32, p2a)
        idxf = S.sbuf("idxf", [128, NTT * TOPK], F32, p2a)
        negm = S.sbuf("negm", [128, 1], F32, p2a)
        esum = S.sbuf("esum", [128, 2], F32, p2a)
        Mb = S.sbuf("Mb", [128, NE], BF16, p2a)
        rank = S.sbuf("rank", [128, NTT * NE], F32, p2a)
        runc = S.sbuf("runc", [128, NE], F32, p2a)
        ca = S.sbuf("ca", [128, NE], F32, p2a)
        cb = S.sbuf("cb", [128, NE], F32, p2a)
        ci = S.sbuf("ci", [128, NE], I32, p2a)
        padded = S.sbuf("padded", [128, NE], F32, p2a)
        pstart = S.sbuf("pstart", [128, NE], F32, p2a)
        base = S.sbuf("base", [128, NE], F32, p2a)
        tmpe = S.sbuf("tmpe", [128, NE], F32, p2a)
        destf = S.sbuf("destf", [128, NTT * TOPK], F32, p2a)
        bstart = S.sbuf("bstart", [128, NBLK], F32, p2a)
        be = S.sbuf("be", [128, NBLK], F32, p2a)
        pio = S.sbuf("pio", [128, KC], F32, p2a)
        wrow_f = S.sbuf("wrow_f", [128, NBLK, KC], F32, p2a)
        brow_f = S.sbuf("brow_f", [128, NBLK], F32, p2a)

        OP("dve", lambda E: E.memset(runc.t[:], 0.0), w=[runc])
        for tt in range(NTT):
            lg = logits.t[:, tt * NE:(tt + 1) * NE]
            OP("dve", lambda E, lg=lg: E.max(top8.t[:], lg), r=[logits], w=[top8])
            OP("dve", lambda E, lg=lg: E.max_index(topi.t[:], top8.t[:], lg), r=[logits, top8], w=[topi])
            OP("dve", lambda E, tt=tt: E.tensor_copy(idxf.t[:, tt * 4:tt * 4 + 4], topi.t[:, 0:4]), r=[topi], w=[idxf])
            OP("dve", lambda E: E.tensor_single_scalar(negm.t[:], top8.t[:, 0:1], -1.0, ALU.mult), r=[top8], w=[negm])
            OP("act", lambda E, tt=tt: E.activation(wts.t[:, tt * 4:tt * 4 + 4], top8.t[:, 0:4], AF.Exp, bias=negm.t[:, 0:1], scale=1.0,
                                                   accum_out=esum.t[:, 0:1]), r=[top8, negm], w=[wts, esum])
            OP("dve", lambda E: E.reciprocal(esum.t[:, 1:2], esum.t[:, 0:1]), r=[esum], w=[esum])
            OP("dve", lambda E, tt=tt: E.tensor_scalar(wts.t[:, tt * 4:tt * 4 + 4], wts.t[:, tt * 4:tt * 4 + 4], esum.t[:, 1:2], None, ALU.mult),
               r=[wts, esum], w=[wts])
            OP("dve", lambda E, tt=tt: E.tensor_scalar(Mb.t[:], iota32.t[:], idxf.t[:, tt * 4:tt * 4 + 1], None, ALU.is_equal), r=[iota32, idxf], w=[Mb])
            for k in range(1, 4):
                OP("dve", lambda E, tt=tt, k=k: E.scalar_tensor_tensor(Mb.t[:], iota32.t[:], idxf.t[:, tt * 4 + k:tt * 4 + k + 1], Mb.t[:], ALU.is_equal, ALU.add),
                   r=[iota32, idxf, Mb], w=[Mb])
            g = Grot.next()
            MM(g, g.t[:, 0:NE], [(tristb.t[:], Mb.t[:])], [tristb, Mb])
            MM(g, g.t[:, NE:2 * NE], [(onesb.t[:], Mb.t[:])], [onesb, Mb])
            OP("dve", lambda E, g=g, tt=tt: E.tensor_tensor(rank.t[:, tt * NE:(tt + 1) * NE], g.t[:, 0:NE], runc.t[:], ALU.add), r=[g, runc], w=[rank])
            OP("dve", lambda E, g=g: E.tensor_tensor(runc.t[:], g.t[:, NE:2 * NE], runc.t[:], ALU.add), r=[g, runc], w=[runc])
        sh = BLK.bit_length() - 1
        OP("dve", lambda E: E.tensor_copy(ci.t[:], runc.t[:]), r=[runc], w=[ci])
        OP("dve", lambda E: E.tensor_single_scalar(ci.t[:], ci.t[:], BLK - 1, ALU.add), r=[ci], w=[ci])
        OP("dve", lambda E: E.tensor_single_scalar(ci.t[:], ci.t[:], sh, ALU.arith_shift_right), r=[ci], w=[ci])
        OP("dve", lambda E: E.tensor_single_scalar(ci.t[:], ci.t[:], sh, ALU.logical_shift_left), r=[ci], w=[ci])
        OP("dve", lambda E: E.tensor_copy(padded.t[:], ci.t[:]), r=[ci], w=[padded])
        OP("dve", lambda E: E.tensor_copy(ca.t[:], padded.t[:]), r=[padded], w=[ca])
        src, dst = ca, cb
        s_ = 1
        while s_ < NE:
            OP("dve", lambda E, src=src, dst=dst, s_=s_: E.tensor_copy(dst.t[:, 0:s_], src.t[:, 0:s_]), r=[src], w=[dst])
            OP("dve", lambda E, src=src, dst=dst, s_=s_: E.tensor_tensor(dst.t[:, s_:NE], src.t[:, s_:NE], src.t[:, 0:NE - s_], ALU.add), r=[src], w=[dst])
            src, dst = dst, src
            s_ *= 2
        pend = src
        OP("dve", lambda E: E.tensor_tensor(pstart.t[:], pend.t[:], padded.t[:], ALU.subtract), r=[pend, padded], w=[pstart])
        for tt in range(NTT):
            OP("dve", lambda E, tt=tt: E.tensor_tensor(base.t[:], rank.t[:, tt * NE:(tt + 1) * NE], pstart.t[:], ALU.add), r=[rank, pstart], w=[base])
            for k in range(4):
                col = tt * 4 + k
                OP("dve", lambda E, col=col: E.scalar_tensor_tensor(tmpe.t[:], iota32.t[:], idxf.t[:, col:col + 1], base.t[:], ALU.is_equal, ALU.mult),
                   r=[iota32, idxf, base], w=[tmpe])
                OP("dve", lambda E, col=col: E.reduce_sum(destf.t[:, col:col + 1], tmpe.t[:], axis=AX.X), r=[tmpe], w=[destf])
        OP("dve", lambda E: E.tensor_copy(dest_i.t[:], destf.t[:]), r=[destf], w=[dest_i])
        OP("pool", lambda E: E.iota(bstart.t[:], pattern=[[BLK, NBLK]], base=0, channel_multiplier=0, allow_small_or_imprecise_dtypes=True), w=[bstart])
        OP("pool", lambda E: E.iota(pio.t[:], pattern=[[128, KC]], base=0, channel_multiplier=1, allow_small_or_imprecise_dtypes=True), w=[pio])
        OP("dve", lambda E: E.memset(be.t[:], 0.0), w=[be])
        for e in range(NE):
            OP("dve", lambda E, e=e: E.scalar_tensor_tensor(be.t[:], bstart.t[:], pend.t[:, e:e + 1], be.t[:], ALU.is_ge, ALU.add), r=[bstart, pend, be], w=[be])
        OP("dve", lambda E: E.tensor_single_scalar(be.t[:], be.t[:], float(NE - 1), ALU.min), r=[be], w=[be])
        for kc in range(KC):
            OP("dve", lambda E, kc=kc: E.tensor_scalar(wrow_f.t[:, :, kc], be.t[:], float(D), pio.t[:, kc:kc + 1], ALU.mult, ALU.add), r=[be, pio], w=[wrow_f])
        OP("dve", lambda E: E.tensor_copy(wrow.t[:], wrow_f.t[:].rearrange("p b k -> p (b k)")), r=[wrow_f], w=[wrow])
        OP("dve", lambda E: E.tensor_scalar(brow_f.t[:], be.t[:], 128.0, pio.t[:, 0:1], ALU.mult, ALU.add), r=[be, pio], w=[brow_f])
        OP("dve", lambda E: E.tensor_copy(brow.t[:], brow_f.t[:]), r=[brow_f], w=[brow])
        OP("dve", lambda E: E.tensor_copy(erow.t[:], be.t[:]), r=[be], w=[erow])

        if STOP == 2:
            return finish()
        S.barrier()
        p2a.close()
        plog.close()
        open_stacks.remove(p2a)
        open_stacks.remove(plog)
        p3 = ExitStack()
        open_stacks.append(p3)
        hrows = Rot([S.sbuf(f"hrows{i}", [128, D], BF16, p3) for i in range(6)])
        for tt in range(NTT):
            hb = hrows.next()
            LOAD("sp", hb, hb.t[:], h2_h[tt * 128:(tt + 1) * 128, :], reads=[B_h2])
            for k in range(4):
                col = tt * 4 + k
                S.dma("pool", lambda E, hb=hb, col=col: E.indirect_dma_start(
                    out=xs_h, out_offset=bass.IndirectOffsetOnAxis(ap=dest_i.t[:, col:col + 1], axis=0),
                    in_=hb.t[:, :], in_offset=None), B_xs, reads=[hb, dest_i, B_xz], writes=[])
        B_xs.w = ("d", B_xs)

        if STOP == 3:
            return finish()
        wunit = Rot([S.sbuf(f"wunit{i}", [128, KC, D], BF16, p3) for i in range(6)])
        bgu_sb = Rot([S.sbuf(f"bgu_sb{i}", [128, 16], F32, p3) for i in range(2)])
        bd_sb = Rot([S.sbuf(f"bd_sb{i}", [128, D], F32, p3) for i in range(2)])
        xrows = Rot([S.sbuf(f"xrows{i}", [128, 4, D], BF16, p3) for i in range(2)])
        gcl = Rot([S.sbuf(f"gcl{i}", [128, BLK], F32, p3) for i in range(2)])
        sgm = Rot([S.sbuf(f"sgm{i}", [128, BLK], F32, p3) for i in range(2)])
        upc = Rot([S.sbuf(f"upc{i}", [128, BLK], F32, p3) for i in range(2)])
        ysb = Rot([S.sbuf(f"ysb{i}", [128, D], F32, p3) for i in range(3)])
        xTs = Rot([S.sbuf(f"xT{i}", [128, KC, BLK], BF16, p3) for i in range(2)])
        actTs = Rot([S.sbuf(f"actT{i}", [128, KC, BLK], BF16, p3) for i in range(2)])

        def blk_loads(blk):
            wgg = wunit.next()
            wgup = wunit.next()
            wdn = wunit.next()
            bg = bgu_sb.next()
            bd = bd_sb.next()
            for (wdst, wsrc, bb) in ((wgg, wg_bf, B_wgu), (wgup, wu_bf, B_wgu), (wdn, wd_bf, B_wd)):
                S.dma("pool", lambda E, wdst=wdst, wsrc=wsrc, blk=blk: E.indirect_dma_start(
                    out=wdst.t[:].rearrange("p k n -> p (k n)"), out_offset=None, in_=wsrc,
                    in_offset=bass.IndirectOffsetOnAxis(ap=brow.t[:, blk:blk + 1], axis=0)), wdst, reads=[bb, brow], writes=[wdst])
            S.dma("pool", lambda E, bg=bg, blk=blk: E.indirect_dma_start(
                out=bg.t[:, :], out_offset=None, in_=bgu_l_d,
                in_offset=bass.IndirectOffsetOnAxis(ap=brow.t[:, blk:blk + 1], axis=0)), bg, reads=[brow], writes=[bg])
            S.dma("pool", lambda E, bd=bd, blk=blk: E.indirect_dma_start(
                out=bd.t[:, :], out_offset=None, in_=bd_d,
                in_offset=bass.IndirectOffsetOnAxis(ap=erow.t[:, blk:blk + 1], axis=0)), bd, reads=[erow], writes=[bd])
            xw = xrows.next()
            LOAD("sp", xw, xw.t[:], xs_h[blk * BLK:(blk + 1) * BLK, :].rearrange("(r p) d -> p r d", p=128), reads=[B_xs])
            return dict(wgg=wgg, wgup=wgup, wdn=wdn, bg=bg, bd=bd, xw=xw)

        def blk_transposes(L):
            xw = L["xw"]
            xT = xTs.next()
            for half in range(2):
                for c4 in range(4):
                    kc = half * 4 + c4
                    for r in range(4):
                        TR(ptb, ptb.t[:, c4 * 512 + r * 128:c4 * 512 + (r + 1) * 128], xw.t[:, r, kc * 128:(kc + 1) * 128], identb, [xw])
                OP("act", lambda E, half=half, xT=xT: E.activation(xT.t[:, half * 4:half * 4 + 4, :], ptb.t[:].rearrange("p (c t) -> p c t", t=BLK), AF.Copy),
                   r=[ptb], w=[xT])
            L["xT"] = xT

        def blk_gu(L):
            wgg, wgup, bg, xT = L["wgg"], L["wgup"], L["bg"], L["xT"]
            actT = actTs.next()
            L["actT"] = actT
            for f in range(KC):
                g_g = Grot.next()
                MM(g_g, g_g.t[:], [(wgg.t[:, kc, f * 128:(f + 1) * 128], xT.t[:, kc, :]) for kc in range(KC)], [wgg, xT])
                g_u = Grot.next()
                MM(g_u, g_u.t[:], [(wgup.t[:, kc, f * 128:(f + 1) * 128], xT.t[:, kc, :]) for kc in range(KC)], [wgup, xT])
                gc_, sg_, up_ = gcl.next(), sgm.next(), upc.next()
                OP("dve", lambda E, g=g_g, gc_=gc_, f=f, bg=bg: E.tensor_scalar(gc_.t[:], g.t[:], bg.t[:, f:f + 1], 7.0, ALU.add, ALU.min), r=[g_g, bg], w=[gc_])
                OP("act", lambda E, gc_=gc_, sg_=sg_: E.activation(sg_.t[:], gc_.t[:], AF.Sigmoid, scale=1.702), r=[gc_], w=[sg_])
                OP("dve", lambda E, g=g_u, up_=up_, f=f, bg=bg: E.tensor_scalar(up_.t[:], g.t[:], bg.t[:, 8 + f:9 + f], 7.0, ALU.add, ALU.min), r=[g_u, bg], w=[up_])
                OP("pool", lambda E, up_=up_: E.tensor_scalar(up_.t[:], up_.t[:], -7.0, 1.0, ALU.max, ALU.add), r=[up_], w=[up_])
                OP("pool", lambda E, gc_=gc_, sg_=sg_: E.tensor_tensor(gc_.t[:], gc_.t[:], sg_.t[:], ALU.mult), r=[gc_, sg_], w=[gc_])
                OP("dve", lambda E, gc_=gc_, up_=up_, f=f, actT=actT: E.tensor_tensor(actT.t[:, f, :], gc_.t[:], up_.t[:], ALU.mult), r=[gc_, up_], w=[actT])

        def blk_down(L, blk):
            wdn, bd, actT = L["wdn"], L["bd"], L["actT"]
            for r in range(4):
                yb = ysb.next()
                for dh in range(2):
                    g = Grot.next()
                    MM(g, g.t[:], [(actT.t[:, fc, r * 128:(r + 1) * 128], wdn.t[:, fc, dh * 512:(dh + 1) * 512]) for fc in range(KC)], [actT, wdn])
                    OP("dve", lambda E, g=g, yb=yb, dh=dh, bd=bd: E.tensor_tensor(yb.t[:, dh * 512:(dh + 1) * 512], g.t[:], bd.t[:, dh * 512:(dh + 1) * 512], ALU.add),
                       r=[g, bd], w=[yb])
                row0 = blk * BLK + r * 128
                S.dma("sp", lambda E, yb=yb, row0=row0: E.dma_start(out=ys_h[row0:row0 + 128, :], in_=yb.t[:]), yb, reads=[yb], writes=[])

        cur = blk_loads(0)
        blk_transposes(cur)
        for blk in range(NBLK):
            nxt = blk_loads(blk + 1) if blk + 1 < NBLK else None
            blk_gu(cur)
            if nxt is not None:
                blk_transposes(nxt)
            blk_down(cur, blk)
            cur = nxt

        if STOP == 4:
            return finish()
        S.barrier()
        p3.close()
        open_stacks.remove(p3)

        p5 = ExitStack()
        open_stacks.append(p5)
        gfin_bc = S.sbuf("gfin_bc", [128, D], F32, p5)
        LOAD("sp", gfin_bc, gfin_bc.t[:], gfin_bc_d)
        x1r = Rot([S.sbuf(f"x1r{i}", [128, D], F32, p5) for i in range(4)])
        ygat = Rot([S.sbuf(f"ygat{i}", [128, D], F32, p5) for i in range(12)])
        accb = Rot([S.sbuf(f"accb{i}", [128, D], F32, p5) for i in range(4)])
        junk5 = S.sbuf("junk5", [128, D], BF16, p5)
        bct5 = S.sbuf("bct5", [128, D], F32, p5)
        M.update({"wadab": Rot([S.sbuf(f"wadab5{i}", [128, KC, 128], F32, p5) for i in range(2)]),
                  "badab": Rot([S.sbuf(f"badab5{i}", [128, 128], F32, p5) for i in range(2)]),
                  "condrep": S.sbuf("condrep5", [128, KC, 128], F32, p5), "bct": bct5, "off": 24})
        ss5 = S.sbuf("ss5", [128, 2], F32, p5)
        for b in range(NSEQ):
            mod_bc(b, range(24, 32))
            for it in range(SEQ // 128):
                tt = b * (SEQ // 128) + it
                xb = x1r.next()
                LOAD("sp", xb, xb.t[:], x1_h[tt * 128:(tt + 1) * 128, :], reads=[B_x1])
                acc = accb.next()
                for k in range(4):
                    col = tt * 4 + k
                    yk = ygat.next()
                    S.dma("pool", lambda E, yk=yk, col=col: E.indirect_dma_start(
                        out=yk.t[:, :], out_offset=None, in_=ys_h,
                        in_offset=bass.IndirectOffsetOnAxis(ap=dest_i.t[:, col:col + 1], axis=0)), yk, reads=[B_ys, dest_i], writes=[yk])
                    if k == 0:
                        OP("dve", lambda E, yk=yk, acc=acc, col=col: E.tensor_scalar(acc.t[:], yk.t[:], wts.t[:, col:col + 1], None, ALU.mult), r=[yk, wts], w=[acc])
                    else:
                        eng = "dve"
                        OP(eng, lambda E, yk=yk, acc=acc, col=col: E.scalar_tensor_tensor(acc.t[:], yk.t[:], wts.t[:, col:col + 1], acc.t[:], ALU.mult, ALU.add),
                           r=[yk, wts, acc], w=[acc])
                OP("dve", lambda E, acc=acc: E.tensor_tensor(acc.t[:], acc.t[:], bct5.t[:, GT2:GT2 + D], ALU.mult), r=[acc, bct5], w=[acc])
                OP("pool", lambda E, acc=acc, xb=xb: E.tensor_tensor(acc.t[:], acc.t[:], xb.t[:], ALU.add), r=[acc, xb], w=[acc])
                OP("act", lambda E, acc=acc: E.activation(junk5.t[:], acc.t[:], AF.Square, accum_out=ss5.t[:, 0:1]), r=[acc], w=[junk5, ss5])
                OP("act", lambda E: E.activation(ss5.t[:, 0:1], ss5.t[:, 0:1], AF.Sqrt, bias=epst.t[:, 0:1], scale=1.0 / D), r=[ss5, epst], w=[ss5])
                OP("dve", lambda E: E.reciprocal(ss5.t[:, 1:2], ss5.t[:, 0:1]), r=[ss5], w=[ss5])
                OP("dve", lambda E, acc=acc: E.scalar_tensor_tensor(acc.t[:], acc.t[:], ss5.t[:, 1:2], gfin_bc.t[:], ALU.mult, ALU.mult), r=[acc, ss5, gfin_bc], w=[acc])
                S.dma("sp", lambda E, acc=acc, tt=tt: E.dma_start(out=out_d[tt * 128:(tt + 1) * 128, :], in_=acc.t[:]), acc, reads=[acc], writes=[])
        return finish()
    return nc


def _prep_shared(inp):
    f = lambda a: np.ascontiguousarray(np.asarray(a, dtype=np.float32))
    rep = lambda v: f(np.broadcast_to(np.asarray(v, np.float32).reshape(1, -1), (128, np.asarray(v).size)))
    fm = lambda v: f(np.asarray(v, np.float32).reshape(-1, 128).T)
    l = 0
    b_ada = np.asarray(inp["b_ada"][l], np.float32)
    d = {
        "w_ada": f(inp["w_ada"][l]),
        "b_ada_fm": fm(b_ada[:2 * D]),
        "b_ada_bc": rep(b_ada[2 * D:]),
        "g_mix_fm": fm(inp["g_mix"][l]),
        "g_ffn_bc": rep(inp["g_ffn"][l]),
        "g_final_bc": rep(inp["g_final"]),
        "w_in": f(inp["w_in"][l]),
        "w_sconv_fm": f(np.asarray(inp["w_sconv"][l], np.float32).reshape(3, KC, 128).transpose(2, 1, 0).reshape(128, KC * 3)),
        "w_sconv_out": f(inp["w_sconv_out"][l]),
        "w_ssm_conv_fm": f(np.asarray(inp["w_ssm_conv"][l], np.float32).reshape(4, 24, 128).transpose(2, 1, 0).reshape(128, 96)),
        "b_ssm_conv_fm": fm(inp["b_ssm_conv"][l]),
        "dt_bias_bc": rep(inp["dt_bias"][l]),
        "a_log_bc": rep(inp["a_log"][l]),
        "d_skip_bc": rep(inp["d_skip"][l]),
        "g_ssm_bc": rep(inp["g_ssm_norm"][l]),
        "w_ssm_out": f(inp["w_ssm_out"][l]),
        "w_o": f(inp["w_o"][l]),
        "w_router": f(inp["w_router"][l]),
        "b_router_bc": rep(inp["b_router"][l]),
        "w_gu": f(np.asarray(inp["w_gu"][l], np.float32).reshape(NE * D, 2 * D)),
        "b_gu_l": f(np.asarray(inp["b_gu"][l], np.float32).reshape(NE, 16, 128).transpose(0, 2, 1).reshape(NE * 128, 16)),
        "w_down": f(np.asarray(inp["w_down"][l], np.float32).reshape(NE * D, D)),
        "b_down": f(inp["b_down"][l]),
    }
    return d


_NC_CACHE = {}
_STOP = 99
_JLIST = (0, 1)


def kernel(**inputs):
    x = np.asarray(inputs["x"], np.float32)
    c = np.asarray(inputs["c"], np.float32)
    bsz, seq, _ = x.shape
    ncores = 8
    nseq = bsz // ncores
    key = (nseq, seq)
    if key not in _NC_CACHE:
        _NC_CACHE[key] = build_nc(nseq, seq, _STOP)
    nc = _NC_CACHE[key]
    shared = _prep_shared(inputs)
    in_maps = []
    for i in range(ncores):
        m = dict(shared)
        m["x"] = np.ascontiguousarray(x[i * nseq:(i + 1) * nseq].reshape(nseq * seq, D))
        cc = c[i * nseq:(i + 1) * nseq]
        m["cT"] = np.ascontiguousarray(cc.reshape(nseq, KC, 128).transpose(2, 1, 0).reshape(128, KC * nseq))
        in_maps.append(m)
    res = run_bass_kernel_spmd(nc, in_maps, core_ids=list(range(ncores)))
    out = np.concatenate([np.asarray(r["out"]).reshape(nseq, seq, D) for r in res.results], axis=0)
    return out.astype(np.float32)
```

```python
import numpy as np
from contextlib import ExitStack
import concourse.bass as bass
import concourse.mybir as mybir
from concourse.bass_utils import run_bass_kernel_spmd

F32 = mybir.dt.float32
BF16 = mybir.dt.bfloat16
I32 = mybir.dt.int32
U32 = mybir.dt.uint32
ALU = mybir.AluOpType
AF = mybir.ActivationFunctionType
AX = mybir.AxisListType

ENGS = ("pe", "act", "dve", "pool", "sp")
SAME_ENGINE_SYNC = ("act", "dve", "pool")
SEM_EPOCH = 30000
LAZY = ("pe",)

D = 1024
KC = 8
DI = 2048
NH = 32
HP = 64
NG = 4
NS = 128
NCOL = 10272
C_SB, C_SC, C_SX, C_Z, C_XBC, C_DT, C_GA, C_GB = 0, 1024, 2048, 3072, 5120, 8192, 8224, 9248
NE = 32
TOPK = 4
BLK = 512
T = 256
EPS = 1e-6


class Buf:
    __slots__ = ("t", "w", "r", "sem", "semcnt", "name")

    def __init__(self, t, name=""):
        self.t = t
        self.w = None
        self.r = []
        self.sem = None
        self.semcnt = 0
        self.name = name


class Rot:
    def __init__(self, bufs):
        self.bufs = bufs
        self.i = 0

    def next(self):
        b = self.bufs[self.i % len(self.bufs)]
        self.i += 1
        return b


class Sched:
    def __init__(self, nc, stack):
        self.nc = nc
        self.stack = stack
        self.q = {e: [] for e in ENGS}
        self.nsem = 0
        self.dma_bufs = []
        self.esem = {}
        self.ecnt = {}
        self.allsems = {e: [] for e in ENGS}
        self.lastcell = {}
        for e in ENGS:
            self._new_epoch(e)
        self.waited = {e: {} for e in ENGS}

    def _new_sem(self, name):
        self.nsem += 1
        return self.stack.enter_context(self.nc.semaphore(f"{name}_{self.nsem}"))

    def _new_epoch(self, e):
        self.esem[e] = self._new_sem("e" + e)
        self.ecnt[e] = 0
        self.allsems[e].append(self.esem[e])

    def sbuf(self, name, shape, dt, stack=None):
        st = stack or self.stack
        return Buf(st.enter_context(self.nc.sbuf_tensor("s_" + name, list(shape), dt)), name)

    def psum(self, name, shape, dt):
        return Buf(self.stack.enter_context(self.nc.psum_tensor("p_" + name, list(shape), dt)), name)

    def _flush(self, e):
        cell = self.lastcell.get(e)
        if cell is None or cell["inc"]:
            return
        cell["inc"] = True
        self.ecnt[e] += 1
        self.lastcell[e] = None
        if self.ecnt[e] >= SEM_EPOCH:
            self._new_epoch(e)

    def _collect(self, eng, reads, writes):
        waits = {}

        def need(ev):
            if ev is None:
                return
            if ev[0] == "e":
                _, e2, sem, c = ev
                if e2 == eng and eng not in SAME_ENGINE_SYNC:
                    return
                if sem is self.esem[e2] and c > self.ecnt[e2]:
                    self._flush(e2)
                val = c
            else:
                sem = ev[1].sem
                val = ev[1].semcnt
            key = id(sem)
            if key not in waits or waits[key][1] < val:
                waits[key] = (sem, val)

        for b in reads:
            need(b.w)
        for b in writes:
            need(b.w)
            for ev in b.r:
                need(ev)
        out = []
        wd = self.waited[eng]
        for key, (sem, val) in waits.items():
            if wd.get(key, 0) >= val:
                continue
            wd[key] = val
            out.append((sem, val))
        return out

    def op(self, eng, fn, reads=(), writes=(), inc=True):
        waits = self._collect(eng, reads, writes)
        sem = self.esem[eng]
        cnt = self.ecnt[eng] + 1
        if eng in LAZY:
            cell = {"inc": False}
            self.lastcell[eng] = cell
        else:
            cell = {"inc": True}
            self.ecnt[eng] = cnt
            if cnt >= SEM_EPOCH:
                self._new_epoch(eng)

        def rec(E, waits=waits, fn=fn, sem=sem, cell=cell):
            for s, v in waits:
                E.wait_ge(s, v)
            ins = fn(E)
            if cell["inc"]:
                ins.then_inc(sem, 1)

        self.q[eng].append(rec)
        ev = ("e", eng, sem, cnt)
        for b in writes:
            b.w = ev
            b.r = []
        for b in reads:
            b.r.append(ev)

    def dma(self, eng, fn, owner, reads=(), writes=()):
        waits = self._collect(eng, reads, writes)
        if owner.sem is None:
            owner.sem = self._new_sem("d" + owner.name)
            self.dma_bufs.append(owner)
        owner.semcnt += 16
        sem = owner.sem

        def rec(E, waits=waits, fn=fn, sem=sem):
            for s, v in waits:
                E.wait_ge(s, v)
            fn(E).then_inc(sem, 16)

        self.q[eng].append(rec)
        ev = ("d", owner)
        for b in writes:
            b.w = ev
            b.r = []
        for b in reads:
            b.r.append(ev)

    def barrier(self):
        for e in ENGS:
            self._flush(e)
        for eng in ENGS:
            waits = []
            wd = self.waited[eng]
            for e2 in ENGS:
                if e2 == eng or self.ecnt[e2] == 0:
                    continue
                sem, val = self.esem[e2], self.ecnt[e2]
                if wd.get(id(sem), 0) < val:
                    wd[id(sem)] = val
                    waits.append((sem, val))
            for ob in self.dma_bufs:
                if wd.get(id(ob.sem), 0) < ob.semcnt:
                    wd[id(ob.sem)] = ob.semcnt
                    waits.append((ob.sem, ob.semcnt))

            def rec(E, waits=waits):
                for s, v in waits:
                    E.wait_ge(s, v)

            self.q[eng].append(rec)

    def emit(self):
        q = self.q
        with self.nc.Block() as block:
            @block.sync
            def _(E):
                for r in q["sp"]:
                    r(E)

            @block.tensor
            def _(E):
                for r in q["pe"]:
                    r(E)

            @block.scalar
            def _(E):
                for r in q["act"]:
                    r(E)

            @block.vector
            def _(E):
                for r in q["dve"]:
                    r(E)

            @block.gpsimd
            def _(E):
                for r in q["pool"]:
                    r(E)


def build_nc(NSEQ, SEQ, STOP=99):
    nc = bass.Bass("TRN2", target_bir_lowering=False)
    NT = NSEQ * SEQ
    NTT = NT // 128
    NTL = SEQ // T
    NROWS = NT * TOPK
    NBLK = NROWS // BLK + NE
    RTOT = NBLK * BLK

    def din(name, shape, dt=F32):
        return nc.dram_tensor(name, list(shape), dt, kind="ExternalInput").ap()

    def dscr(name, shape, dt):
        return nc.dram_tensor(name, list(shape), dt, kind="Internal").ap()

    x_d = din("x", [NT, D])
    cT_d = din("cT", [128, KC * NSEQ])
    wada_d = din("w_ada", [D, 6 * D])
    bada_fm_d = din("b_ada_fm", [128, 16])
    bada_bc_d = din("b_ada_bc", [128, 4 * D])
    gmix_fm_d = din("g_mix_fm", [128, KC])
    gffn_bc_d = din("g_ffn_bc", [128, D])
    gfin_bc_d = din("g_final_bc", [128, D])
    win_d = din("w_in", [D, NCOL])
    wsc_fm_d = din("w_sconv_fm", [128, KC * 3])
    wsco_d = din("w_sconv_out", [D, D])
    wxc_fm_d = din("w_ssm_conv_fm", [128, 24 * 4])
    bxc_fm_d = din("b_ssm_conv_fm", [128, 24])
    dtb_bc_d = din("dt_bias_bc", [128, NH])
    alog_bc_d = din("a_log_bc", [128, NH])
    dsk_bc_d = din("d_skip_bc", [128, NH])
    gssm_bc_d = din("g_ssm_bc", [128, DI])
    wsso_d = din("w_ssm_out", [DI, D])
    wo_d = din("w_o", [D, D])
    wr_d = din("w_router", [D, NE])
    br_bc_d = din("b_router_bc", [128, NE])
    wgu_d = din("w_gu", [NE * D, 2 * D])
    bgu_l_d = din("b_gu_l", [NE * 128, 16])
    wd_d = din("w_down", [NE * D, D])
    bd_d = din("b_down", [NE, D])
    out_d = nc.dram_tensor("out", [NT, D], F32, kind="ExternalOutput").ap()

    win_bf = dscr("win_bf", [D, NCOL], BF16)
    wsco_bf = dscr("wsco_bf", [D, D], BF16)
    wsso_bf = dscr("wsso_bf", [DI, D], BF16)
    wo_bf = dscr("wo_bf", [D, D], BF16)
    wg_bf = dscr("wg_bf", [NE * 128, KC * D], BF16)
    wu_bf = dscr("wu_bf", [NE * 128, KC * D], BF16)
    wd_bf = dscr("wd_bf", [NE * 128, KC * D], BF16)
    x1_h = dscr("x1_h", [NT, D], F32)
    h2_h = dscr("h2_h", [NT, D], BF16)
    xs_h = dscr("xs_h", [RTOT, D], BF16)
    ys_h = dscr("ys_h", [RTOT, D], F32)

    with ExitStack() as st:
        S = Sched(nc, st)
        def OP(eng, fn, r=(), w=()):
            S.op(eng, fn, reads=r, writes=w)

        def MM(outb, out_ap, pairs, reads):
            n = len(pairs)
            for i, (l, r_) in enumerate(pairs):
                S.op("pe", lambda E, l=l, r_=r_, i=i: E.matmul(out_ap, l, r_, start=(i == 0), stop=(i == n - 1)),
                     reads=reads, writes=[outb], inc=(i == n - 1))

        def TR(outb, out_ap, in_ap, idn, reads):
            S.op("pe", lambda E: E.transpose(out_ap, in_ap, idn.t[:]), reads=list(reads) + [idn], writes=[outb])

        def LOAD(eng, dst, dst_ap, src_ap, reads=()):
            S.dma(eng, lambda E: E.dma_start(out=dst_ap, in_=src_ap), dst, reads=reads, writes=[dst])

        open_stacks = []

        def finish():
            S.barrier()
            S.emit()
            for stx in reversed(open_stacks):
                stx.close()
            return nc

        B_win, B_wsco, B_wsso, B_wo = Buf(None, "winbf"), Buf(None, "wscobf"), Buf(None, "wssobf"), Buf(None, "wobf")
        B_wgu, B_wd = Buf(None, "wgubf"), Buf(None, "wdbf")
        B_x1, B_h2, B_xs, B_ys, B_out = Buf(None, "x1h"), Buf(None, "h2h"), Buf(None, "xsh"), Buf(None, "ysh"), Buf(None, "outh")

        for kc in range(KC):
            S.dma("pool", lambda E, kc=kc: E.dma_start(out=win_bf[kc * 128:(kc + 1) * 128, :], in_=win_d[kc * 128:(kc + 1) * 128, :]),
                  B_win, writes=[B_win])
        for (dst, src, bb, rows) in ((wsco_bf, wsco_d, B_wsco, D), (wsso_bf, wsso_d, B_wsso, DI), (wo_bf, wo_d, B_wo, D)):
            for r0 in range(0, rows, 512):
                S.dma("pool", lambda E, dst=dst, src=src, r0=r0: E.dma_start(out=dst[r0:r0 + 512, :], in_=src[r0:r0 + 512, :]),
                      bb, writes=[bb])

        G = [S.psum(f"G{i}", [128, 512], F32) for i in range(5)]
        ptb = S.psum("ptb", [128, 2048], BF16)
        ptB = S.psum("ptB", [128, 1024], BF16)
        Grot = Rot(G)

        io = S.sbuf("io", [128, 128], F32)
        ident = S.sbuf("ident", [128, 128], F32)
        identb = S.sbuf("identb", [128, 128], BF16)
        triu = S.sbuf("triu", [128, 128], F32)
        ones = S.sbuf("ones", [128, 128], F32)
        onesb = S.sbuf("onesb", [128, 128], BF16)
        tristb = S.sbuf("tristb", [128, 128], BF16)
        epst = S.sbuf("epst", [128, 1], F32)
        iota32 = S.sbuf("iota32", [128, NE], F32)
        OP("pool", lambda E: E.iota(io.t[:], pattern=[[1, 128]], base=0, channel_multiplier=-1,
                                    allow_small_or_imprecise_dtypes=True), w=[io])
        OP("pool", lambda E: E.iota(iota32.t[:], pattern=[[1, NE]], base=0, channel_multiplier=0,
                                    allow_small_or_imprecise_dtypes=True), w=[iota32])
        OP("dve", lambda E: E.tensor_single_scalar(ident.t[:], io.t[:], 0.0, ALU.is_equal), r=[io], w=[ident])
        OP("dve", lambda E: E.tensor_single_scalar(identb.t[:], io.t[:], 0.0, ALU.is_equal), r=[io], w=[identb])
        OP("dve", lambda E: E.tensor_single_scalar(triu.t[:], io.t[:], 0.0, ALU.is_ge), r=[io], w=[triu])
        OP("dve", lambda E: E.tensor_single_scalar(tristb.t[:], io.t[:], 0.0, ALU.is_gt), r=[io], w=[tristb])
        OP("dve", lambda E: E.memset(ones.t[:], 1.0), w=[ones])
        OP("dve", lambda E: E.memset(onesb.t[:], 1.0), w=[onesb])
        OP("dve", lambda E: E.memset(epst.t[:], EPS), w=[epst])

        def small(name, shape, src, dt=F32):
            b = S.sbuf(name, shape, dt)
            LOAD("sp", b, b.t[:], src)
            return b

        cT = small("cT", [128, KC * NSEQ], cT_d)
        bada_fm = small("bada_fm", [128, 16], bada_fm_d)
        gmix_fm = small("gmix_fm", [128, KC], gmix_fm_d)
        wsc_fm = small("wsc_fm", [128, KC * 3], wsc_fm_d)
        wxc_fm = small("wxc_fm", [128, 96], wxc_fm_d)
        bxc_fm = small("bxc_fm", [128, 24], bxc_fm_d)
        dtb_bc = small("dtb_bc", [128, NH], dtb_bc_d)
        a_bc = small("a_bc", [128, NH], alog_bc_d)
        dsk_bc = small("dsk_bc", [128, NH], dsk_bc_d)
        br_bc = small("br_bc", [128, NE], br_bc_d)
        wr_sb = S.sbuf("wr_sb", [128, KC, NE], F32)
        LOAD("sp", wr_sb, wr_sb.t[:], wr_d.rearrange("(kc p) n -> p kc n", p=128))
        wdt_sb = S.sbuf("wdt_sb", [128, KC, NH], BF16)
        OP("act", lambda E: E.activation(a_bc.t[:], a_bc.t[:], AF.Exp), r=[a_bc], w=[a_bc])
        OP("dve", lambda E: E.tensor_single_scalar(a_bc.t[:], a_bc.t[:], -1.0, ALU.mult), r=[a_bc], w=[a_bc])
        OP("act", lambda E: E.activation(cT.t[:], cT.t[:], AF.Silu), r=[cT], w=[cT])

        gs1 = S.sbuf("gs1", [128, KC * NSEQ], F32)
        sh1 = S.sbuf("sh1", [128, KC * NSEQ], F32)
        wts = S.sbuf("wts", [128, NTT * TOPK], F32)
        dest_i = S.sbuf("dest_i", [128, NTT * TOPK], I32)
        wrow = S.sbuf("wrow", [128, NBLK * KC], I32)
        brow = S.sbuf("brow", [128, NBLK], I32)
        erow = S.sbuf("erow", [128, NBLK], I32)
        plog = ExitStack()
        p1 = ExitStack()
        open_stacks.extend([plog, p1])
        logits = S.sbuf("logits", [128, NTT * NE], F32, plog)
        wadab = Rot([S.sbuf(f"wadab{i}", [128, KC, 128], F32, p1) for i in range(2)])
        badab = Rot([S.sbuf(f"badab{i}", [128, 128], F32, p1) for i in range(2)])
        condrep = S.sbuf("condrep", [128, KC, 128], F32, p1)
        bct = S.sbuf("bct", [128, 3 * D], F32, p1)
        gffn_bc = S.sbuf("gffn_bc", [128, D], F32, p1)
        LOAD("sp", gffn_bc, gffn_bc.t[:], gffn_bc_d)
        M = {"wadab": wadab, "badab": badab, "condrep": condrep, "bct": bct, "off": 0}
        wada_r = wada_d.rearrange("(kc p) n -> p kc n", p=128)

        for ch in range(16):
            wb = wadab.next()
            LOAD("sp", wb, wb.t[:], wada_r[:, :, ch * 128:(ch + 1) * 128])
            for jj in range(1):
                j = ch
                g = Grot.next()
                MM(g, g.t[:, 0:NSEQ], [(wb.t[:, kc, jj * 128:(jj + 1) * 128], cT.t[:, kc * NSEQ:(kc + 1) * NSEQ]) for kc in range(KC)], [wb, cT])
                if j < 8:
                    for b in range(NSEQ):
                        OP("act", lambda E, g=g, j=j, b=b: E.activation(sh1.t[:, j * NSEQ + b:j * NSEQ + b + 1], g.t[:, b:b + 1], AF.Identity,
                                                                         bias=bada_fm.t[:, j:j + 1], scale=1.0), r=[g, bada_fm], w=[sh1])
                else:
                    c = j - 8
                    for b in range(NSEQ):
                        OP("act", lambda E, g=g, j=j, b=b, c=c: E.activation(gs1.t[:, c * NSEQ + b:c * NSEQ + b + 1], g.t[:, b:b + 1], AF.Identity,
                                                                              bias=bada_fm.t[:, j:j + 1], scale=1.0), r=[g, bada_fm], w=[gs1])
                        OP("dve", lambda E, b=b, c=c: E.tensor_scalar(gs1.t[:, c * NSEQ + b:c * NSEQ + b + 1], gs1.t[:, c * NSEQ + b:c * NSEQ + b + 1],
                                                                     1.0, gmix_fm.t[:, c:c + 1], ALU.add, ALU.mult), r=[gs1, gmix_fm], w=[gs1])

        def mod_bc(b, chunks):
            wadab, badab, condrep, bct, off = M["wadab"], M["badab"], M["condrep"], M["bct"], M["off"]
            for kc in range(KC):
                OP("dve", lambda E, kc=kc: E.tensor_copy(condrep.t[:, kc, :], cT.t[:, kc * NSEQ + b:kc * NSEQ + b + 1].to_broadcast([128, 128])),
                   r=[cT], w=[condrep])
            for ch in chunks:
                wb = wadab.next()
                bb = badab.next()
                LOAD("sp", wb, wb.t[:], wada_r[:, :, 2 * D + ch * 128:2 * D + (ch + 1) * 128])
                LOAD("sp", bb, bb.t[:], bada_bc_d[:, ch * 128:(ch + 1) * 128])
                g = Grot.next()
                MM(g, g.t[:, 0:128], [(condrep.t[:, kc, :], wb.t[:, kc, :]) for kc in range(KC)], [wb, condrep])
                dsl = slice((ch - off) * 128, (ch - off + 1) * 128)
                OP("dve", lambda E, g=g, bb=bb, dsl=dsl: E.tensor_tensor(bct.t[:, dsl], g.t[:, 0:128], bb.t[:], ALU.add),
                   r=[g, bb], w=[bct])
                if 16 <= ch < 24:
                    OP("dve", lambda E, ch=ch, dsl=dsl: E.scalar_tensor_tensor(bct.t[:, dsl], bct.t[:, dsl], 1.0,
                                                                      gffn_bc.t[:, (ch - 16) * 128:(ch - 15) * 128], ALU.add, ALU.mult),
                       r=[bct, gffn_bc], w=[bct])

        GT1, SH2, GS2, GT2 = 0, D, 2 * D, 0
        if STOP == 0:
            return finish()

        gssm_bc = S.sbuf("gssm_bc", [128, DI], F32, p1)
        LOAD("sp", gssm_bc, gssm_bc.t[:], gssm_bc_d)
        LOAD("sp", wdt_sb, wdt_sb.t[:], win_bf.rearrange("(kc p) n -> p kc n", p=128)[:, :, C_DT:C_DT + NH], reads=[B_win])
        wst = Rot([S.sbuf(f"wst{i}", [128, KC, 512], BF16, p1) for i in range(4)])
        xr = Rot([S.sbuf(f"xr{i}", [128, D], F32, p1) for i in range(2)])
        junk = S.sbuf("junk", [128, D], BF16, p1)
        ss = S.sbuf("ss", [128, 4], F32, p1)
        rstd = S.sbuf("rstd", [128, 4], F32, p1)
        hT = S.sbuf("hT", [128, KC, T], BF16, p1)
        vT = S.sbuf("vT", [128, KC, T], BF16, p1)
        tmpA = Rot([S.sbuf(f"tmpA{i}", [128, T], F32, p1) for i in range(2)])
        tmpY = Rot([S.sbuf(f"tmpY{i}", [128, T], F32, p1) for i in range(2)])
        utmp = Rot([S.sbuf(f"utmp{i}", [128, T + 2], F32, p1) for i in range(2)])
        uhalo = S.sbuf("uhalo", [128, KC, 2], F32, p1)
        xraw = Rot([S.sbuf(f"xraw{i}", [128, T + 3], F32, p1) for i in range(2)])
        xhalo = S.sbuf("xhalo", [128, 24, 3], F32, p1)
        xsT = S.sbuf("xsT", [128, 16, T], BF16, p1)
        BT = S.sbuf("BT", [128, NG, T], BF16, p1)
        CT = S.sbuf("CT", [128, NG, T], BF16, p1)
        zs = S.sbuf("zs", [128, 2, DI], BF16, p1)
        dtt = S.sbuf("dtt", [128, 2, NH], F32, p1)
        dat = S.sbuf("dat", [128, 2, NH], F32, p1)
        dtmp = S.sbuf("dtmp", [128, NH], F32, p1)
        acum = S.sbuf("acum", [128, NH], F32, p1)
        dend = S.sbuf("dend", [128, NH], F32, p1)
        dchunk = S.sbuf("dchunk", [128, NH], F32, p1)
        eacum = S.sbuf("eacum", [128, NH], F32, p1)
        xs_tok = S.sbuf("xs_tok", [128, DI], BF16, p1)
        xdt = S.sbuf("xdt", [128, DI], BF16, p1)
        xdte = S.sbuf("xdte", [128, DI], BF16, p1)
        B_tok = S.sbuf("B_tok", [128, NG * NS], BF16, p1)
        CBm = S.sbuf("CBm", [128, NG, 128], F32, p1)
        seg = Rot([S.sbuf(f"seg{i}", [128, 4, 128], F32, p1) for i in range(2)])
        Sb = S.sbuf("Sb", [128, 8, 128], BF16, p1)
        st32 = S.sbuf("st32", [128, DI], F32, p1)
        stbf = S.sbuf("stbf", [128, DI], BF16, p1)
        yg = Rot([S.sbuf(f"yg{i}", [128, 512], F32, p1) for i in range(2)])
        yt2 = Rot([S.sbuf(f"yt2{i}", [128, 512], F32, p1) for i in range(1)])
        yn = Rot([S.sbuf(f"yn{i}", [128, 512], BF16, p1) for i in range(2)])
        ssg = S.sbuf("ssg", [128, 2], F32, p1)
        ynT = S.sbuf("ynT", [128, 16, T], BF16, p1)
        mpart = S.sbuf("mpart", [128, 4, T], F32, p1)
        mT = S.sbuf("mT", [128, KC, T], BF16, p1)
        h2 = S.sbuf("h2", [128, D], F32, p1)
        h2b = Rot([S.sbuf(f"h2b{i}", [128, D], BF16, p1) for i in range(1)])
        h2T = S.sbuf("h2T", [128, KC, 128], F32, p1)

        win_r = win_bf.rearrange("(kc p) n -> p kc n", p=128)
        wsco_r = wsco_bf.rearrange("(kc p) n -> p kc n", p=128)
        wsso_r = wsso_bf.rearrange("(kc p) n -> p kc n", p=128)
        wo_r = wo_bf.rearrange("(kc p) n -> p kc n", p=128)

        def wload(src_r, bb, c0, k0=0):
            w = wst.next()
            LOAD("sp", w, w.t[:], src_r[:, k0:k0 + KC, c0:c0 + 512], reads=[bb])
            return w

        def rms_stats(src, col, n):
            OP("act", lambda E: E.activation(junk.t[:, 0:n], src, AF.Square, accum_out=ss.t[:, col:col + 1]), r=[], w=[junk, ss])
            OP("act", lambda E: E.activation(ss.t[:, col:col + 1], ss.t[:, col:col + 1], AF.Sqrt, bias=epst.t[:, 0:1], scale=1.0 / n),
               r=[ss, epst], w=[ss])
            OP("dve", lambda E: E.reciprocal(rstd.t[:, col:col + 1], ss.t[:, col:col + 1]), r=[ss], w=[rstd])

        cast_jobs = []
        for e in range(NE):
            cast_jobs.append((wg_bf, wgu_d, e, 0, B_wgu))
            cast_jobs.append((wu_bf, wgu_d, e, D, B_wgu))
            cast_jobs.append((wd_bf, wd_d, e, 0, B_wd))

        def issue_casts(n):
            for _ in range(n):
                if not cast_jobs:
                    return
                dst, src, e, c0, bb = cast_jobs.pop(0)
                S.dma("pool", lambda E, dst=dst, src=src, e=e, c0=c0: E.dma_start(
                    out=dst[e * 128:(e + 1) * 128, :].rearrange("p (k n) -> p k n", n=D),
                    in_=src[e * D:(e + 1) * D, c0:c0 + D].rearrange("(k p) n -> p k n", p=128)), bb, writes=[bb])

        casts_per_tile = -(-len(cast_jobs) // (NSEQ * NTL))
        zt = S.sbuf("zt", [128, 2 * D], BF16, p1)
        OP("pool", lambda E: E.memset(zt.t[:], 0.0), w=[zt])
        B_xz = Buf(None, "xsz")
        zero_jobs = list(range(0, RTOT, 256))

        def issue_zero(n):
            for _ in range(n):
                if not zero_jobs:
                    return
                q0 = zero_jobs.pop(0)
                S.dma("sp", lambda E, q0=q0: E.dma_start(out=xs_h[q0:q0 + 256, :].rearrange("(p r) d -> p (r d)", r=2), in_=zt.t[:]),
                      B_xz, reads=[zt], writes=[])
            B_xz.w = ("d", B_xz)

        zeros_per_tile = -(-len(zero_jobs) // (NSEQ * NTL))
        for b in range(NSEQ):
            mod_bc(b, range(0, 24))
            OP("dve", lambda E: E.memset(st32.t[:], 0.0), w=[st32])
            OP("dve", lambda E: E.memset(stbf.t[:], 0.0), w=[stbf])
            OP("dve", lambda E: E.memset(uhalo.t[:], 0.0), w=[uhalo])
            OP("dve", lambda E: E.memset(xhalo.t[:], 0.0), w=[xhalo])
            for it in range(NTL):
                r0 = b * SEQ + it * T
                xs_ = []
                for j in range(2):
                    xb = xr.next()
                    xs_.append(xb)
                    LOAD("sp", xb, xb.t[:], x_d[r0 + j * 128:r0 + (j + 1) * 128, :])
                    OP("act", lambda E, xb=xb, j=j: E.activation(junk.t[:], xb.t[:], AF.Square, accum_out=ss.t[:, j:j + 1]), r=[xb], w=[junk, ss])
                    OP("act", lambda E, j=j: E.activation(ss.t[:, j:j + 1], ss.t[:, j:j + 1], AF.Sqrt, bias=epst.t[:, 0:1], scale=1.0 / D),
                       r=[ss, epst], w=[ss])
                    OP("dve", lambda E, j=j: E.reciprocal(rstd.t[:, j:j + 1], ss.t[:, j:j + 1]), r=[ss], w=[rstd])
                    OP("dve", lambda E, xb=xb, j=j: E.tensor_scalar(xb.t[:], xb.t[:], rstd.t[:, j:j + 1], None, ALU.mult), r=[xb, rstd], w=[xb])
                for c in range(KC):
                    g = Grot.next()
                    for j in range(2):
                        TR(g, g.t[:, j * 128:(j + 1) * 128], xs_[j].t[:, c * 128:(c + 1) * 128], ident, [xs_[j]])
                    OP("act", lambda E, g=g, c=c, b=b: E.activation(hT.t[:, c, :], g.t[:, 0:T], AF.Identity,
                                                               bias=sh1.t[:, c * NSEQ + b:c * NSEQ + b + 1],
                                                               scale=gs1.t[:, c * NSEQ + b:c * NSEQ + b + 1]), r=[g, sh1, gs1], w=[hT])

                def proj_fm(w, cc):
                    g = Grot.next()
                    MM(g, g.t[:, 0:T], [(w.t[:, kc, cc * 128:(cc + 1) * 128], hT.t[:, kc, :]) for kc in range(KC)], [w, hT])
                    return g

                if STOP == 11:
                    return finish()
                for q in range(2):
                    w_sb = wload(win_r, B_win, C_SB + q * 512)
                    w_sx = wload(win_r, B_win, C_SX + q * 512)
                    w_sc = wload(win_r, B_win, C_SC + q * 512)
                    for cc in range(4):
                        c = q * 4 + cc
                        g_sb = proj_fm(w_sb, cc)
                        g_sx = proj_fm(w_sx, cc)
                        ta = tmpA.next()
                        OP("act", lambda E, ta=ta, g=g_sx: E.activation(ta.t[:], g.t[:, 0:T], AF.Copy), r=[g_sx], w=[ta])
                        u = utmp.next()
                        OP("pool", lambda E, u=u, c=c: E.tensor_copy(u.t[:, 0:2], uhalo.t[:, c, :]), r=[uhalo], w=[u])
                        OP("dve", lambda E, u=u, g=g_sb, ta=ta: E.tensor_tensor(u.t[:, 2:T + 2], g.t[:, 0:T], ta.t[:], ALU.mult), r=[g_sb, ta], w=[u])
                        OP("pool", lambda E, u=u, c=c: E.tensor_copy(uhalo.t[:, c, :], u.t[:, T:T + 2]), r=[u], w=[uhalo])
                        g_sc = proj_fm(w_sc, cc)
                        y = tmpY.next()
                        OP("pool", lambda E, y=y, u=u, c=c: E.tensor_scalar(y.t[:], u.t[:, 2:T + 2], wsc_fm.t[:, c * 3 + 2:c * 3 + 3], None, ALU.mult),
                           r=[u, wsc_fm], w=[y])
                        OP("dve", lambda E, y=y, u=u, c=c: E.scalar_tensor_tensor(y.t[:], u.t[:, 1:T + 1], wsc_fm.t[:, c * 3 + 1:c * 3 + 2], y.t[:], ALU.mult, ALU.add),
                           r=[u, wsc_fm, y], w=[y])
                        OP("dve", lambda E, y=y, u=u, c=c: E.scalar_tensor_tensor(y.t[:], u.t[:, 0:T], wsc_fm.t[:, c * 3:c * 3 + 1], y.t[:], ALU.mult, ALU.add),
                           r=[u, wsc_fm, y], w=[y])
                        OP("dve", lambda E, y=y, g=g_sc, c=c: E.tensor_tensor(vT.t[:, c, :], g.t[:, 0:T], y.t[:], ALU.mult), r=[g_sc, y], w=[vT])

                if STOP == 12:
                    return finish()
                for zq in range(4):
                    w_z = wload(win_r, B_win, C_Z + zq * 512)
                    for j in range(2):
                        g = Grot.next()
                        MM(g, g.t[:], [(hT.t[:, kc, j * 128:(j + 1) * 128], w_z.t[:, kc, :]) for kc in range(KC)], [w_z, hT])
                        OP("act", lambda E, g=g, j=j, zq=zq: E.activation(zs.t[:, j, zq * 512:(zq + 1) * 512], g.t[:], AF.Silu), r=[g], w=[zs])
                for j in range(2):
                    g = Grot.next()
                    MM(g, g.t[:, 0:NH], [(hT.t[:, kc, j * 128:(j + 1) * 128], wdt_sb.t[:, kc, :]) for kc in range(KC)], [wdt_sb, hT])
                    OP("dve", lambda E, g=g: E.tensor_tensor(dtmp.t[:], g.t[:, 0:NH], dtb_bc.t[:], ALU.add), r=[g, dtb_bc], w=[dtmp])
                    OP("act", lambda E: E.activation(dtmp.t[:], dtmp.t[:], AF.Exp), r=[dtmp], w=[dtmp])
                    OP("act", lambda E, j=j: E.activation(dtt.t[:, j, :], dtmp.t[:], AF.Ln, bias=1.0, scale=1.0), r=[dtmp], w=[dtt])
                    OP("dve", lambda E, j=j: E.tensor_tensor(dat.t[:, j, :], dtt.t[:, j, :], a_bc.t[:], ALU.mult), r=[dtt, a_bc], w=[dat])
                for xq in range(6):
                    w_x = wload(win_r, B_win, C_XBC + xq * 512)
                    for cc in range(4):
                        cx = xq * 4 + cc
                        g = proj_fm(w_x, cc)
                        xw = xraw.next()
                        OP("pool", lambda E, xw=xw, cx=cx: E.tensor_copy(xw.t[:, 0:3], xhalo.t[:, cx, :]), r=[xhalo], w=[xw])
                        OP("act", lambda E, xw=xw, g=g: E.activation(xw.t[:, 3:T + 3], g.t[:, 0:T], AF.Copy), r=[g], w=[xw])
                        OP("pool", lambda E, xw=xw, cx=cx: E.tensor_copy(xhalo.t[:, cx, :], xw.t[:, T:T + 3]), r=[xw], w=[xhalo])
                        y = tmpY.next()
                        OP("dve", lambda E, y=y, xw=xw, cx=cx: E.tensor_scalar(y.t[:], xw.t[:, 3:T + 3], wxc_fm.t[:, cx * 4 + 3:cx * 4 + 4], None, ALU.mult),
                           r=[xw, wxc_fm], w=[y])
                        for k in (2, 1, 0):
                            eng = "dve"
                            OP(eng, lambda E, y=y, xw=xw, cx=cx, k=k: E.scalar_tensor_tensor(y.t[:], xw.t[:, k:k + T], wxc_fm.t[:, cx * 4 + k:cx * 4 + k + 1], y.t[:],
                                                                                           ALU.mult, ALU.add), r=[xw, wxc_fm, y], w=[y])
                        if cx < 16:
                            dstb, dst = xsT, xsT.t[:, cx, :]
                        elif cx < 20:
                            dstb, dst = BT, BT.t[:, cx - 16, :]
                        else:
                            dstb, dst = CT, CT.t[:, cx - 20, :]
                        OP("act", lambda E, y=y, dst=dst, cx=cx: E.activation(dst, y.t[:], AF.Silu, bias=bxc_fm.t[:, cx:cx + 1], scale=1.0),
                           r=[y, bxc_fm], w=[dstb])

                if STOP == 13:
                    return finish()
                issue_zero(zeros_per_tile)
                for jpos, j in enumerate(_JLIST):
                    SD = (STOP - 100 * jpos) if 141 + 100 * jpos <= STOP <= 148 + 100 * jpos else -1
                    js = slice(j * 128, (j + 1) * 128)
                    g0 = G[0]
                    MM(g0, g0.t[:, 0:NH], [(triu.t[:], dat.t[:, j, :])], [triu, dat])
                    MM(g0, g0.t[:, NH:2 * NH], [(ones.t[:], dat.t[:, j, :])], [ones, dat])
                    OP("dve", lambda E: E.tensor_copy(acum.t[:], g0.t[:, 0:NH]), r=[g0], w=[acum])
                    OP("dve", lambda E: E.tensor_tensor(dend.t[:], g0.t[:, NH:2 * NH], acum.t[:], ALU.subtract), r=[g0, acum], w=[dend])
                    OP("act", lambda E: E.activation(dend.t[:], dend.t[:], AF.Exp), r=[dend], w=[dend])
                    OP("act", lambda E: E.activation(dchunk.t[:], g0.t[:, NH:2 * NH], AF.Exp), r=[g0], w=[dchunk])
                    OP("act", lambda E: E.activation(eacum.t[:], acum.t[:], AF.Exp), r=[acum], w=[eacum])
                    if STOP == 240 and jpos == 1:
                        return finish()
                    for cx in range(16):
                        TR(ptb, ptb.t[:, cx * 128:(cx + 1) * 128], xsT.t[:, cx, js], identb, [xsT])
                    if STOP == 2401 and jpos == 1:
                        return finish()
                    for g_ in range(NG):
                        TR(ptB, ptB.t[:, g_ * 128:(g_ + 1) * 128], BT.t[:, g_, js], identb, [BT])
                    if STOP == 2402 and jpos == 1:
                        return finish()
                    OP("act", lambda E: E.activation(xs_tok.t[:], ptb.t[:], AF.Copy), r=[ptb], w=[xs_tok])
                    if STOP == 2403 and jpos == 1:
                        return finish()
                    OP("dve", lambda E, j=j: E.tensor_tensor(xdt.t[:].rearrange("p (h q) -> p h q", q=HP), xs_tok.t[:].rearrange("p (h q) -> p h q", q=HP),
                                                             dtt.t[:, j, :].unsqueeze(2).to_broadcast([128, NH, HP]), ALU.mult), r=[xs_tok, dtt], w=[xdt])
                    if STOP == 2404 and jpos == 1:
                        return finish()
                    OP("dve", lambda E: E.tensor_tensor(xdte.t[:].rearrange("p (h q) -> p h q", q=HP), xdt.t[:].rearrange("p (h q) -> p h q", q=HP),
                                                         dend.t[:].unsqueeze(2).to_broadcast([128, NH, HP]), ALU.mult), r=[xdt, dend], w=[xdte])
                    OP("act", lambda E: E.activation(B_tok.t[:], ptB.t[:, 0:NG * NS], AF.Copy), r=[ptB], w=[B_tok])
                    if SD == 141:
                        return finish()
                    g1 = G[1]
                    for g_ in range(NG):
                        MM(g1, g1.t[:, g_ * 128:(g_ + 1) * 128], [(BT.t[:, g_, js], CT.t[:, g_, js])], [BT, CT])
                    OP("dve", lambda E: E.tensor_tensor(CBm.t[:], g1.t[:].rearrange("p (g i) -> p g i", i=128),
                                                        triu.t[:].unsqueeze(1).to_broadcast([128, NG, 128]), ALU.mult), r=[g1, triu], w=[CBm])
                    if SD == 142:
                        return finish()
                    for g_ in range(NG):
                        gsl = slice(g_ * 512, (g_ + 1) * 512)
                        for half in range(2):
                            ga_ = G[2 + half]
                            for hh in range(4):
                                h = g_ * 8 + half * 4 + hh
                                MM(ga_, ga_.t[:, hh * 128:(hh + 1) * 128], [(dat.t[:, j, h:h + 1].to_broadcast([128, 128]), triu.t[:])], [dat, triu])
                            sg_ = seg.next()
                            h0 = g_ * 8 + half * 4
                            OP("dve", lambda E, ga_=ga_, sg_=sg_, h0=h0: E.tensor_tensor(sg_.t[:], ga_.t[:].rearrange("p (h i) -> p h i", i=128),
                                                                                       acum.t[:, h0:h0 + 4].unsqueeze(2).to_broadcast([128, 4, 128]), ALU.subtract),
                               r=[ga_, acum], w=[sg_])
                            OP("act", lambda E, sg_=sg_: E.activation(sg_.t[:], sg_.t[:], AF.Relu, scale=-1.0), r=[sg_], w=[sg_])
                            OP("act", lambda E, sg_=sg_: E.activation(sg_.t[:], sg_.t[:], AF.Exp, scale=-1.0), r=[sg_], w=[sg_])
                            OP("dve", lambda E, sg_=sg_, half=half, g_=g_: E.tensor_tensor(Sb.t[:, half * 4:half * 4 + 4, :], sg_.t[:],
                                                                                         CBm.t[:, g_:g_ + 1, :].to_broadcast([128, 4, 128]), ALU.mult),
                               r=[sg_, CBm], w=[Sb])
                        if SD == 143:
                            return finish()
                        g4 = G[4]
                        for h8 in range(8):
                            h = g_ * 8 + h8
                            MM(g4, g4.t[:, h8 * HP:(h8 + 1) * HP], [(Sb.t[:, h8, :], xdt.t[:, h * HP:(h + 1) * HP])], [Sb, xdt])
                        MM(g0, g0.t[:], [(CT.t[:, g_, js], stbf.t[:, gsl])], [CT, stbf])
                        y_ = yg.next()
                        OP("dve", lambda E, y_=y_, g_=g_: E.tensor_tensor(y_.t[:].rearrange("p (h q) -> p h q", q=HP), g0.t[:].rearrange("p (h q) -> p h q", q=HP),
                                                                         eacum.t[:, g_ * 8:(g_ + 1) * 8].unsqueeze(2).to_broadcast([128, 8, HP]), ALU.mult),
                           r=[g0, eacum], w=[y_])
                        OP("dve", lambda E, y_=y_: E.tensor_tensor(y_.t[:], g4.t[:], y_.t[:], ALU.add), r=[g4, y_], w=[y_])
                        t2 = yt2.next()
                        OP("dve", lambda E, t2=t2, g_=g_, gsl=gsl: E.tensor_tensor(t2.t[:].rearrange("p (h q) -> p h q", q=HP), xs_tok.t[:, gsl].rearrange("p (h q) -> p h q", q=HP),
                                                                                  dsk_bc.t[:, g_ * 8:(g_ + 1) * 8].unsqueeze(2).to_broadcast([128, 8, HP]), ALU.mult),
                           r=[xs_tok, dsk_bc], w=[t2])
                        OP("dve", lambda E, t2=t2, y_=y_: E.tensor_tensor(y_.t[:], y_.t[:], t2.t[:], ALU.add), r=[t2, y_], w=[y_])
                        if SD == 144:
                            return finish()
                        OP("dve", lambda E, y_=y_, j=j, gsl=gsl: E.tensor_tensor(y_.t[:], y_.t[:], zs.t[:, j, gsl], ALU.mult), r=[y_, zs], w=[y_])
                        OP("act", lambda E, y_=y_: E.activation(junk.t[:, 0:512], y_.t[:], AF.Square, accum_out=ssg.t[:, 0:1]), r=[y_], w=[junk, ssg])
                        OP("act", lambda E: E.activation(ssg.t[:, 0:1], ssg.t[:, 0:1], AF.Sqrt, bias=epst.t[:, 0:1], scale=1.0 / 512), r=[ssg, epst], w=[ssg])
                        OP("dve", lambda E: E.reciprocal(ssg.t[:, 1:2], ssg.t[:, 0:1]), r=[ssg], w=[ssg])
                        yn_ = yn.next()
                        OP("dve", lambda E, y_=y_, yn_=yn_, gsl=gsl: E.scalar_tensor_tensor(yn_.t[:], y_.t[:], ssg.t[:, 1:2], gssm_bc.t[:, gsl], ALU.mult, ALU.mult),
                           r=[y_, ssg, gssm_bc], w=[yn_])
                        for q in range(4):
                            TR(ptB, ptB.t[:, 512 + q * 128:512 + (q + 1) * 128], yn_.t[:, q * 128:(q + 1) * 128], identb, [yn_])
                        OP("act", lambda E, g_=g_, js=js: E.activation(ynT.t[:, g_ * 4:(g_ + 1) * 4, js], ptB.t[:, 512:1024].rearrange("p (q t) -> p q t", t=128), AF.Copy),
                           r=[ptB], w=[ynT])
                        if SD == 145:
                            return finish()
                        MM(g1, g1.t[:], [(B_tok.t[:, g_ * 128:(g_ + 1) * 128], xdte.t[:, gsl])], [B_tok, xdte])
                        OP("dve", lambda E, g_=g_, gsl=gsl: E.tensor_tensor(st32.t[:, gsl].rearrange("p (h q) -> p h q", q=HP), st32.t[:, gsl].rearrange("p (h q) -> p h q", q=HP),
                                                                           dchunk.t[:, g_ * 8:(g_ + 1) * 8].unsqueeze(2).to_broadcast([128, 8, HP]), ALU.mult),
                           r=[st32, dchunk], w=[st32])
                        OP("dve", lambda E, gsl=gsl: E.tensor_tensor(st32.t[:, gsl], g1.t[:], st32.t[:, gsl], ALU.add), r=[g1, st32], w=[st32])
                        OP("act", lambda E, gsl=gsl: E.activation(stbf.t[:, gsl], st32.t[:, gsl], AF.Copy), r=[st32], w=[stbf])
                        if SD == 146:
                            return finish()
                        if SD == 147 and g_ == 1:
                            return finish()
                    if SD == 148:
                        return finish()

                if STOP == 14:
                    return finish()
                issue_casts(casts_per_tile)
                for dq in range(2):
                    w_a = wload(wsco_r, B_wsco, dq * 512)
                    w_ga = wload(win_r, B_win, C_GA + dq * 512)
                    for dcc in range(4):
                        g_y = Grot.next()
                        MM(g_y, g_y.t[:, 0:T], [(w_a.t[:, kc, dcc * 128:(dcc + 1) * 128], vT.t[:, kc, :]) for kc in range(KC)], [w_a, vT])
                        g_g = proj_fm(w_ga, dcc)
                        ta = tmpA.next()
                        OP("act", lambda E, ta=ta, g=g_g: E.activation(ta.t[:], g.t[:, 0:T], AF.Sigmoid), r=[g_g], w=[ta])
                        OP("dve", lambda E, ta=ta, g=g_y, dcc=dcc: E.tensor_tensor(mpart.t[:, dcc, :], g.t[:, 0:T], ta.t[:], ALU.mult), r=[g_y, ta], w=[mpart])
                    w_b0 = wload(wsso_r, B_wsso, dq * 512, 0)
                    w_b1 = wload(wsso_r, B_wsso, dq * 512, 8)
                    w_gb = wload(win_r, B_win, C_GB + dq * 512)
                    for dcc in range(4):
                        g_y = Grot.next()
                        MM(g_y, g_y.t[:, 0:T], [(w_b0.t[:, kc, dcc * 128:(dcc + 1) * 128], ynT.t[:, kc, :]) for kc in range(KC)] +
                           [(w_b1.t[:, kc, dcc * 128:(dcc + 1) * 128], ynT.t[:, 8 + kc, :]) for kc in range(KC)], [w_b0, w_b1, ynT])
                        g_g = proj_fm(w_gb, dcc)
                        ta = tmpA.next()
                        OP("act", lambda E, ta=ta, g=g_g: E.activation(ta.t[:], g.t[:, 0:T], AF.Sigmoid), r=[g_g], w=[ta])
                        OP("dve", lambda E, ta=ta, g=g_y: E.tensor_tensor(ta.t[:], g.t[:, 0:T], ta.t[:], ALU.mult), r=[g_y, ta], w=[ta])
                        OP("dve", lambda E, ta=ta, dq=dq, dcc=dcc: E.tensor_tensor(mT.t[:, dq * 4 + dcc, :], ta.t[:], mpart.t[:, dcc, :], ALU.add),
                           r=[ta, mpart], w=[mT])

                if STOP == 15:
                    return finish()
                x1b = [xr.next(), xr.next()]
                for j in range(2):
                    LOAD("sp", x1b[j], x1b[j].t[:], x_d[r0 + j * 128:r0 + (j + 1) * 128, :], reads=[])
                for dh in range(2):
                    w_o_ = wload(wo_r, B_wo, dh * 512)
                    dsl = slice(dh * 512, (dh + 1) * 512)
                    for j in range(2):
                        g = Grot.next()
                        MM(g, g.t[:], [(mT.t[:, kc, j * 128:(j + 1) * 128], w_o_.t[:, kc, :]) for kc in range(KC)], [w_o_, mT])
                        t2 = yt2.next()
                        OP("dve", lambda E, g=g, t2=t2, dsl=dsl: E.tensor_tensor(t2.t[:], g.t[:], bct.t[:, GT1 + dsl.start:GT1 + dsl.stop], ALU.mult), r=[g, bct], w=[t2])
                        OP("dve", lambda E, t2=t2, xb_=x1b[j], dsl=dsl: E.tensor_tensor(xb_.t[:, dsl], xb_.t[:, dsl], t2.t[:], ALU.add), r=[t2, x1b[j]], w=[x1b[j]])
                for j in range(2):
                    tt = (r0 // 128) + j
                    xb = x1b[j]
                    S.dma("sp", lambda E, xb=xb, tt=tt: E.dma_start(out=x1_h[tt * 128:(tt + 1) * 128, :], in_=xb.t[:]), xb, reads=[xb], writes=[B_x1])
                    OP("act", lambda E, xb=xb: E.activation(junk.t[:], xb.t[:], AF.Square, accum_out=ss.t[:, 2:3]), r=[xb], w=[junk, ss])
                    OP("act", lambda E: E.activation(ss.t[:, 2:3], ss.t[:, 2:3], AF.Sqrt, bias=epst.t[:, 0:1], scale=1.0 / D), r=[ss, epst], w=[ss])
                    OP("dve", lambda E: E.reciprocal(rstd.t[:, 2:3], ss.t[:, 2:3]), r=[ss], w=[rstd])
                    OP("dve", lambda E, xb=xb: E.scalar_tensor_tensor(h2.t[:], xb.t[:], rstd.t[:, 2:3], bct.t[:, GS2:GS2 + D], ALU.mult, ALU.mult),
                       r=[xb, rstd, bct], w=[h2])
                    OP("dve", lambda E: E.tensor_tensor(h2.t[:], h2.t[:], bct.t[:, SH2:SH2 + D], ALU.add), r=[h2, bct], w=[h2])
                    hb = h2b.next()
                    OP("act", lambda E, hb=hb: E.activation(hb.t[:], h2.t[:], AF.Copy), r=[h2], w=[hb])
                    S.dma("sp", lambda E, hb=hb, tt=tt: E.dma_start(out=h2_h[tt * 128:(tt + 1) * 128, :], in_=hb.t[:]), hb, reads=[hb], writes=[B_h2])
                    for half in range(2):
                        g = Grot.next()
                        for c4 in range(4):
                            c = half * 4 + c4
                            TR(g, g.t[:, c4 * 128:(c4 + 1) * 128], h2.t[:, c * 128:(c + 1) * 128], ident, [h2])
                        OP("act", lambda E, g=g, half=half: E.activation(h2T.t[:, half * 4:half * 4 + 4, :], g.t[:].rearrange("p (c t) -> p c t", t=128), AF.Copy),
                           r=[g], w=[h2T])
                    g = Grot.next()
                    MM(g, g.t[:, 0:NE], [(h2T.t[:, kc, :], wr_sb.t[:, kc, :]) for kc in range(KC)], [h2T, wr_sb])
                    OP("dve", lambda E, g=g, tt=tt: E.tensor_tensor(logits.t[:, tt * NE:(tt + 1) * NE], g.t[:, 0:NE], br_bc.t[:], ALU.add), r=[g, br_bc], w=[logits])

        issue_casts(len(cast_jobs))
        issue_zero(len(zero_jobs))
        if STOP == 1:
            return finish()
        S.barrier()
        p1.close()
        open_stacks.remove(p1)

        p2a = ExitStack()
        open_stacks.append(p2a)
        top8 = S.sbuf("top8", [128, 8], F32, p2a)
        topi = S.sbuf("topi", [128, 8], U32, p2a)
        idxf = S.sbuf("idxf", [128, NTT * TOPK], F32, p2a)
        negm = S.sbuf("negm", [128, 1], F32, p2a)
        esum = S.sbuf("esum", [128, 2], F32, p2a)
        Mb = S.sbuf("Mb", [128, NE], BF16, p2a)
        rank = S.sbuf("rank", [128, NTT * NE], F32, p2a)
        runc = S.sbuf("runc", [128, NE], F32, p2a)
        ca = S.sbuf("ca", [128, NE], F32, p2a)
        cb = S.sbuf("cb", [128, NE], F32, p2a)
        ci = S.sbuf("ci", [128, NE], I32, p2a)
        padded = S.sbuf("padded", [128, NE], F32, p2a)
        pstart = S.sbuf("pstart", [128, NE], F32, p2a)
        base = S.sbuf("base", [128, NE], F32, p2a)
        tmpe = S.sbuf("tmpe", [128, NE], F32, p2a)
        destf = S.sbuf("destf", [128, NTT * TOPK], F32, p2a)
        bstart = S.sbuf("bstart", [128, NBLK], F32, p2a)
        be = S.sbuf("be", [128, NBLK], F32, p2a)
        pio = S.sbuf("pio", [128, KC], F32, p2a)
        wrow_f = S.sbuf("wrow_f", [128, NBLK, KC], F32, p2a)
        brow_f = S.sbuf("brow_f", [128, NBLK], F32, p2a)

        OP("dve", lambda E: E.memset(runc.t[:], 0.0), w=[runc])
        for tt in range(NTT):
            lg = logits.t[:, tt * NE:(tt + 1) * NE]
            OP("dve", lambda E, lg=lg: E.max(top8.t[:], lg), r=[logits], w=[top8])
            OP("dve", lambda E, lg=lg: E.max_index(topi.t[:], top8.t[:], lg), r=[logits, top8], w=[topi])
            OP("dve", lambda E, tt=tt: E.tensor_copy(idxf.t[:, tt * 4:tt * 4 + 4], topi.t[:, 0:4]), r=[topi], w=[idxf])
            OP("dve", lambda E: E.tensor_single_scalar(negm.t[:], top8.t[:, 0:1], -1.0, ALU.mult), r=[top8], w=[negm])
            OP("act", lambda E, tt=tt: E.activation(wts.t[:, tt * 4:tt * 4 + 4], top8.t[:, 0:4], AF.Exp, bias=negm.t[:, 0:1], scale=1.0,
                                                   accum_out=esum.t[:, 0:1]), r=[top8, negm], w=[wts, esum])
            OP("dve", lambda E: E.reciprocal(esum.t[:, 1:2], esum.t[:, 0:1]), r=[esum], w=[esum])
            OP("dve", lambda E, tt=tt: E.tensor_scalar(wts.t[:, tt * 4:tt * 4 + 4], wts.t[:, tt * 4:tt * 4 + 4], esum.t[:, 1:2], None, ALU.mult),
               r=[wts, esum], w=[wts])
            OP("dve", lambda E, tt=tt: E.tensor_scalar(Mb.t[:], iota32.t[:], idxf.t[:, tt * 4:tt * 4 + 1], None, ALU.is_equal), r=[iota32, idxf], w=[Mb])
            for k in range(1, 4):
                OP("dve", lambda E, tt=tt, k=k: E.scalar_tensor_tensor(Mb.t[:], iota32.t[:], idxf.t[:, tt * 4 + k:tt * 4 + k + 1], Mb.t[:], ALU.is_equal, ALU.add),
                   r=[iota32, idxf, Mb], w=[Mb])
            g = Grot.next()
            MM(g, g.t[:, 0:NE], [(tristb.t[:], Mb.t[:])], [tristb, Mb])
            MM(g, g.t[:, NE:2 * NE], [(onesb.t[:], Mb.t[:])], [onesb, Mb])
            OP("dve", lambda E, g=g, tt=tt: E.tensor_tensor(rank.t[:, tt * NE:(tt + 1) * NE], g.t[:, 0:NE], runc.t[:], ALU.add), r=[g, runc], w=[rank])
            OP("dve", lambda E, g=g: E.tensor_tensor(runc.t[:], g.t[:, NE:2 * NE], runc.t[:], ALU.add), r=[g, runc], w=[runc])
        sh = BLK.bit_length() - 1
        OP("dve", lambda E: E.tensor_copy(ci.t[:], runc.t[:]), r=[runc], w=[ci])
        OP("dve", lambda E: E.tensor_single_scalar(ci.t[:], ci.t[:], BLK - 1, ALU.add), r=[ci], w=[ci])
        OP("dve", lambda E: E.tensor_single_scalar(ci.t[:], ci.t[:], sh, ALU.arith_shift_right), r=[ci], w=[ci])
        OP("dve", lambda E: E.tensor_single_scalar(ci.t[:], ci.t[:], sh, ALU.logical_shift_left), r=[ci], w=[ci])
        OP("dve", lambda E: E.tensor_copy(padded.t[:], ci.t[:]), r=[ci], w=[padded])
        OP("dve", lambda E: E.tensor_copy(ca.t[:], padded.t[:]), r=[padded], w=[ca])
        src, dst = ca, cb
        s_ = 1
        while s_ < NE:
            OP("dve", lambda E, src=src, dst=dst, s_=s_: E.tensor_copy(dst.t[:, 0:s_], src.t[:, 0:s_]), r=[src], w=[dst])
            OP("dve", lambda E, src=src, dst=dst, s_=s_: E.tensor_tensor(dst.t[:, s_:NE], src.t[:, s_:NE], src.t[:, 0:NE - s_], ALU.add), r=[src], w=[dst])
            src, dst = dst, src
            s_ *= 2
        pend = src
        OP("dve", lambda E: E.tensor_tensor(pstart.t[:], pend.t[:], padded.t[:], ALU.subtract), r=[pend, padded], w=[pstart])
        for tt in range(NTT):
            OP("dve", lambda E, tt=tt: E.tensor_tensor(base.t[:], rank.t[:, tt * NE:(tt + 1) * NE], pstart.t[:], ALU.add), r=[rank, pstart], w=[base])
            for k in range(4):
                col = tt * 4 + k
                OP("dve", lambda E, col=col: E.scalar_tensor_tensor(tmpe.t[:], iota32.t[:], idxf.t[:, col:col + 1], base.t[:], ALU.is_equal, ALU.mult),
                   r=[iota32, idxf, base], w=[tmpe])
                OP("dve", lambda E, col=col: E.reduce_sum(destf.t[:, col:col + 1], tmpe.t[:], axis=AX.X), r=[tmpe], w=[destf])
        OP("dve", lambda E: E.tensor_copy(dest_i.t[:], destf.t[:]), r=[destf], w=[dest_i])
        OP("pool", lambda E: E.iota(bstart.t[:], pattern=[[BLK, NBLK]], base=0, channel_multiplier=0, allow_small_or_imprecise_dtypes=True), w=[bstart])
        OP("pool", lambda E: E.iota(pio.t[:], pattern=[[128, KC]], base=0, channel_multiplier=1, allow_small_or_imprecise_dtypes=True), w=[pio])
        OP("dve", lambda E: E.memset(be.t[:], 0.0), w=[be])
        for e in range(NE):
            OP("dve", lambda E, e=e: E.scalar_tensor_tensor(be.t[:], bstart.t[:], pend.t[:, e:e + 1], be.t[:], ALU.is_ge, ALU.add), r=[bstart, pend, be], w=[be])
        OP("dve", lambda E: E.tensor_single_scalar(be.t[:], be.t[:], float(NE - 1), ALU.min), r=[be], w=[be])
        for kc in range(KC):
            OP("dve", lambda E, kc=kc: E.tensor_scalar(wrow_f.t[:, :, kc], be.t[:], float(D), pio.t[:, kc:kc + 1], ALU.mult, ALU.add), r=[be, pio], w=[wrow_f])
        OP("dve", lambda E: E.tensor_copy(wrow.t[:], wrow_f.t[:].rearrange("p b k -> p (b k)")), r=[wrow_f], w=[wrow])
        OP("dve", lambda E: E.tensor_scalar(brow_f.t[:], be.t[:], 128.0, pio.t[:, 0:1], ALU.mult, ALU.add), r=[be, pio], w=[brow_f])
        OP("dve", lambda E: E.tensor_copy(brow.t[:], brow_f.t[:]), r=[brow_f], w=[brow])
        OP("dve", lambda E: E.tensor_copy(erow.t[:], be.t[:]), r=[be], w=[erow])

        if STOP == 2:
            return finish()
        S.barrier()
        p2a.close()
        plog.close()
        open_stacks.remove(p2a)
        open_stacks.remove(plog)
        p3 = ExitStack()
        open_stacks.append(p3)
        hrows = Rot([S.sbuf(f"hrows{i}", [128, D], BF16, p3) for i in range(6)])
        for tt in range(NTT):
            hb = hrows.next()
            LOAD("sp", hb, hb.t[:], h2_h[tt * 128:(tt + 1) * 128, :], reads=[B_h2])
            for k in range(4):
                col = tt * 4 + k
                S.dma("pool", lambda E, hb=hb, col=col: E.indirect_dma_start(
                    out=xs_h, out_offset=bass.IndirectOffsetOnAxis(ap=dest_i.t[:, col:col + 1], axis=0),
                    in_=hb.t[:, :], in_offset=None), B_xs, reads=[hb, dest_i, B_xz], writes=[])
        B_xs.w = ("d", B_xs)

        if STOP == 3:
            return finish()
        wunit = Rot([S.sbuf(f"wunit{i}", [128, KC, D], BF16, p3) for i in range(6)])
        bgu_sb = Rot([S.sbuf(f"bgu_sb{i}", [128, 16], F32, p3) for i in range(2)])
        bd_sb = Rot([S.sbuf(f"bd_sb{i}", [128, D], F32, p3) for i in range(2)])
        xrows = Rot([S.sbuf(f"xrows{i}", [128, 4, D], BF16, p3) for i in range(2)])
        gcl = Rot([S.sbuf(f"gcl{i}", [128, BLK], F32, p3) for i in range(2)])
        sgm = Rot([S.sbuf(f"sgm{i}", [128, BLK], F32, p3) for i in range(2)])
        upc = Rot([S.sbuf(f"upc{i}", [128, BLK], F32, p3) for i in range(2)])
        ysb = Rot([S.sbuf(f"ysb{i}", [128, D], F32, p3) for i in range(3)])
        xTs = Rot([S.sbuf(f"xT{i}", [128, KC, BLK], BF16, p3) for i in range(2)])
        actTs = Rot([S.sbuf(f"actT{i}", [128, KC, BLK], BF16, p3) for i in range(2)])

        def blk_loads(blk):
            wgg = wunit.next()
            wgup = wunit.next()
            wdn = wunit.next()
            bg = bgu_sb.next()
            bd = bd_sb.next()
            for (wdst, wsrc, bb) in ((wgg, wg_bf, B_wgu), (wgup, wu_bf, B_wgu), (wdn, wd_bf, B_wd)):
                S.dma("pool", lambda E, wdst=wdst, wsrc=wsrc, blk=blk: E.indirect_dma_start(
                    out=wdst.t[:].rearrange("p k n -> p (k n)"), out_offset=None, in_=wsrc,
                    in_offset=bass.IndirectOffsetOnAxis(ap=brow.t[:, blk:blk + 1], axis=0)), wdst, reads=[bb, brow], writes=[wdst])
            S.dma("pool", lambda E, bg=bg, blk=blk: E.indirect_dma_start(
                out=bg.t[:, :], out_offset=None, in_=bgu_l_d,
                in_offset=bass.IndirectOffsetOnAxis(ap=brow.t[:, blk:blk + 1], axis=0)), bg, reads=[brow], writes=[bg])
            S.dma("pool", lambda E, bd=bd, blk=blk: E.indirect_dma_start(
                out=bd.t[:, :], out_offset=None, in_=bd_d,
                in_offset=bass.IndirectOffsetOnAxis(ap=erow.t[:, blk:blk + 1], axis=0)), bd, reads=[erow], writes=[bd])
            xw = xrows.next()
            LOAD("sp", xw, xw.t[:], xs_h[blk * BLK:(blk + 1) * BLK, :].rearrange("(r p) d -> p r d", p=128), reads=[B_xs])
            return dict(wgg=wgg, wgup=wgup, wdn=wdn, bg=bg, bd=bd, xw=xw)

        def blk_transposes(L):
            xw = L["xw"]
            xT = xTs.next()
            for half in range(2):
                for c4 in range(4):
                    kc = half * 4 + c4
                    for r in range(4):
                        TR(ptb, ptb.t[:, c4 * 512 + r * 128:c4 * 512 + (r + 1) * 128], xw.t[:, r, kc * 128:(kc + 1) * 128], identb, [xw])
                OP("act", lambda E, half=half, xT=xT: E.activation(xT.t[:, half * 4:half * 4 + 4, :], ptb.t[:].rearrange("p (c t) -> p c t", t=BLK), AF.Copy),
                   r=[ptb], w=[xT])
            L["xT"] = xT

        def blk_gu(L):
            wgg, wgup, bg, xT = L["wgg"], L["wgup"], L["bg"], L["xT"]
            actT = actTs.next()
            L["actT"] = actT
            for f in range(KC):
                g_g = Grot.next()
                MM(g_g, g_g.t[:], [(wgg.t[:, kc, f * 128:(f + 1) * 128], xT.t[:, kc, :]) for kc in range(KC)], [wgg, xT])
                g_u = Grot.next()
                MM(g_u, g_u.t[:], [(wgup.t[:, kc, f * 128:(f + 1) * 128], xT.t[:, kc, :]) for kc in range(KC)], [wgup, xT])
                gc_, sg_, up_ = gcl.next(), sgm.next(), upc.next()
                OP("dve", lambda E, g=g_g, gc_=gc_, f=f, bg=bg: E.tensor_scalar(gc_.t[:], g.t[:], bg.t[:, f:f + 1], 7.0, ALU.add, ALU.min), r=[g_g, bg], w=[gc_])
                OP("act", lambda E, gc_=gc_, sg_=sg_: E.activation(sg_.t[:], gc_.t[:], AF.Sigmoid, scale=1.702), r=[gc_], w=[sg_])
                OP("dve", lambda E, g=g_u, up_=up_, f=f, bg=bg: E.tensor_scalar(up_.t[:], g.t[:], bg.t[:, 8 + f:9 + f], 7.0, ALU.add, ALU.min), r=[g_u, bg], w=[up_])
                OP("dve", lambda E, up_=up_: E.tensor_scalar(up_.t[:], up_.t[:], -7.0, 1.0, ALU.max, ALU.add), r=[up_], w=[up_])
                OP("dve", lambda E, gc_=gc_, sg_=sg_: E.tensor_tensor(gc_.t[:], gc_.t[:], sg_.t[:], ALU.mult), r=[gc_, sg_], w=[gc_])
                OP("dve", lambda E, gc_=gc_, up_=up_, f=f, actT=actT: E.tensor_tensor(actT.t[:, f, :], gc_.t[:], up_.t[:], ALU.mult), r=[gc_, up_], w=[actT])

        def blk_down(L, blk):
            wdn, bd, actT = L["wdn"], L["bd"], L["actT"]
            for r in range(4):
                yb = ysb.next()
                for dh in range(2):
                    g = Grot.next()
                    MM(g, g.t[:], [(actT.t[:, fc, r * 128:(r + 1) * 128], wdn.t[:, fc, dh * 512:(dh + 1) * 512]) for fc in range(KC)], [actT, wdn])
                    OP("dve", lambda E, g=g, yb=yb, dh=dh, bd=bd: E.tensor_tensor(yb.t[:, dh * 512:(dh + 1) * 512], g.t[:], bd.t[:, dh * 512:(dh + 1) * 512], ALU.add),
                       r=[g, bd], w=[yb])
                row0 = blk * BLK + r * 128
                S.dma("sp", lambda E, yb=yb, row0=row0: E.dma_start(out=ys_h[row0:row0 + 128, :], in_=yb.t[:]), yb, reads=[yb], writes=[])

        cur = blk_loads(0)
        blk_transposes(cur)
        for blk in range(NBLK):
            nxt = blk_loads(blk + 1) if blk + 1 < NBLK else None
            blk_gu(cur)
            if nxt is not None:
                blk_transposes(nxt)
            blk_down(cur, blk)
            cur = nxt

        if STOP == 4:
            return finish()
        S.barrier()
        p3.close()
        open_stacks.remove(p3)

        p5 = ExitStack()
        open_stacks.append(p5)
        gfin_bc = S.sbuf("gfin_bc", [128, D], F32, p5)
        LOAD("sp", gfin_bc, gfin_bc.t[:], gfin_bc_d)
        x1r = Rot([S.sbuf(f"x1r{i}", [128, D], F32, p5) for i in range(4)])
        ygat = Rot([S.sbuf(f"ygat{i}", [128, D], F32, p5) for i in range(12)])
        accb = Rot([S.sbuf(f"accb{i}", [128, D], F32, p5) for i in range(4)])
        junk5 = S.sbuf("junk5", [128, D], BF16, p5)
        bct5 = S.sbuf("bct5", [128, D], F32, p5)
        M.update({"wadab": Rot([S.sbuf(f"wadab5{i}", [128, KC, 128], F32, p5) for i in range(2)]),
                  "badab": Rot([S.sbuf(f"badab5{i}", [128, 128], F32, p5) for i in range(2)]),
                  "condrep": S.sbuf("condrep5", [128, KC, 128], F32, p5), "bct": bct5, "off": 24})
        ss5 = S.sbuf("ss5", [128, 2], F32, p5)
        for b in range(NSEQ):
            mod_bc(b, range(24, 32))
            for it in range(SEQ // 128):
                tt = b * (SEQ // 128) + it
                xb = x1r.next()
                LOAD("sp", xb, xb.t[:], x1_h[tt * 128:(tt + 1) * 128, :], reads=[B_x1])
                acc = accb.next()
                for k in range(4):
                    col = tt * 4 + k
                    yk = ygat.next()
                    S.dma("pool", lambda E, yk=yk, col=col: E.indirect_dma_start(
                        out=yk.t[:, :], out_offset=None, in_=ys_h,
                        in_offset=bass.IndirectOffsetOnAxis(ap=dest_i.t[:, col:col + 1], axis=0)), yk, reads=[B_ys, dest_i], writes=[yk])
                    if k == 0:
                        OP("dve", lambda E, yk=yk, acc=acc, col=col: E.tensor_scalar(acc.t[:], yk.t[:], wts.t[:, col:col + 1], None, ALU.mult), r=[yk, wts], w=[acc])
                    else:
                        eng = "dve"
                        OP(eng, lambda E, yk=yk, acc=acc, col=col: E.scalar_tensor_tensor(acc.t[:], yk.t[:], wts.t[:, col:col + 1], acc.t[:], ALU.mult, ALU.add),
                           r=[yk, wts, acc], w=[acc])
                OP("dve", lambda E, acc=acc: E.tensor_tensor(acc.t[:], acc.t[:], bct5.t[:, GT2:GT2 + D], ALU.mult), r=[acc, bct5], w=[acc])
                OP("pool", lambda E, acc=acc, xb=xb: E.tensor_tensor(acc.t[:], acc.t[:], xb.t[:], ALU.add), r=[acc, xb], w=[acc])
                OP("act", lambda E, acc=acc: E.activation(junk5.t[:], acc.t[:], AF.Square, accum_out=ss5.t[:, 0:1]), r=[acc], w=[junk5, ss5])
                OP("act", lambda E: E.activation(ss5.t[:, 0:1], ss5.t[:, 0:1], AF.Sqrt, bias=epst.t[:, 0:1], scale=1.0 / D), r=[ss5, epst], w=[ss5])
                OP("dve", lambda E: E.reciprocal(ss5.t[:, 1:2], ss5.t[:, 0:1]), r=[ss5], w=[ss5])
                OP("dve", lambda E, acc=acc: E.scalar_tensor_tensor(acc.t[:], acc.t[:], ss5.t[:, 1:2], gfin_bc.t[:], ALU.mult, ALU.mult), r=[acc, ss5, gfin_bc], w=[acc])
                S.dma("sp", lambda E, acc=acc, tt=tt: E.dma_start(out=out_d[tt * 128:(tt + 1) * 128, :], in_=acc.t[:]), acc, reads=[acc], writes=[])
        return finish()
    return nc


def _prep_shared(inp):
    f = lambda a: np.ascontiguousarray(np.asarray(a, dtype=np.float32))
    rep = lambda v: f(np.broadcast_to(np.asarray(v, np.float32).reshape(1, -1), (128, np.asarray(v).size)))
    fm = lambda v: f(np.asarray(v, np.float32).reshape(-1, 128).T)
    l = 0
    b_ada = np.asarray(inp["b_ada"][l], np.float32)
    d = {
        "w_ada": f(inp["w_ada"][l]),
        "b_ada_fm": fm(b_ada[:2 * D]),
        "b_ada_bc": rep(b_ada[2 * D:]),
        "g_mix_fm": fm(inp["g_mix"][l]),
        "g_ffn_bc": rep(inp["g_ffn"][l]),
        "g_final_bc": rep(inp["g_final"]),
        "w_in": f(inp["w_in"][l]),
        "w_sconv_fm": f(np.asarray(inp["w_sconv"][l], np.float32).reshape(3, KC, 128).transpose(2, 1, 0).reshape(128, KC * 3)),
        "w_sconv_out": f(inp["w_sconv_out"][l]),
        "w_ssm_conv_fm": f(np.asarray(inp["w_ssm_conv"][l], np.float32).reshape(4, 24, 128).transpose(2, 1, 0).reshape(128, 96)),
        "b_ssm_conv_fm": fm(inp["b_ssm_conv"][l]),
        "dt_bias_bc": rep(inp["dt_bias"][l]),
        "a_log_bc": rep(inp["a_log"][l]),
        "d_skip_bc": rep(inp["d_skip"][l]),
        "g_ssm_bc": rep(inp["g_ssm_norm"][l]),
        "w_ssm_out": f(inp["w_ssm_out"][l]),
        "w_o": f(inp["w_o"][l]),
        "w_router": f(inp["w_router"][l]),
        "b_router_bc": rep(inp["b_router"][l]),
        "w_gu": f(np.asarray(inp["w_gu"][l], np.float32).reshape(NE * D, 2 * D)),
        "b_gu_l": f(np.asarray(inp["b_gu"][l], np.float32).reshape(NE, 16, 128).transpose(0, 2, 1).reshape(NE * 128, 16)),
        "w_down": f(np.asarray(inp["w_down"][l], np.float32).reshape(NE * D, D)),
        "b_down": f(inp["b_down"][l]),
    }
    return d


_NC_CACHE = {}
_STOP = 99
_JLIST = (0, 1)


def kernel(**inputs):
    x = np.asarray(inputs["x"], np.float32)
    c = np.asarray(inputs["c"], np.float32)
    bsz, seq, _ = x.shape
    ncores = 8
    nseq = bsz // ncores
    key = (nseq, seq)
    if key not in _NC_CACHE:
        _NC_CACHE[key] = build_nc(nseq, seq, _STOP)
    nc = _NC_CACHE[key]
    shared = _prep_shared(inputs)
    in_maps = []
    for i in range(ncores):
        m = dict(shared)
        m["x"] = np.ascontiguousarray(x[i * nseq:(i + 1) * nseq].reshape(nseq * seq, D))
        cc = c[i * nseq:(i + 1) * nseq]
        m["cT"] = np.ascontiguousarray(cc.reshape(nseq, KC, 128).transpose(2, 1, 0).reshape(128, KC * nseq))
        in_maps.append(m)
    res = run_bass_kernel_spmd(nc, in_maps, core_ids=list(range(ncores)))
    out = np.concatenate([np.asarray(r["out"]).reshape(nseq, seq, D) for r in res.results], axis=0)
    return out.astype(np.float32)
```
